# Optimizing a Trainium2 kernel written in Bass

```python
import jax, jax.numpy as jnp
from jax import lax
import numpy as np

D_MODEL = 2048
BATCH = 2
SEQ = 16384
DEPTH = 1

N_META = 16
GRID_W = 64
HEAD_DIM = 128
N_Q_HEADS = 8
N_KV_HEADS = 2
Q_PER_KV = N_Q_HEADS // N_KV_HEADS
ATTN_WIDTH = N_Q_HEADS * HEAD_DIM
KV_WIDTH = N_KV_HEADS * HEAD_DIM
MIX_WIDTH = D_MODEL
POOL_WIDTH = MIX_WIDTH - ATTN_WIDTH
POOL_WINDOWS = (2, 4, 8, 16)
N_POOL_GROUPS = len(POOL_WINDOWS)
POOL_GROUP = POOL_WIDTH // N_POOL_GROUPS
IN_WIDTH = ATTN_WIDTH + 2 * KV_WIDTH + POOL_WIDTH
Q_BLOCK = 128
ROPE_THETA = 10000.0
ROPE_AXIS_DIM = HEAD_DIM // 2
N_EXPERT_GROUPS = 4
EXPERTS_PER_GROUP = 8
N_EXPERTS = N_EXPERT_GROUPS * EXPERTS_PER_GROUP
TOP_K = 2
D_EXPERT = D_MODEL // 4
EXPERT_BLOCK = 128
EPS = 1e-6

kernel_name = "hymba_pool_axialgqa_hiermoe_encoder"


def _rms_norm(x, g):
    xf = x.astype(jnp.float32)
    y = xf * lax.rsqrt(jnp.mean(xf * xf, axis=-1, keepdims=True) + EPS)
    return (y * g.astype(jnp.float32)).astype(x.dtype)


def _axial_angles(n_tok):
    rows = n_tok // GRID_W
    t = jnp.arange(rows * GRID_W)
    zeros = jnp.zeros((N_META,), jnp.float32)
    row = jnp.concatenate([zeros, (t // GRID_W).astype(jnp.float32)])
    col = jnp.concatenate([zeros, (t % GRID_W).astype(jnp.float32)])
    inv_freq = jnp.power(ROPE_THETA, -jnp.arange(0, ROPE_AXIS_DIM, 2, dtype=jnp.float32) / ROPE_AXIS_DIM)
    ang_r = row[:, None] * inv_freq[None, :]
    ang_c = col[:, None] * inv_freq[None, :]
    return jnp.cos(ang_r), jnp.sin(ang_r), jnp.cos(ang_c), jnp.sin(ang_c)


def _rotate(x, cos, sin):
    half = x.shape[-1] // 2
    x1, x2 = x[..., :half], x[..., half:]
    c = cos[None, :, None, :]
    s = sin[None, :, None, :]
    return jnp.concatenate([x1 * c - x2 * s, x2 * c + x1 * s], axis=-1)


def _axial_rope(x, rope):
    cr, sr, cc, sc = rope
    xf = x.astype(jnp.float32)
    out = jnp.concatenate([_rotate(xf[..., :ROPE_AXIS_DIM], cr, sr),
                           _rotate(xf[..., ROPE_AXIS_DIM:], cc, sc)], axis=-1)
    return out.astype(x.dtype)


def _axial_gqa(q, k, v, q_g, k_g, rope):
    B, L, _ = q.shape
    q = _axial_rope(_rms_norm(q.reshape(B, L, N_Q_HEADS, HEAD_DIM), q_g), rope)
    k = _axial_rope(_rms_norm(k.reshape(B, L, N_KV_HEADS, HEAD_DIM), k_g), rope)
    v = v.reshape(B, L, N_KV_HEADS, HEAD_DIM)
    n_blk = -(-L // Q_BLOCK)
    Lp = n_blk * Q_BLOCK
    q = jnp.pad(q, ((0, 0), (0, Lp - L), (0, 0), (0, 0)))
    qb = q.reshape(B, n_blk, Q_BLOCK, N_KV_HEADS, Q_PER_KV, HEAD_DIM).transpose(1, 0, 2, 3, 4, 5)
    scale = HEAD_DIM ** -0.5

    def block(q_blk):
        s = jnp.einsum('bqkgd,bskd->bkgqs', q_blk, k).astype(jnp.float32) * scale
        p = jax.nn.softmax(s, axis=-1).astype(v.dtype)
        return jnp.einsum('bkgqs,bskd->bqkgd', p, v)

    o = lax.map(block, qb)
    o = o.transpose(1, 0, 2, 3, 4, 5).reshape(B, Lp, ATTN_WIDTH)
    return o[:, :L]


def _multiscale_pool(p, w, scale):
    B, L, C = p.shape
    pf = p.astype(jnp.float32)
    cs = jnp.pad(jnp.cumsum(pf, axis=1), ((0, 0), (1, 0), (0, 0)))
    t = jnp.arange(L)
    outs = []
    for gi, win in enumerate(POOL_WINDOWS):
        lo_c, hi_c = gi * POOL_GROUP, (gi + 1) * POOL_GROUP
        lo = jnp.clip(t - win // 2, 0, L)
        hi = jnp.clip(t - win // 2 + win, 0, L)
        csg = cs[:, :, lo_c:hi_c]
        mean = (csg[:, hi] - csg[:, lo]) / (hi - lo).astype(jnp.float32)[None, :, None]
        outs.append(mean - pf[:, :, lo_c:hi_c])
    m = jnp.stack(outs, axis=2).astype(p.dtype)
    y = jnp.einsum('blgc,gcd->blgd', m, w).reshape(B, L, C)
    return y * scale


def _hier_moe(xs, w_rg, b_rg, w_re, b_re, w_gate, w_up, w_down):
    N, D = xs.shape
    g_prob = jax.nn.softmax((xs @ w_rg).astype(jnp.float32) + b_rg.astype(jnp.float32), axis=-1)
    g_p, g_idx = lax.top_k(g_prob, 1)
    e_logits = ((xs @ w_re).astype(jnp.float32) + b_re.astype(jnp.float32)).reshape(N, N_EXPERT_GROUPS, EXPERTS_PER_GROUP)
    sel = jnp.broadcast_to(g_idx[:, :, None], (N, 1, EXPERTS_PER_GROUP))
    e_prob = jax.nn.softmax(jnp.take_along_axis(e_logits, sel, axis=1)[:, 0], axis=-1)
    e_p, e_idx = lax.top_k(e_prob, TOP_K)
    gates = g_p * (e_p / jnp.sum(e_p, axis=-1, keepdims=True))
    expert = g_idx * EXPERTS_PER_GROUP + e_idx

    M = N * TOP_K
    flat_e = expert.reshape(-1)
    flat_g = gates.reshape(-1)
    flat_t = jnp.repeat(jnp.arange(N, dtype=jnp.int32), TOP_K)
    order = jnp.argsort(flat_e)
    s_e, s_t, s_g = flat_e[order], flat_t[order], flat_g[order]
    counts = jnp.bincount(flat_e, length=N_EXPERTS)
    starts = jnp.cumsum(counts) - counts
    padded = (counts + EXPERT_BLOCK - 1) // EXPERT_BLOCK * EXPERT_BLOCK
    pstarts = jnp.cumsum(padded) - padded
    pends = pstarts + padded
    dest = pstarts[s_e] + (jnp.arange(M) - starts[s_e])
    P = (-(-M // EXPERT_BLOCK) + N_EXPERTS) * EXPERT_BLOCK
    n_blk = P // EXPERT_BLOCK
    slot_t = jnp.zeros((P,), jnp.int32).at[dest].set(s_t)
    slot_g = jnp.zeros((P,), jnp.float32).at[dest].set(s_g)
    blk_start = jnp.arange(n_blk) * EXPERT_BLOCK
    blk_e = jnp.minimum(jnp.sum(pends[None, :] <= blk_start[:, None], axis=1), N_EXPERTS - 1)

    def expert_block(args):
        tok, g, e = args
        xb = xs[tok]
        h = jax.nn.silu(xb @ w_gate[e]) * (xb @ w_up[e])
        return (h @ w_down[e]) * g[:, None].astype(xs.dtype)

    out = lax.map(expert_block, (slot_t.reshape(n_blk, EXPERT_BLOCK),
                                 slot_g.reshape(n_blk, EXPERT_BLOCK), blk_e))
    return jnp.zeros_like(xs).at[slot_t].add(out.reshape(P, D))


def setup_inputs(seed: int = 0) -> dict:
    key = jax.random.key(seed)
    ks = jax.random.split(key, 20)
    f = jnp.float32
    nrm = lambda k, shape, s: jax.random.normal(k, shape, f) * s
    return {
        "x": nrm(ks[0], (BATCH, SEQ, D_MODEL), 1.0),
        "meta_tokens": nrm(ks[1], (N_META, D_MODEL), 1.0),
        "norm1_g": 1.0 + nrm(ks[2], (DEPTH, D_MODEL), 0.02),
        "w_in": nrm(ks[3], (DEPTH, D_MODEL, IN_WIDTH), D_MODEL ** -0.5),
        "q_norm_g": 1.0 + nrm(ks[4], (DEPTH, HEAD_DIM), 0.02),
        "k_norm_g": 1.0 + nrm(ks[5], (DEPTH, HEAD_DIM), 0.02),
        "pool_w": nrm(ks[6], (DEPTH, N_POOL_GROUPS, POOL_GROUP, POOL_GROUP), POOL_GROUP ** -0.5),
        "pool_scale": 1.0 + nrm(ks[7], (DEPTH, POOL_WIDTH), 0.02),
        "w_out": nrm(ks[8], (DEPTH, MIX_WIDTH, D_MODEL), MIX_WIDTH ** -0.5),
        "norm2_g": 1.0 + nrm(ks[9], (DEPTH, D_MODEL), 0.02),
        "w_router_group": nrm(ks[10], (DEPTH, D_MODEL, N_EXPERT_GROUPS), D_MODEL ** -0.5),
        "b_router_group": nrm(ks[11], (DEPTH, N_EXPERT_GROUPS), 0.01),
        "w_router_expert": nrm(ks[12], (DEPTH, D_MODEL, N_EXPERTS), D_MODEL ** -0.5),
        "b_router_expert": nrm(ks[13], (DEPTH, N_EXPERTS), 0.01),
        "w_gate": nrm(ks[14], (DEPTH, N_EXPERTS, D_MODEL, D_EXPERT), D_MODEL ** -0.5),
        "w_up": nrm(ks[15], (DEPTH, N_EXPERTS, D_MODEL, D_EXPERT), D_MODEL ** -0.5),
        "w_down": nrm(ks[16], (DEPTH, N_EXPERTS, D_EXPERT, D_MODEL), D_EXPERT ** -0.5),
    }


def reference(x, meta_tokens, norm1_g, w_in, q_norm_g, k_norm_g, pool_w, pool_scale, w_out,
              norm2_g, w_router_group, b_router_group, w_router_expert, b_router_expert,
              w_gate, w_up, w_down):
    B, S, D = x.shape
    meta = jnp.broadcast_to(meta_tokens[None].astype(x.dtype), (B, N_META, D))
    h = jnp.concatenate([meta, x], axis=1)
    L = h.shape[1]
    rope = _axial_angles(S)
    q_end = ATTN_WIDTH
    k_end = q_end + KV_WIDTH
    v_end = k_end + KV_WIDTH
    for l in range(DEPTH):
        a = _rms_norm(h, norm1_g[l])
        proj = a @ w_in[l]
        attn = _axial_gqa(proj[..., :q_end], proj[..., q_end:k_end], proj[..., k_end:v_end],
                          q_norm_g[l], k_norm_g[l], rope)
        pool = _multiscale_pool(proj[..., v_end:], pool_w[l], pool_scale[l])
        h = h + jnp.concatenate([attn, pool], axis=-1) @ w_out[l]
        b = _rms_norm(h, norm2_g[l]).reshape(B * L, D)
        h = h + _hier_moe(b, w_router_group[l], b_router_group[l], w_router_expert[l],
                          b_router_expert[l], w_gate[l], w_up[l], w_down[l]).reshape(B, L, D)
    return h[:, N_META:]
```

```python
from contextlib import ExitStack
import ml_dtypes
from concourse.bass_utils import run_bass_kernel_spmd
import numpy as np
import concourse.bass as bass
import concourse.mybir as mybir
F32=mybir.dt.float32; BF16=mybir.dt.bfloat16; I32=mybir.dt.int32
AF=mybir.ActivationFunctionType; ALU=mybir.AluOpType; AX=mybir.AxisListType

class EngQ:
    def __init__(self, nc, es, name, e):
        self.name=name; self.e=e; self.h=es.enter_context(nc.semaphore("tl_"+name)); self.cnt=0; self.seen={}; self.uid="tl_"+name
    def wait(self, *evs):
        for ev in evs:
            if ev is None: continue
            if isinstance(ev, (list,tuple)) and len(ev)>0 and not hasattr(ev[0],'h'):
                self.wait(*ev); continue
            src,val=ev
            if src is self and False: pass
            if self.seen.get(src.uid,0)>=val: continue
            self.e.wait_ge(src.h,val); self.seen[src.uid]=val
    def sig(self, ins):
        self.cnt+=1; ins.then_inc(self.h,1); return (self,self.cnt)

class DSem:
    def __init__(self, nc, es, name):
        self.h=es.enter_context(nc.semaphore(name)); self.cnt=0; self.uid=name

class K:
    def __init__(self, nc, es):
        self.nc=nc; self.es=es
        self.pe=EngQ(nc,es,"pe",nc.tensor); self.dve=EngQ(nc,es,"dve",nc.vector)
        self.act=EngQ(nc,es,"act",nc.scalar); self.pool=EngQ(nc,es,"pool",nc.gpsimd); self.sp=EngQ(nc,es,"sp",nc.sync)
        self.engs=[self.pe,self.dve,self.act,self.pool,self.sp]
        self._n=0; self.pes=es
    def sb(self, shape, dt, name=None):
        self._n+=1; return self.pes.enter_context(self.nc.sbuf_tensor("s_"+(name or f"sb{self._n}"), shape, dt))
    def ps(self, shape, dt, name=None):
        self._n+=1; return self.pes.enter_context(self.nc.psum_tensor("p_"+(name or f"ps{self._n}"), shape, dt))
    def dsem(self, name=None):
        self._n+=1; return DSem(self.nc,self.es,name or f"ds{self._n}")
    def dma(self, q, sem, out, in_, deps=(), **kw):
        q.wait(*deps)
        ins=q.e.dma_start(out=out,in_=in_,**kw); ins.then_inc(sem.h,16); sem.cnt+=16
        return (sem,sem.cnt)
    def barrier(self, extra=()):
        last=[(q,q.cnt) for q in self.engs if q.cnt>0]
        for q in self.engs:
            for ev in last:
                q.wait(ev)
            q.wait(*extra)
    def begin_phase(self):
        from contextlib import ExitStack
        self.pes=ExitStack(); self.pes.__enter__()
    def end_phase(self, extra=()):
        self.barrier(extra)
        self.pes.__exit__(None,None,None); self.pes=self.es

BF=ml_dtypes.bfloat16
N_META=16; GRID_W=64; L=16400; S=16384
def rope_tables(pos_row, pos_col):
    inv=np.power(10000.0,-np.arange(0,64,2,dtype=np.float32)/64).astype(np.float32)
    ar=pos_row[:,None].astype(np.float32)*inv[None,:]; ac=pos_col[:,None].astype(np.float32)*inv[None,:]
    cr,sr,cc,sc=np.cos(ar),np.sin(ar),np.cos(ac),np.sin(ac)
    C=np.concatenate([cr,cr,cc,cc],1).astype(np.float32); Sg=np.concatenate([-sr,sr,-sc,sc],1).astype(np.float32)
    return C,Sg
def band_mats(j, NQT):
    A=np.zeros((6,4,128,128),np.float32)
    wins=(2,4,8,16)
    for g,w in enumerate(wins):
        for out in range(128):
            lo=out-w//2; hi=out+w//2-1
            for off in range(lo,hi+1):
                if 0<=off<128: A[0,g,off,out]+=1.0/w
                elif off<0:
                    A[1,g,128+off,out]+=1.0/w
                    A[3,g,8+off,out]+=1.0/w
                else:
                    A[2,g,off-128,out]+=1.0/w
                    A[4,g,8+(off-128),out]+=1.0/w
            A[0,g,out,out]-=1.0
        A[5,g]=A[0,g]
        if j==3:
            for out in range(128):
                t=16+S-128+out
                lo_t=max(t-w//2,0); hi_t=min(t-w//2+w,L)
                cnt=hi_t-lo_t
                if cnt!=w:
                    A[5,g,:,out]=0
                    for tt in range(lo_t,hi_t):
                        off=tt-(16+S-128)
                        if 0<=off<128: A[5,g,off,out]+=1.0/cnt
                    A[5,g,out,out]-=1.0
    return np.ascontiguousarray(A.reshape(24,128,128).transpose(1,0,2)).astype(BF)


def prep_consts(k, ident_src=None):
    nc=k.nc
    c={}
    c['ones_bf']=k.sb([128,128],BF16,"c_ones_bf")
    c['ident_bf']=k.sb([128,128],BF16,"c_ident_bf")
    c['ident_f']=k.sb([128,128],F32,"c_ident_f")
    return c

def phase_a(k, C, d, NKVT, NQT):
    nc=k.nc; pe=k.pe; act=k.act; dve=k.dve; pool=k.pool; sp=k.sp
    EPS=1e-6
    wsb=k.sb([128,16,2560],BF16,"w_in_bf")
    wst=[k.sb([128,2560],F32,f"wst{i}") for i in range(2)]
    g1=k.sb([128,16],F32,"g1t");
    gqr=k.sb([128,128],F32,"gqr"); gkr=k.sb([128,128],F32,"gkr")
    pw=k.sb([128,8,256],BF16,"pool_w_bf"); pwst=k.sb([128,8,256],F32,"pool_w_st")
    psc=k.sb([128,8],F32,"pool_sc")
    Aband=k.sb([128,24,128],BF16,"Aband")
    cs=k.dsem("a_const"); wl=[k.dsem(f"a_wl{i}") for i in range(2)]
    k.dma(sp,cs,g1[:],d['g1t'][:,:]); k.dma(sp,cs,gqr[:],d['gqr'][:,:]); k.dma(sp,cs,gkr[:],d['gkr'][:,:])
    k.dma(sp,cs,pwst[:],d['pool_w'][:,:,:]); k.dma(sp,cs,psc[:],d['pool_sc'][:,:]);
    ev_c=k.dma(sp,cs,Aband[:],d['Aband'][:,:,:])
    ident=C['ident_bf']
    epsb=k.sb([128,1],F32,"epsb"); e_eps=dve.sig(nc.vector.memset(epsb[:],EPS)); act.wait(e_eps)
    k.dma(sp,cs,ident[:],d['ident_bf'][:,:]); ev_c=k.dma(sp,cs,C['ident_f'][:],d['ident_f'][:,:])
    dve.wait(ev_c)
    ev_pw=dve.sig(nc.vector.tensor_copy(out=pw[:],in_=pwst[:]))
    wev=[None,None]; cev=[None,None]; w_done=None
    for c in range(16):
        s=c%2
        wev[s]=k.dma(sp,wl[s],wst[s][:],d['w_in'][c*128:(c+1)*128,:],deps=[cev[s]])
        dve.wait(wev[s])
        cev[s]=dve.sig(nc.vector.tensor_scalar(out=wsb[:,c,:],in0=wst[s][:],scalar1=g1[:,c:c+1],scalar2=None,op0=ALU.mult))
    w_done=cev
    xt=[k.sb([128,2048],F32,f"xt{i}") for i in range(2)]
    xb=[k.sb([128,2048],BF16,f"xb{i}") for i in range(2)]
    xT=[k.sb([128,16,128],BF16,f"xT{i}") for i in range(2)]
    junk=k.sb([128,2048],BF16,"junk")
    ssq=[k.sb([128,1],F32,f"ssq{i}") for i in range(2)]
    rstd=[k.sb([128,1],F32,f"rstd{i}") for i in range(2)]
    ct=[k.sb([128,128],F32,f"ct{i}") for i in range(2)]; stt=[k.sb([128,128],F32,f"stt{i}") for i in range(2)]
    xl=[k.dsem(f"a_xl{i}") for i in range(2)]
    tp_ps=[k.ps([128,8,128],BF16,f"tp_ps{i}") for i in range(2)]
    pj_ps=[k.ps([128,512],F32,f"pj_ps{i}") for i in range(4)]
    qs=k.sb([128,1024],F32,"qs"); sq=k.sb([128,1024],F32,"sq"); t1=k.sb([128,1024],F32,"t1"); t2=k.sb([128,1024],F32,"t2")
    hs=k.sb([128,8],F32,"hs"); hr=k.sb([128,8],F32,"hr")
    qb=k.sb([128,1024],BF16,"qbf"); qT_sb=[k.sb([128,8,128],BF16,f"qT_sb{i}") for i in range(2)]
    v_sb=[k.sb([128,256],BF16,f"v_sbA{i}") for i in range(2)]
    pring=[k.sb([128,1024],BF16,f"pring{i}") for i in range(4)]
    mT=k.sb([128,8,128],BF16,"mT"); yT=[k.sb([128,8,128],BF16,f"yT{i}") for i in range(2)]
    st_q=[k.dsem(f"a_stq{i}") for i in range(2)]; st_v=[k.dsem(f"a_stv{i}") for i in range(2)]; st_y=[k.dsem(f"a_sty{i}") for i in range(2)]
    st_ev_q=[None,None]; st_ev_v=[None,None]; st_ev_y=[None,None]
    out_evs=[]
    state={'n':0,'free_x':[None,None],'free_xT':[None,None],'tp_free':[None,None],'pj_free':[None]*4,'pjn':0, 'work':None,'qT_n':0,'v_n':0}

    def norm_rope(src_ps_list, H, rst, gr, ctile, stile, dest_bf, extra_wait=()):
        W=H*128
        evs=[]
        for bi,(pst,ev) in enumerate(src_ps_list):
            act.wait(ev, state['work'])
            w=min(512,W-bi*512)
            evs.append(act.sig(nc.scalar.activation(out=qs[:,bi*512:bi*512+w],in_=pst[:,0:w],func=AF.Copy,scale=rst[:,0:1])))
        dve.wait(*evs); dve.wait(*extra_wait)
        e=dve.sig(nc.vector.tensor_tensor(out=sq[:,0:W],in0=qs[:,0:W],in1=qs[:,0:W],op=ALU.mult)); dve.wait(e)
        e=dve.sig(nc.vector.tensor_reduce(out=hs[:,0:H],in_=sq[:,0:W].rearrange("p (h d) -> p h d",h=H),axis=AX.X,op=ALU.add)); dve.wait(e)
        act.wait(e)
        e=act.sig(nc.scalar.activation(out=hr[:,0:H],in_=hs[:,0:H],func=AF.Sqrt,scale=1.0/128,bias=epsb[:,0:1])); dve.wait(e)
        e=dve.sig(nc.vector.reciprocal(out=hr[:,0:H],in_=hr[:,0:H])); dve.wait(e)
        q3=qs[:,0:W].rearrange("p (h d) -> p h d",h=H)
        e=dve.sig(nc.vector.tensor_tensor(out=sq[:,0:W].rearrange("p (h d) -> p h d",h=H),in0=q3,in1=hr[:,0:H].unsqueeze(2).to_broadcast([128,H,128]),op=ALU.mult)); dve.wait(e)
        s3=sq[:,0:W].rearrange("p (h d) -> p h d",h=H)
        e=dve.sig(nc.vector.tensor_tensor(out=q3,in0=s3,in1=gr[:].unsqueeze(1).to_broadcast([128,H,128]),op=ALU.mult)); dve.wait(e)
        e1=dve.sig(nc.vector.tensor_tensor(out=t1[:,0:W].rearrange("p (h d) -> p h d",h=H),in0=q3,in1=ctile[:].unsqueeze(1).to_broadcast([128,H,128]),op=ALU.mult))
        q5=qs[:,0:W].rearrange("p (h b f e) -> p h b f e",h=H,b=2,f=2)
        t5=t2[:,0:W].rearrange("p (h b f e) -> p h b f e",h=H,b=2,f=2)
        s5=stile[:].rearrange("p (b f e) -> p b f e",b=2,f=2)
        for f in range(2):
            for b in range(2):
                e2=dve.sig(nc.vector.tensor_tensor(out=t5[:,:,b,f,:],in0=q5[:,:,b,1-f,:],in1=s5[:,b,f,:].unsqueeze(1).to_broadcast([128,H,32]),op=ALU.mult))
        dve.wait(e1,e2)
        e=dve.sig(nc.vector.tensor_tensor(out=dest_bf[:,0:W],in0=t1[:,0:W],in1=t2[:,0:W],op=ALU.add))
        state['work']=e
        return e

    def tile(src_ap, rows, ctab, stab, tok0, do_kv=None, do_q=None, do_pool=None):
        n=state['n']; s=n%2; state['n']+=1
        dd=[state['free_x'][s]]
        if rows<128:
            dve.wait(state['free_x'][s]); z=dve.sig(nc.vector.memset(xt[s][:],0.0)); dd=[z]
        evx=k.dma(sp,xl[s],xt[s][0:rows,:],src_ap,deps=dd)
        if ctab is not None:
            k.dma(sp,xl[s],ct[s][0:rows,:],ctab[tok0:tok0+rows,:],deps=dd)
            evx=k.dma(sp,xl[s],stt[s][0:rows,:],stab[tok0:tok0+rows,:],deps=dd)
        act.wait(evx)
        e_ss=act.sig(nc.scalar.activation(out=junk[:],in_=xt[s][:],func=AF.Square,accum_out=ssq[s][:]))
        pool.wait(evx, state['free_xT'][s])
        e_xb=pool.sig(nc.gpsimd.tensor_copy(out=xb[s][:],in_=xt[s][:]))
        act.wait(e_ss)
        e=act.sig(nc.scalar.activation(out=rstd[s][:],in_=ssq[s][:],func=AF.Sqrt,scale=1.0/2048,bias=epsb[:,0:1])); dve.wait(e)
        e_rs=dve.sig(nc.vector.reciprocal(out=rstd[s][:],in_=rstd[s][:]))
        pe.wait(e_xb)
        tev=[]
        for half in range(2):
            pe.wait(state['tp_free'][half])
            for c8 in range(8):
                c=half*8+c8
                ins=nc.tensor.transpose(tp_ps[half][:,c8,:],xb[s][:,c*128:(c+1)*128],ident[:])
            tev.append(pe.sig(ins))
        state['free_x'][s]=None
        dve.wait(tev[0], state['free_xT'][s]);
        e0=dve.sig(nc.vector.tensor_copy(out=xT[s][:,0:8,:],in_=tp_ps[0][:]))
        act.wait(tev[1], state['free_xT'][s])
        e1=act.sig(nc.scalar.copy(out=xT[s][:,8:16,:],in_=tp_ps[1][:]))
        state['tp_free']=[e0,e1]
        xT_ready=[e0,e1]
        def proj(col0, ncols):
            res=[]
            for b0 in range(0,ncols,512):
                w=min(512,ncols-b0); j=state['pjn']%4; state['pjn']+=1
                pe.wait(xT_ready, w_done, state['pj_free'][j])
                for c in range(16):
                    ins=nc.tensor.matmul(pj_ps[j][:,0:w],lhsT=xT[s][:,c,:],rhs=wsb[:,c,col0+b0:col0+b0+w],start=(c==0),stop=(c==15))
                res.append((pj_ps[j],pe.sig(ins),j))
            return res
        last_pe=None
        if do_kv is not None:
            KT,V,tidx=do_kv
            r=proj(1024,512)[0]; pst,ev,j=r
            vs=state['v_n']%2; state['v_n']+=1
            act.wait(ev, e_rs, st_ev_v[vs])
            e_v=act.sig(nc.scalar.activation(out=v_sb[vs][:],in_=pst[:,256:512],func=AF.Copy,scale=rstd[s][:,0:1]))
            st_ev_v[vs]=k.dma(sp,st_v[vs],V[tidx,0:rows,:],v_sb[vs][0:rows,:],deps=[e_v]); out_evs.append(st_ev_v[vs])
            e_k=norm_rope([(pst,ev)],2,rstd[s],gkr,ct[s],stt[s],qb,extra_wait=[e_rs])
            state['pj_free'][j]=[e_k,e_v]
            qs_=state['qT_n']%2; state['qT_n']+=1
            pe.wait(e_k, state['tp_free'][0])
            for h in range(2):
                ins=nc.tensor.transpose(tp_ps[0][:,h,:],qb[:,h*128:(h+1)*128],ident[:])
            e_t=pe.sig(ins)
            dve.wait(e_t, st_ev_q[qs_])
            e_c=dve.sig(nc.vector.tensor_copy(out=qT_sb[qs_][:,0:2,:],in_=tp_ps[0][:,0:2,:]))
            state['tp_free'][0]=e_c
            st_ev_q[qs_]=k.dma(sp,st_q[qs_],KT[:,:,tok0:tok0+rows].rearrange("h d t -> d h t"),qT_sb[qs_][:,0:2,0:rows],deps=[e_c]); out_evs.append(st_ev_q[qs_])
            last_pe=e_t
        if do_q is not None:
            QT=do_q
            r=proj(0,1024)
            e_q=norm_rope([(r[0][0],r[0][1]),(r[1][0],r[1][1])],8,rstd[s],gqr,ct[s],stt[s],qb,extra_wait=[e_rs])
            state['pj_free'][r[0][2]]=e_q; state['pj_free'][r[1][2]]=e_q
            qs_=state['qT_n']%2; state['qT_n']+=1
            pe.wait(e_q, state['tp_free'][0])
            for h in range(8):
                ins=nc.tensor.transpose(tp_ps[0][:,h,:],qb[:,h*128:(h+1)*128],ident[:])
            e_t=pe.sig(ins)
            dve.wait(e_t, st_ev_q[qs_])
            e_c=dve.sig(nc.vector.tensor_copy(out=qT_sb[qs_][:],in_=tp_ps[0][:]))
            state['tp_free'][0]=e_c
            st_ev_q[qs_]=k.dma(sp,st_q[qs_],QT[:,:,tok0:tok0+128].rearrange("h d t -> d h t"),qT_sb[qs_][:],deps=[e_c]); out_evs.append(st_ev_q[qs_])
            last_pe=e_t
        if do_pool is not None:
            slot,prev_free=do_pool
            r=proj(1536,1024)
            evs=[]
            act.wait(e_rs, prev_free)
            for bi in range(2):
                act.wait(r[bi][1])
                e=act.sig(nc.scalar.activation(out=pring[slot][:,bi*512:(bi+1)*512],in_=r[bi][0][:],func=AF.Copy,scale=rstd[s][:,0:1]))
                state['pj_free'][r[bi][2]]=e; evs.append(e)
            state['p_ready']=evs[-1]
            last_pe=r[1][1]
        state['free_x'][s]=last_pe if last_pe is not None else None
        state['free_x'][s]=[e_ss,e_xb]
        state['free_xT'][s]=last_pe
        return

    def pool_tile(i, srcs, mixT):
        ys=i%2
        pe.wait(state['p_ready'], state['tp_free'][1], ev_c)
        mps=pj_ps[0];
        j0=state['pjn']%4; j1=(state['pjn']+1)%4; state['pjn']+=2
        pe.wait(state['pj_free'][j0], state['pj_free'][j1])
        for ch in range(8):
            g=ch//2; bank=pj_ps[j0] if ch<4 else pj_ps[j1]
            for si,(slot,ab) in enumerate(srcs):
                ins=nc.tensor.matmul(bank[:,(ch%4)*128:(ch%4+1)*128],lhsT=pring[slot][:,ch*128:(ch+1)*128],rhs=Aband[:,ab*4+g,:],start=(si==0),stop=(si==len(srcs)-1))
        e_m=pe.sig(ins)
        dve.wait(e_m, state.get('mT_free'))
        e0=dve.sig(nc.vector.tensor_copy(out=mT[:,0:4,:],in_=pj_ps[j0][:].rearrange("p (c t) -> p c t",c=4)))
        e1=dve.sig(nc.vector.tensor_copy(out=mT[:,4:8,:],in_=pj_ps[j1][:].rearrange("p (c t) -> p c t",c=4)))
        pe.wait(e0,e1,ev_pw)
        for ch in range(8):
            g=ch//2; dd=ch%2; bank=pj_ps[j0] if ch<4 else pj_ps[j1]
            for cc in range(2):
                ins=nc.tensor.matmul(bank[:,(ch%4)*128:(ch%4+1)*128],lhsT=pw[:,g*2+cc,dd*128:(dd+1)*128],rhs=mT[:,g*2+cc,:],start=(cc==0),stop=(cc==1))
        e_y=pe.sig(ins)
        state['mT_free']=e_y
        dve.wait(e_y, st_ev_y[ys])
        for hb,bank in enumerate((pj_ps[j0],pj_ps[j1])):
            e=dve.sig(nc.vector.tensor_tensor(out=yT[ys][:,hb*4:(hb+1)*4,:],in0=bank[:].rearrange("p (c t) -> p c t",c=4),in1=psc[:,hb*4:(hb+1)*4].unsqueeze(2).to_broadcast([128,4,128]),op=ALU.mult))
        state['pj_free'][j0]=e; state['pj_free'][j1]=e
        st_ev_y[ys]=k.dma(sp,st_y[ys],mixT[8:16,:,i*128:(i+1)*128].rearrange("c d t -> d c t"),yT[ys][:],deps=[e]); out_evs.append(st_ev_y[ys])
        return e_m

    for t in range(NKVT):
        tile(d['xkv'][t*128:(t+1)*128,:],128,d['ckv'],d['skv'],t*128,do_kv=(d['KT'],d['V'],t))
    tile(d['meta'][0:16,:],16,d['ckv'],d['skv'],NKVT*128,do_kv=(d['KT'],d['V'],NKVT))
    tile(d['xh'][:,:],128,None,None,0,do_pool=(3,None))
    em=[None]*(NQT+1)
    def srcs_for(i):
        sr=[]
        sr.append(((i-1)%3,1) if i>0 else (3,3))
        sr.append((i%3,0 if i<NQT-1 else 5))
        sr.append(((i+1)%3,2) if i<NQT-1 else (3,4))
        return sr
    for i in range(NQT):
        tile(d['xq'][i*128:(i+1)*128,:],128,d['cq'],d['sq'],i*128,do_q=d['QT'],do_pool=(i%3,em[i-2] if i>=2 else None))
        if i>=1: em[i-1]=pool_tile(i-1,srcs_for(i-1),d['mixT'])
    em[NQT-1]=pool_tile(NQT-1,srcs_for(NQT-1),d['mixT'])
    return out_evs


def attention_phase(k, QT, KT, V, attnT, nbias, ones_bf, NQT, NKF, PART, scale, deps=()):
    nc=k.nc
    NQ=NQT*128; NK=NKF*128+PART; NKT=NKF+(1 if PART else 0)
    kt_sb=k.sb([128,NK],BF16,"kt_sb"); v_sb=k.sb([128,NKT,128],BF16,"v_sb"); q_sb=k.sb([128,4,NQ],BF16,"q_sb")
    pT=[k.sb([128,512],BF16,f"pT{i}") for i in range(2)]
    rec=k.sb([128,512],F32,"rec"); ob=[k.sb([128,512],BF16,f"ob{i}") for i in range(2)]
    s_ps=[k.ps([128,512],F32,f"s_ps{i}") for i in range(3)]
    o_ps=[k.ps([128,512],F32,f"o_ps{i}") for i in range(2)]
    l_ps=[k.ps([128,512],F32,f"l_ps{i}") for i in range(2)]
    ld=k.dsem("attn_ld"); st=[k.dsem(f"attn_st{i}") for i in range(2)]
    out_evs=[]; pe=k.pe; act=k.act; dve=k.dve; sp=k.sp
    last_pe_of_head=None; norm_ev=[None,None]; st_ev=[None,None]
    gq=0
    for h in range(2):
        dd=list(deps)+([last_pe_of_head] if last_pe_of_head else [])
        NCH=8
        cw=(NK+NCH-1)//NCH
        for c in range(NCH):
            lo=c*cw; hi=min(NK,lo+cw)
            ev_k=k.dma(sp,ld,kt_sb[:,lo:hi],KT[h,:,lo:hi],deps=dd)
        ev_v=k.dma(sp,ld,v_sb[:,0:NKF,:],V[0:NKF,:,h*128:(h+1)*128].rearrange("t p d -> p t d"),deps=dd)
        if PART:
            ev_v=k.dma(sp,ld,v_sb[0:PART,NKF,:],V[NKF,0:PART,h*128:(h+1)*128],deps=dd)
        ev_q=k.dma(sp,ld,q_sb[:],QT[4*h:4*h+4].rearrange("h d q -> d h q"),deps=dd)
        ld_ev=(ld,ld.cnt)
        steps=[(qi,kt) for qi in range(NQT) for kt in range(NKT)]
        n=len(steps)
        s_ev=[None]*n; e_ev=[None]*n
        def issue_S(i):
            qi,kt=steps[i]; rows=128 if kt<NKF else PART
            pe.wait(ld_ev)
            ins=nc.tensor.matmul(s_ps[i%3][0:rows,:], lhsT=kt_sb[:,kt*128:kt*128+rows], rhs=q_sb[:,:,qi*128:(qi+1)*128], start=True, stop=True)
            s_ev[i]=pe.sig(ins)
        issue_S(0)
        if n>1: issue_S(1)
        for i,(qi,kt) in enumerate(steps):
            rows=128 if kt<NKF else PART
            par=(gq+qi)%2
            act.wait(s_ev[i])
            ins=nc.scalar.activation(out=pT[i%2][0:rows,:], in_=s_ps[i%3][0:rows,:], func=AF.Exp, bias=nbias[0:rows,0:1], scale=scale)
            e_ev[i]=act.sig(ins)
            pe.wait(e_ev[i])
            if kt==0: pe.wait(norm_ev[par])
            nc.tensor.matmul(o_ps[par][:], lhsT=v_sb[0:rows,kt,:], rhs=pT[i%2][0:rows,:], start=(kt==0), stop=(kt==NKT-1))
            ins=nc.tensor.matmul(l_ps[par][:], lhsT=ones_bf[0:rows,:], rhs=pT[i%2][0:rows,:], start=(kt==0), stop=(kt==NKT-1))
            pv_ev=pe.sig(ins)
            if i+2<n: issue_S(i+2)
            if kt==NKT-1:
                dve.wait(pv_ev, st_ev[par], norm_ev[1-par])
                r1=dve.sig(nc.vector.reciprocal(out=rec[:], in_=l_ps[par][:]))
                dve.wait(r1)
                ins=nc.vector.tensor_tensor(out=ob[par][:], in0=o_ps[par][:], in1=rec[:], op=ALU.mult)
                norm_ev[par]=dve.sig(ins)
                st_ev[par]=k.dma(sp,st[par],attnT[4*h:4*h+4,:,qi*128:(qi+1)*128].rearrange("h d q -> d h q"),
                                 ob[par][:].rearrange("d (h q) -> d h q",h=4),deps=[norm_ev[par]])
                out_evs.append(st_ev[par])
                last_pe_of_head=pv_ev
        gq+=NQT
    return out_evs


def phase_c(k, C, d, NQT, NBLK):
    nc=k.nc; pe=k.pe; act=k.act; dve=k.dve; pool=k.pool; sp=k.sp
    EPS=1e-6; T=NQT; S2=2*T
    ident_f=C['ident_f']
    wo=k.sb([128,16,2048],BF16,"wo"); wst=[k.sb([128,2048],F32,f"wost{i}") for i in range(2)]
    g2r=k.sb([128,2048],F32,"g2r"); wr=k.sb([128,16,36],F32,"wr"); brr=k.sb([128,36],F32,"brr")
    epsb=k.sb([128,1],F32,"epsb2")
    LGT=C['LGT']
    cs=k.dsem("c_const"); wl=[k.dsem(f"c_wl{i}") for i in range(2)]
    k.dma(sp,cs,g2r[:],d['g2r'][:,:]); k.dma(sp,cs,wr[:],d['w_r'][:,:,:]); ev_c=k.dma(sp,cs,brr[:],d['b_r'][:,:])
    e_eps=dve.sig(nc.vector.memset(epsb[:],EPS))
    wev=[None,None]; cev=[None,None]
    for c in range(16):
        s=c%2
        wev[s]=k.dma(sp,wl[s],wst[s][:],d['w_out'][c*128:(c+1)*128,:],deps=[cev[s]])
        q=dve if s==0 else pool
        q.wait(wev[s])
        cev[s]=q.sig((nc.vector if s==0 else nc.gpsimd).tensor_copy(out=wo[:,c,:],in_=wst[s][:]))
    w_done=list(cev)
    mx=[k.sb([128,16,128],BF16,f"mx{i}") for i in range(2)]
    xt=[k.sb([128,2048],F32,f"cxt{i}") for i in range(2)]
    h1=[k.sb([128,2048],F32,f"h1_{i}") for i in range(2)]
    bf=k.sb([128,2048],F32,"bf"); b16=[k.sb([128,2048],BF16,f"b16_{i}") for i in range(2)]
    bT=k.sb([128,16,128],F32,"bT32"); junk=k.sb([128,2048],BF16,"cjunk")
    ssq=k.sb([128,1],F32,"cssq"); rstd=k.sb([128,1],F32,"crstd")
    po=[k.ps([128,512],F32,f"po{i}") for i in range(4)]
    tp=[k.ps([128,4,128],F32,f"ctp{i}") for i in range(2)]
    lp=k.ps([128,36],F32,"lp")
    ld=[k.dsem(f"c_ld{i}") for i in range(2)]; sth=[k.dsem(f"c_sth{i}") for i in range(2)]; stb=[k.dsem(f"c_stb{i}") for i in range(2)]
    mx_free=[None,None]; xt_free=[None,None]; h1_free=[None,None]; b16_free=[None,None]; po_free=[None]*4; tp_free=[None,None]
    bf_free=None; bT_free=None; lp_free=None
    out_evs=[]
    for i in range(T):
        s=i%2
        ev_m=k.dma(sp,ld[s],mx[s][:],d['mixT'][:,:,i*128:(i+1)*128].rearrange("c d t -> d c t"),deps=[mx_free[s]])
        ev_x=k.dma(sp,ld[s],xt[s][:],d['xq'][i*128:(i+1)*128,:],deps=[xt_free[s]])
        pe.wait(ev_x, w_done)
        pev=[]
        for n in range(4):
            pe.wait(po_free[n])
            for c in range(16):
                ins=nc.tensor.matmul(po[n][:],lhsT=mx[s][:,c,:],rhs=wo[:,c,n*512:(n+1)*512],start=(c==0),stop=(c==15))
            pev.append(pe.sig(ins))
        mx_free[s]=pev[3]
        dve.wait(h1_free[s])
        for n in range(4):
            dve.wait(pev[n])
            e=dve.sig(nc.vector.tensor_tensor(out=h1[s][:,n*512:(n+1)*512],in0=po[n][:],in1=xt[s][:,n*512:(n+1)*512],op=ALU.add))
            po_free[n]=e
        e_h1=e; xt_free[s]=e
        ev_sh=k.dma(sp,sth[s],d['h1'][i*128:(i+1)*128,:],h1[s][:],deps=[e_h1]); out_evs.append(ev_sh)
        act.wait(e_h1, e_eps)
        e=act.sig(nc.scalar.activation(out=junk[:],in_=h1[s][:],func=AF.Square,accum_out=ssq[:])); act.wait(e)
        e=act.sig(nc.scalar.activation(out=rstd[:],in_=ssq[:],func=AF.Sqrt,scale=1.0/2048,bias=epsb[:,0:1])); dve.wait(e)
        e=dve.sig(nc.vector.reciprocal(out=rstd[:],in_=rstd[:])); dve.wait(e, bf_free, ev_c)
        e_bf=dve.sig(nc.vector.scalar_tensor_tensor(out=bf[:],in0=h1[s][:],scalar=rstd[:,0:1],in1=g2r[:],op0=ALU.mult,op1=ALU.mult))
        h1_free[s]=[e_bf,ev_sh]
        pool.wait(e_bf, b16_free[s])
        e_b16=pool.sig(nc.gpsimd.tensor_copy(out=b16[s][:],in_=bf[:]))
        b16_free[s]=k.dma(sp,stb[s],d['b16'][i*128:(i+1)*128,:],b16[s][:],deps=[e_b16]); out_evs.append(b16_free[s])
        pe.wait(e_bf)
        tev=[]
        for gch in range(4):
            j=gch%2
            pe.wait(tp_free[j])
            for c4 in range(4):
                c=gch*4+c4
                ins=nc.tensor.transpose(tp[j][:,c4,:],bf[:,c*128:(c+1)*128],ident_f[:])
            e_t=pe.sig(ins)
            q=dve if j==0 else act
            q.wait(e_t, bT_free)
            if j==0: e=dve.sig(nc.vector.tensor_copy(out=bT[:,gch*4:gch*4+4,:],in_=tp[j][:]))
            else: e=act.sig(nc.scalar.copy(out=bT[:,gch*4:gch*4+4,:],in_=tp[j][:]))
            tp_free[j]=e; tev.append(e)
        bf_free=[e_t,e_b16]
        pe.wait(*tev); pe.wait(lp_free, ev_c)
        for c in range(16):
            ins=nc.tensor.matmul(lp[:],lhsT=bT[:,c,:],rhs=wr[:,c,:],start=(c==0),stop=(c==15))
        e_l=pe.sig(ins); bT_free=e_l
        dve.wait(e_l)
        lp_free=dve.sig(nc.vector.tensor_tensor(out=LGT[:,i,:],in0=lp[:],in1=brr[:],op=ALU.add))
    return out_evs

def phase_c2(k, C, d, NQT, NBLK):
    nc=k.nc; pe=k.pe; act=k.act; dve=k.dve; pool=k.pool; sp=k.sp
    T=NQT; S2=2*T; LGT=C['LGT']
    po=[k.ps([128,512],F32,f"c2po{i}") for i in range(2)]; po_free=[None,None]
    junk=k.sb([128,2048],BF16,"c2junk"); b16=[k.sb([128,2048],BF16,f"c2b16_{i}") for i in range(2)]
    ld=[k.dsem(f"c2_ld{i}") for i in range(2)]
    lp_free=None; out_evs=[]
    V=nc.vector
    def dv(ins, *w):
        return dve.sig(ins)
    cnt=[0]
    def T_(shape,dt=F32):
        cnt[0]+=1; return k.sb(shape,dt,f"rt{cnt[0]}")
    LG=LGT[:,:,0:4]; LE=LGT[:,:,4:36]
    gmax=T_([128,T]); dd=T_([128,T,4]); ohg=T_([128,T,4]); eg=T_([128,T,4]); sg=T_([128,T]); gp=T_([128,T])
    tmp=T_([128,T,32]); sel=T_([128,T,8]); v1=T_([128,T]); m1=T_([128,T,8]); sel2=T_([128,T,8]); v2=T_([128,T]); m2=T_([128,T,8])
    rr=T_([128,T]); g1_=T_([128,T]); Mall=T_([128,S2,32]); Mbf=T_([128,S2,32],BF16)
    Utri=T_([128,128],BF16); onesb=T_([128,128],BF16)
    rs=k.dsem("c_rs")
    k.dma(sp,rs,Utri[:],d['Utri'][:,:]); ev_u=k.dma(sp,rs,onesb[:],d['ones_bf'][:,:])
    thr=T_([128,32]); blkst=T_([128,NBLK])
    k.dma(sp,rs,thr[:],d['thr'][:,:]); ev_u=k.dma(sp,rs,blkst[:],d['blkst'][:,:])
    dve.wait(lp_free)
    def op(ins):
        e=dve.sig(ins); dve.wait(e); return e
    op(V.tensor_reduce(out=gmax[:],in_=LG,axis=AX.X,op=ALU.max))
    op(V.tensor_tensor(out=dd[:],in0=LG,in1=gmax[:].unsqueeze(2).to_broadcast([128,T,4]),op=ALU.subtract))
    op(V.tensor_single_scalar(out=ohg[:],in_=dd[:],scalar=0.0,op=ALU.is_ge))
    e=op(V.tensor_copy(out=eg[:],in_=dd[:]))
    act.wait(e); e=act.sig(nc.scalar.activation(out=eg[:],in_=dd[:],func=AF.Exp)); dve.wait(e)
    op(V.tensor_reduce(out=sg[:],in_=eg[:],axis=AX.X,op=ALU.add))
    op(V.reciprocal(out=gp[:],in_=sg[:]))
    op(V.tensor_tensor(out=tmp[:].rearrange("p t (g e) -> p t g e",g=4),in0=LE.rearrange("p t (g e) -> p t g e",g=4),in1=ohg[:].unsqueeze(3).to_broadcast([128,T,4,8]),op=ALU.mult))
    op(V.tensor_reduce(out=sel[:],in_=tmp[:].rearrange("p t (g e) -> p t e g",g=4),axis=AX.X,op=ALU.add))
    op(V.tensor_reduce(out=v1[:],in_=sel[:],axis=AX.X,op=ALU.max))
    op(V.tensor_tensor(out=m1[:],in0=sel[:],in1=v1[:].unsqueeze(2).to_broadcast([128,T,8]),op=ALU.is_ge))
    op(V.scalar_tensor_tensor(out=sel2[:],in0=m1[:],scalar=-1e30,in1=sel[:],op0=ALU.mult,op1=ALU.add))
    op(V.tensor_reduce(out=v2[:],in_=sel2[:],axis=AX.X,op=ALU.max))
    op(V.tensor_tensor(out=m2[:],in0=sel2[:],in1=v2[:].unsqueeze(2).to_broadcast([128,T,8]),op=ALU.is_ge))
    e=op(V.tensor_tensor(out=rr[:],in0=v2[:],in1=v1[:],op=ALU.subtract))
    act.wait(e); e=act.sig(nc.scalar.activation(out=rr[:],in_=rr[:],func=AF.Exp)); dve.wait(e)
    op(V.tensor_scalar(out=rr[:],in0=rr[:],scalar1=1.0,scalar2=None,op0=ALU.add))
    op(V.reciprocal(out=rr[:],in_=rr[:]))
    gates=C['gates']
    op(V.tensor_tensor(out=gates[:,:,0],in0=gp[:],in1=rr[:],op=ALU.mult))
    op(V.tensor_tensor(out=gates[:,:,1],in0=gp[:],in1=gates[:,:,0],op=ALU.subtract))
    M4=Mall[:].rearrange("p (t k) (g e) -> p t k g e",k=2,g=4)
    for kk,mk in enumerate((m1,m2)):
        op(V.tensor_tensor(out=M4[:,:,kk,:,:],in0=ohg[:].unsqueeze(3).to_broadcast([128,T,4,8]),in1=mk[:].unsqueeze(2).to_broadcast([128,T,4,8]),op=ALU.mult))
    e_mb=op(V.tensor_copy(out=Mbf[:],in_=Mall[:]))
    R=T_([128,S2,32]); Tot=T_([128,S2,32])
    nch=(S2*32+511)//512
    pe.wait(e_mb, ev_u)
    Mflat=Mbf[:].rearrange("p s e -> p (s e)"); Rflat=R[:].rearrange("p s e -> p (s e)"); Tflat=Tot[:].rearrange("p s e -> p (s e)")
    for c in range(nch):
        w=min(512,S2*32-c*512)
        pe.wait(po_free[0],po_free[1])
        ins=nc.tensor.matmul(po[0][:,0:w],lhsT=Utri[:],rhs=Mflat[:,c*512:c*512+w],start=True,stop=True)
        ins=nc.tensor.matmul(po[1][:,0:w],lhsT=onesb[:],rhs=Mflat[:,c*512:c*512+w],start=True,stop=True)
        e=pe.sig(ins); dve.wait(e)
        op(V.tensor_copy(out=Rflat[:,c*512:c*512+w],in_=po[0][:,0:w]))
        e=op(V.tensor_copy(out=Tflat[:,c*512:c*512+w],in_=po[1][:,0:w]))
        po_free[0]=e; po_free[1]=e
    base=T_([128,S2+1,32])
    op(V.memset(base[:,0,:],0.0))
    for s_ in range(S2):
        op(V.tensor_tensor(out=base[:,s_+1,:],in0=base[:,s_,:],in1=Tot[:,s_,:],op=ALU.add))
    counts=base[:,S2,:]
    cmp=T_([128,32,32]); nb=T_([128,32]); padded=T_([128,32]); pst=T_([128,33])
    op(V.tensor_tensor(out=cmp[:],in0=counts.unsqueeze(2).to_broadcast([128,32,32]),in1=thr[:].unsqueeze(1).to_broadcast([128,32,32]),op=ALU.is_gt))
    op(V.tensor_reduce(out=nb[:],in_=cmp[:],axis=AX.X,op=ALU.add))
    op(V.tensor_scalar(out=padded[:],in0=nb[:],scalar1=128.0,scalar2=None,op0=ALU.mult))
    op(V.memset(pst[:,0:1],0.0))
    for e_ in range(32):
        op(V.tensor_tensor(out=pst[:,e_+1:e_+2],in0=pst[:,e_:e_+1],in1=padded[:,e_:e_+1],op=ALU.add))
    RB=T_([128,S2,32]); posf=T_([128,S2])
    op(V.tensor_tensor(out=RB[:],in0=R[:],in1=base[:,0:S2,:],op=ALU.add))
    op(V.tensor_tensor(out=RB[:],in0=RB[:],in1=pst[:,0:32].unsqueeze(1).to_broadcast([128,S2,32]),op=ALU.add))
    op(V.tensor_tensor(out=RB[:],in0=RB[:],in1=Mall[:],op=ALU.mult))
    op(V.tensor_reduce(out=posf[:],in_=RB[:],axis=AX.X,op=ALU.add))
    e_pos=op(V.tensor_copy(out=C['pos_i'][:],in_=posf[:]))
    cmp2=T_([128,NBLK,32]); bef=T_([128,NBLK])
    op(V.tensor_tensor(out=cmp2[:],in0=pst[:,1:33].unsqueeze(1).to_broadcast([128,NBLK,32]),in1=blkst[:].unsqueeze(2).to_broadcast([128,NBLK,32]),op=ALU.is_le))
    op(V.tensor_reduce(out=bef[:],in_=cmp2[:],axis=AX.X,op=ALU.add))
    op(V.tensor_scalar(out=bef[:],in0=bef[:],scalar1=31.0,scalar2=None,op0=ALU.min))
    e_blk=op(V.tensor_copy(out=C['blk_e'][:],in_=bef[:]))
    sc=k.dsem("c_scat"); zs=k.dsem("c_zero")
    e_z=dve.sig(nc.vector.memset(junk[:],0.0))
    zev=None
    for r0 in range(0,NBLK,8):
        nb_=min(8,NBLK-r0)
        zev=k.dma(sp,zs,d['xs'][r0*128:(r0+nb_)*128,:].rearrange("(r p) n -> p r n",p=128),junk[:].unsqueeze(1).to_broadcast([128,nb_,2048]),deps=[e_z])
    pool.wait(zev)
    sp.wait(*out_evs)
    pool.wait(e_pos)
    scat_free=[None,None]
    for i in range(T):
        s=i%2
        ev=k.dma(sp,ld[s],b16[s][:],d['b16'][i*128:(i+1)*128,:],deps=[scat_free[s]]+out_evs)
        pool.wait(ev)
        for kk in range(2):
            ins=nc.gpsimd.indirect_dma_start(out=d['xs'][:,:],out_offset=bass.IndirectOffsetOnAxis(ap=C['pos_i'][:,i*2+kk:i*2+kk+1],axis=0),in_=b16[s][:],in_offset=None)
            ins.then_inc(sc.h,16); sc.cnt+=16
        scat_free[s]=(sc,sc.cnt)
        pool.wait(scat_free[s])
    return [(sc,sc.cnt), e_blk, e_pos]


def weight_convert(k, d, deps=()):
    nc=k.nc; sp=k.sp
    st=[k.sb([128,8192],F32,f"wc_st{i}") for i in range(2)]; bf=[k.sb([128,8192],BF16,f"wc_bf{i}") for i in range(2)]
    ld=[k.dsem(f"wc_ld{i}") for i in range(2)]; so=[k.dsem(f"wc_so{i}") for i in range(2)]
    st_free=[None,None]; bf_free=[None,None]; n=0; evs=[]
    for e in range(32):
        for (src,dst) in ((d['w_gate'],d['wg_bf']),(d['w_up'],d['wu_bf']),(d['w_down'],d['wd_bf'])):
            s=n%2; n+=1
            ev=k.dma(sp,ld[s],st[s][:].rearrange("p (c f) -> p c f",c=src.shape[1]//128),src[e].rearrange("(c p) f -> p c f",p=128),deps=[st_free[s]]+list(deps))
            cev=[]
            for qi,(q,eng) in enumerate(((k.dve,nc.vector),(k.pool,nc.gpsimd),(k.act,nc.scalar))):
                lo=(0,3072,5632)[qi]; hi=(3072,5632,8192)[qi]
                q.wait(ev, bf_free[s])
                if eng is nc.scalar: ins=eng.copy(out=bf[s][:,lo:hi],in_=st[s][:,lo:hi])
                else: ins=eng.tensor_copy(out=bf[s][:,lo:hi],in_=st[s][:,lo:hi])
                cev.append(q.sig(ins))
            st_free[s]=cev
            bf_free[s]=k.dma(sp,so[s],dst[e*512:(e+1)*512,:].rearrange("(g p) n -> p g n",p=128),bf[s][:].rearrange("p (g n) -> p g n",g=4),deps=cev); evs.append(bf_free[s])
    return evs

DEBUG_STATIC_W=False
def phase_d(k, C, d, NBLK, deps=()):
    nc=k.nc; pe=k.pe; act=k.act; dve=k.dve; pool=k.pool; sp=k.sp
    ident=C['ident_bf']
    wg=[k.sb([128,16*512],BF16,f"wg{i}") for i in range(2)]; wu=[k.sb([128,16*512],BF16,f"wu{i}") for i in range(2)]
    wd=[k.sb([128,4*2048],BF16,f"wd{i}") for i in range(2)]
    xs=[k.sb([128,2048],BF16,f"xs{i}") for i in range(2)]; xsT=k.sb([128,16,128],BF16,"xsT")
    sg=k.sb([128,512],F32,"sg"); hb=k.sb([128,512],BF16,"hb"); hT=k.sb([128,4,128],BF16,"hT")
    ysb=[k.sb([128,2048],F32,f"ysb{i}") for i in range(2)]
    idxf=k.sb([128,4,NBLK],F32,"idxf"); idxi=k.sb([128,4,NBLK],I32,"idxi"); befl=k.sb([128,NBLK],F32,"befl")
    tp=[k.ps([128,8,128],BF16,f"dtp{i}") for i in range(2)]
    pg=k.ps([128,512],F32,"pg"); pu=k.ps([128,512],F32,"pu"); pd=[k.ps([128,512],F32,f"pd{i}") for i in range(4)]
    wl=[k.dsem(f"d_wl{i}") for i in range(2)]; xl=[k.dsem(f"d_xl{i}") for i in range(2)]; ys=[k.dsem(f"d_ys{i}") for i in range(2)]
    dve.wait(*deps)
    e=dve.sig(nc.vector.tensor_copy(out=befl[:],in_=C['blk_e'][:])); dve.wait(e)
    e=dve.sig(nc.vector.tensor_scalar(out=befl[:],in0=befl[:],scalar1=512.0,scalar2=None,op0=ALU.mult)); dve.wait(e)
    e=dve.sig(nc.vector.tensor_scalar(out=befl[:],in0=befl[:],scalar1=C['iota_p'][:,0:1],scalar2=None,op0=ALU.add)); dve.wait(e)
    for g in range(4):
        e=dve.sig(nc.vector.tensor_scalar(out=idxf[:,g,:],in0=befl[:],scalar1=float(g*128),scalar2=None,op0=ALU.add)); dve.wait(e)
    e_idx=dve.sig(nc.vector.tensor_copy(out=idxi[:],in_=idxf[:]))
    wgv=d['wg_bf'][:,:]; wuv=d['wu_bf'][:,:]; wdv=d['wd_bf'][:,:]
    w_free=[None,None]; xs_free=[None,None]; y_free=[None,None]
    wev=[None]*NBLK; xev=[None]*NBLK
    def issue_loads(b):
        s=b%2
        pool.wait(e_idx, w_free[s], *deps)
        if DEBUG_STATIC_W:
            for (dst,src) in ((wg[s],d['wg_bf']),(wu[s],d['wu_bf']),(wd[s],d['wd_bf'])):
                wev[b]=k.dma(sp,wl[s],dst[:].rearrange("p (g n) -> p g n",g=4),src[(b%32)*512:(b%32+1)*512,:].rearrange("(g p) n -> p g n",p=128),deps=[w_free[s]]+list(deps))
            xev[b]=k.dma(sp,xl[s],xs[s][:],d['xs'][b*128:(b+1)*128,:],deps=[xs_free[s]]+list(deps))
            return
        for (dst,src) in ((wg[s],wgv),(wu[s],wuv),(wd[s],wdv)):
            for g in range(4):
                ins=nc.gpsimd.indirect_dma_start(out=dst[:,g*2048:(g+1)*2048],out_offset=None,in_=src,in_offset=bass.IndirectOffsetOnAxis(ap=idxi[:,g,b:b+1],axis=0))
                ins.then_inc(wl[s].h,16); wl[s].cnt+=16
                if g%2==1: pool.wait((wl[s],wl[s].cnt))
        wev[b]=(wl[s],wl[s].cnt)
        xev[b]=k.dma(sp,xl[s],xs[s][:],d['xs'][b*128:(b+1)*128,:],deps=[xs_free[s]]+list(deps))
    issue_loads(0)
    out_evs=[]
    xsT_free=None; hT_free=None; sg_free=None; hb_free=None; pd_free=None; tp_free=[None,None]; pgu_free=None
    for b in range(NBLK):
        s=b%2
        if b+1<NBLK: issue_loads(b+1)
        pe.wait(xev[b])
        tev=[]
        for half in range(2):
            pe.wait(tp_free[half])
            for c8 in range(8):
                c=half*8+c8
                ins=nc.tensor.transpose(tp[half][:,c8,:],xs[s][:,c*128:(c+1)*128],ident[:])
            tev.append(pe.sig(ins))
        xs_free[s]=tev[1]
        dve.wait(tev[0], xsT_free); e0=dve.sig(nc.vector.tensor_copy(out=xsT[:,0:8,:],in_=tp[0][:]))
        act.wait(tev[1], xsT_free); e1=act.sig(nc.scalar.copy(out=xsT[:,8:16,:],in_=tp[1][:]))
        tp_free=[e0,e1]
        pe.wait(e0,e1,wev[b],pgu_free)
        for c in range(16):
            nc.tensor.matmul(pg[:],lhsT=xsT[:,c,:],rhs=wg[s][:,c*512:(c+1)*512],start=(c==0),stop=(c==15))
        e_g=pe.sig(nc.tensor.matmul(pg[:],lhsT=xsT[:,0,:],rhs=wg[s][:,0:512],start=False,stop=True,skip_group_check=True)) if False else None
        for c in range(16):
            ins=nc.tensor.matmul(pu[:],lhsT=xsT[:,c,:],rhs=wu[s][:,c*512:(c+1)*512],start=(c==0),stop=(c==15))
        e_u=pe.sig(ins); xsT_free=e_u
        act.wait(e_u, sg_free)
        e_s=act.sig(nc.scalar.activation(out=sg[:],in_=pg[:],func=AF.Silu))
        dve.wait(e_s, hb_free)
        e_h=dve.sig(nc.vector.tensor_tensor(out=hb[:],in0=sg[:],in1=pu[:],op=ALU.mult))
        sg_free=e_h; pgu_free=e_h
        pe.wait(e_h, tp_free[0])
        for c in range(4):
            ins=nc.tensor.transpose(tp[0][:,c,:],hb[:,c*128:(c+1)*128],ident[:])
        e_t=pe.sig(ins); hb_free=e_t
        dve.wait(e_t, hT_free)
        e_c=dve.sig(nc.vector.tensor_copy(out=hT[:],in_=tp[0][:,0:4,:]))
        tp_free[0]=e_c
        pe.wait(e_c, pd_free)
        dev=[]
        for n in range(4):
            for c in range(4):
                ins=nc.tensor.matmul(pd[n][:],lhsT=hT[:,c,:],rhs=wd[s][:,c*2048+n*512:c*2048+(n+1)*512],start=(c==0),stop=(c==3))
            dev.append(pe.sig(ins))
        hT_free=dev[3]; w_free[s]=dev[3]
        evs_=[]
        for n in range(4):
            q=dve if n%2==0 else act
            q.wait(dev[n], y_free[s])
            if n%2==0: e=dve.sig(nc.vector.tensor_copy(out=ysb[s][:,n*512:(n+1)*512],in_=pd[n][:]))
            else: e=act.sig(nc.scalar.copy(out=ysb[s][:,n*512:(n+1)*512],in_=pd[n][:]))
            evs_.append(e)
        pd_free=evs_
        y_free[s]=k.dma(sp,ys[s],d['Y'][b*128:(b+1)*128,:],ysb[s][:],deps=evs_)
        out_evs.append(y_free[s])
    return out_evs

def phase_e(k, C, d, NQT, deps=()):
    nc=k.nc; dve=k.dve; pool=k.pool; sp=k.sp
    h1=[k.sb([128,2048],F32,f"eh1_{i}") for i in range(2)]; y0=[k.sb([128,2048],F32,f"ey0_{i}") for i in range(2)]; y1=[k.sb([128,2048],F32,f"ey1_{i}") for i in range(2)]
    ob=[k.sb([128,2048],F32,f"eo_{i}") for i in range(2)]
    ld=[k.dsem(f"e_ld{i}") for i in range(2)]; gl=[k.dsem(f"e_gl{i}") for i in range(2)]; st=[k.dsem(f"e_st{i}") for i in range(2)]
    in_free=[None,None]; o_free=[None,None]; out_evs=[]
    gates=C['gates']
    for i in range(NQT):
        s=i%2
        ev_h=k.dma(sp,ld[s],h1[s][:],d['h1'][i*128:(i+1)*128,:],deps=[in_free[s]]+list(deps))
        pool.wait(in_free[s], *deps)
        for kk,dst in enumerate((y0[s],y1[s])):
            ins=nc.gpsimd.indirect_dma_start(out=dst[:],out_offset=None,in_=d['Y'][:,:],in_offset=bass.IndirectOffsetOnAxis(ap=C['pos_i'][:,i*2+kk:i*2+kk+1],axis=0))
            ins.then_inc(gl[s].h,16); gl[s].cnt+=16
        ev_g=(gl[s],gl[s].cnt)
        pool.wait(ev_g)
        dve.wait(ev_h,ev_g,o_free[s])
        e=dve.sig(nc.vector.scalar_tensor_tensor(out=ob[s][:],in0=y0[s][:],scalar=gates[:,i,0:1],in1=h1[s][:],op0=ALU.mult,op1=ALU.add)); dve.wait(e)
        e=dve.sig(nc.vector.scalar_tensor_tensor(out=ob[s][:],in0=y1[s][:],scalar=gates[:,i,1:2],in1=ob[s][:],op0=ALU.mult,op1=ALU.add))
        in_free[s]=e
        o_free[s]=k.dma(sp,st[s],d['out'][i*128:(i+1)*128,:],ob[s][:],deps=[e]); out_evs.append(o_free[s])
    return out_evs


def build(NKVT, NQT, PART=16):
    NQ=NQT*128; NK=NKVT*128+PART; NBLK=2*NQT+32
    nc=bass.Bass("TRN2", target_bir_lowering=False)
    def din(name,shape,dt=F32): return nc.dram_tensor(name,list(shape),dt,kind="ExternalInput").ap()
    def scr(name,shape,dt): return nc.dram_tensor(name,list(shape),dt).ap()
    d={}
    d['xkv']=din('xkv',[NKVT*128,2048]); d['xq']=din('xq',[NQ,2048]); d['xh']=din('xh',[128,2048]); d['meta']=din('meta',[128,2048])
    d['g1t']=din('g1t',[128,16]); d['gqr']=din('gqr',[128,128]); d['gkr']=din('gkr',[128,128])
    d['pool_w']=din('pool_w',[128,8,256]); d['pool_sc']=din('pool_sc',[128,8]); d['Aband']=din('Aband',[128,24,128],BF16)
    d['ident_bf']=din('ident_bf',[128,128],BF16); d['ident_f']=din('ident_f',[128,128]); d['ones_bf']=din('ones_bf',[128,128],BF16)
    d['w_in']=din('w_in',[2048,2560])
    d['ckv']=din('ckv',[(NKVT+1)*128,128]); d['skv']=din('skv',[(NKVT+1)*128,128]); d['cq']=din('cq',[NQ,128]); d['sq']=din('sq',[NQ,128])
    d['w_out']=din('w_out',[2048,2048]); d['g2r']=din('g2r',[128,2048]); d['w_r']=din('w_r',[128,16,36]); d['b_r']=din('b_r',[128,36])
    d['Utri']=din('Utri',[128,128],BF16); d['thr']=din('thr',[128,32]); d['blkst']=din('blkst',[128,NBLK]); d['iota_p']=din('iota_p',[128,1])
    d['w_gate']=din('w_gate',[32,2048,512]); d['w_up']=din('w_up',[32,2048,512]); d['w_down']=din('w_down',[32,512,2048])
    d['out']=nc.dram_tensor('out',[NQ,2048],F32,kind="ExternalOutput").ap()
    d['KT']=scr('KT',[2,128,NK],BF16); d['V']=scr('V',[NKVT+1,128,256],BF16); d['QT']=scr('QT',[8,128,NQ],BF16); d['mixT']=scr('mixT',[16,128,NQ],BF16)
    d['h1']=scr('h1',[NQ,2048],F32); d['b16']=scr('b16',[NQ,2048],BF16); d['xs']=scr('xs',[NBLK*128,2048],BF16); d['Y']=scr('Y',[NBLK*128,2048],F32)
    d['wg_bf']=scr('wg_bf',[16384,2048],BF16); d['wu_bf']=scr('wu_bf',[16384,2048],BF16); d['wd_bf']=scr('wd_bf',[16384,2048],BF16)
    with ExitStack() as es:
        k=K(nc,es); C={}
        C['ident_bf']=k.sb([128,128],BF16,"c_ident_bf"); C['ident_f']=k.sb([128,128],F32,"c_ident_f"); C['ones_bf']=k.sb([128,128],BF16,"c_ones_bf")
        C['iota_p']=k.sb([128,1],F32,"c_iota_p"); C['nbias']=k.sb([128,1],F32,"c_nbias")
        C['LGT']=k.sb([128,NQT,36],F32,"c_LGT"); C['pos_i']=k.sb([128,2*NQT],I32,"c_pos_i"); C['gates']=k.sb([128,NQT,2],F32,"c_gates"); C['blk_e']=k.sb([128,NBLK],I32,"c_blk_e")
        gqa=k.sb([128,128],F32,"c_gqa"); gka=k.sb([128,128],F32,"c_gka"); mq=k.sb([128,1],F32,"c_mq"); mk=k.sb([128,1],F32,"c_mk")
        s0=k.dsem("c0")
        k.dma(k.sp,s0,C['ones_bf'][:],d['ones_bf'][:,:]); k.dma(k.sp,s0,C['iota_p'][:],d['iota_p'][:,:])
        k.dma(k.sp,s0,gqa[:],d['gqr'][:,:]); ev=k.dma(k.sp,s0,gka[:],d['gkr'][:,:])
        dve=k.dve; V=nc.vector
        dve.wait(ev)
        def op(ins):
            e=dve.sig(ins); dve.wait(e); return e
        m2=k.sb([128,2],F32,"c_m2")
        for (ga,mm,col) in ((gqa,mq,0),(gka,mk,1)):
            op(V.tensor_reduce(out=mm[:],in_=ga[:],axis=AX.X,op=ALU.max))
            op(V.tensor_scalar(out=ga[:],in0=ga[:],scalar1=-1.0,scalar2=None,op0=ALU.mult))
            op(V.tensor_reduce(out=m2[:,col:col+1],in_=ga[:],axis=AX.X,op=ALU.max))
            op(V.tensor_tensor(out=mm[:],in0=mm[:],in1=m2[:,col:col+1],op=ALU.max))
        op(V.tensor_tensor(out=mq[:],in0=mq[:],in1=mk[:],op=ALU.mult))
        e_nb=op(V.tensor_scalar(out=C['nbias'][:],in0=mq[:],scalar1=-(128.0**0.5),scalar2=None,op0=ALU.mult))
        k.begin_phase(); evs=weight_convert(k,d); k.end_phase(evs)
        k.begin_phase(); evs=phase_a(k,C,d,NKVT,NQT); k.end_phase(evs)
        k.begin_phase(); evs=attention_phase(k,d['QT'],d['KT'],d['V'],d['mixT'],C['nbias'],C['ones_bf'],NQT,NKVT,PART,128.0**-0.5,deps=[e_nb,(s0,s0.cnt)]); k.end_phase(evs)
        k.begin_phase(); evs=phase_c(k,C,d,NQT,NBLK); k.end_phase(evs)
        k.begin_phase(); evs=phase_c2(k,C,d,NQT,NBLK); k.end_phase(evs)
        k.begin_phase(); evs=phase_d(k,C,d,NBLK); k.end_phase(evs)
        k.begin_phase(); evs=phase_e(k,C,d,NQT); k.end_phase(evs)
    return nc

def const_inputs(j, NKVT, NQT, r0, NBLK):
    BF_=BF
    c={}
    r=np.arange(NKVT*128)
    Ck,Sk=rope_tables((r//GRID_W).astype(np.float32),(r%GRID_W).astype(np.float32))
    Cm,Sm=rope_tables(np.zeros(128,np.float32),np.zeros(128,np.float32))
    c['ckv']=np.concatenate([Ck,Cm]); c['skv']=np.concatenate([Sk,Sm])
    c['cq']=np.ascontiguousarray(Ck[r0:r0+NQT*128]); c['sq']=np.ascontiguousarray(Sk[r0:r0+NQT*128])
    c['Aband']=band_mats(j,NQT)
    c['ident_bf']=np.eye(128).astype(BF_); c['ident_f']=np.eye(128,dtype=np.float32); c['ones_bf']=np.ones((128,128),BF_)
    c['Utri']=np.triu(np.ones((128,128),np.float32),1).astype(BF_)
    c['thr']=np.tile((128*np.arange(32,dtype=np.float32))[None],(128,1)); c['blkst']=np.tile((128*np.arange(NBLK,dtype=np.float32))[None],(128,1))
    c['iota_p']=np.arange(128,dtype=np.float32).reshape(128,1)
    return c

def layout_params(p):
    f=lambda a: np.ascontiguousarray(np.asarray(a,dtype=np.float32))
    o={}
    o['g1t']=f(np.asarray(p['norm1_g'])[0].reshape(16,128).T)
    o['gqr']=f(np.tile(np.asarray(p['q_norm_g'])[0][None],(128,1))); o['gkr']=f(np.tile(np.asarray(p['k_norm_g'])[0][None],(128,1)))
    o['pool_w']=f(np.asarray(p['pool_w'])[0].reshape(4,2,128,256).transpose(2,0,1,3).reshape(128,8,256))
    o['pool_sc']=f(np.asarray(p['pool_scale'])[0].reshape(8,128).T)
    o['w_in']=f(np.asarray(p['w_in'])[0]); o['w_out']=f(np.asarray(p['w_out'])[0])
    o['g2r']=f(np.tile(np.asarray(p['norm2_g'])[0][None],(128,1)))
    wr=np.concatenate([np.asarray(p['w_router_group'])[0],np.asarray(p['w_router_expert'])[0]],1)
    o['w_r']=f(wr.reshape(16,128,36).transpose(1,0,2))
    o['b_r']=f(np.tile(np.concatenate([np.asarray(p['b_router_group'])[0],np.asarray(p['b_router_expert'])[0]])[None],(128,1)))
    o['w_gate']=f(np.asarray(p['w_gate'])[0]); o['w_up']=f(np.asarray(p['w_up'])[0]); o['w_down']=f(np.asarray(p['w_down'])[0])
    return o

def kernel(**inputs):
    x=np.asarray(inputs['x'],dtype=np.float32); meta=np.asarray(inputs['meta_tokens'],dtype=np.float32)
    B,S,D=x.shape
    NKVT=S//128; NQT=NKVT//4; NBLK=2*NQT+32
    P=layout_params(inputs)
    mp=np.zeros((128,D),np.float32); mp[:N_META]=meta
    nc=build(NKVT,NQT)
    in_maps=[]
    for c in range(8):
        b=c//4; j=c%4; r0=j*NQT*128; r1=r0+NQT*128
        m=dict(P); m.update(const_inputs(j,NKVT,NQT,r0,NBLK))
        m['xkv']=np.ascontiguousarray(x[b]); m['xq']=np.ascontiguousarray(x[b,r0:r1]); m['meta']=mp
        xh=np.zeros((128,D),np.float32)
        xh[0:8]=meta[8:16] if j==0 else x[b,r0-8:r0]
        if j<3: xh[8:16]=x[b,r1:r1+8]
        m['xh']=xh
        in_maps.append(m)
    res=run_bass_kernel_spmd(nc,in_maps,core_ids=list(range(8)))
    out=np.zeros((B,S,D),np.float32)
    for c in range(8):
        b=c//4; j=c%4; r0=j*NQT*128
        out[b,r0:r0+NQT*128]=np.asarray(res.results[c]['out'],dtype=np.float32)
    return out
```

```python
from contextlib import ExitStack
import ml_dtypes
from concourse.bass_utils import run_bass_kernel_spmd
import numpy as np
import concourse.bass as bass
import concourse.mybir as mybir
F32=mybir.dt.float32; BF16=mybir.dt.bfloat16; I32=mybir.dt.int32
AF=mybir.ActivationFunctionType; ALU=mybir.AluOpType; AX=mybir.AxisListType

class EngQ:
    def __init__(self, nc, es, name, e):
        self.name=name; self.e=e; self.h=es.enter_context(nc.semaphore("tl_"+name)); self.cnt=0; self.seen={}; self.uid="tl_"+name
    def wait(self, *evs):
        for ev in evs:
            if ev is None: continue
            if isinstance(ev, (list,tuple)) and len(ev)>0 and not hasattr(ev[0],'h'):
                self.wait(*ev); continue
            src,val=ev
            if src is self and False: pass
            if self.seen.get(src.uid,0)>=val: continue
            self.e.wait_ge(src.h,val); self.seen[src.uid]=val
    def sig(self, ins):
        self.cnt+=1; ins.then_inc(self.h,1); return (self,self.cnt)

class DSem:
    def __init__(self, nc, es, name):
        self.h=es.enter_context(nc.semaphore(name)); self.cnt=0; self.uid=name

class K:
    def __init__(self, nc, es):
        self.nc=nc; self.es=es
        self.pe=EngQ(nc,es,"pe",nc.tensor); self.dve=EngQ(nc,es,"dve",nc.vector)
        self.act=EngQ(nc,es,"act",nc.scalar); self.pool=EngQ(nc,es,"pool",nc.gpsimd); self.sp=EngQ(nc,es,"sp",nc.sync)
        self.engs=[self.pe,self.dve,self.act,self.pool,self.sp]
        self._n=0; self.pes=es
    def sb(self, shape, dt, name=None):
        self._n+=1; return self.pes.enter_context(self.nc.sbuf_tensor("s_"+(name or f"sb{self._n}"), shape, dt))
    def ps(self, shape, dt, name=None):
        self._n+=1; return self.pes.enter_context(self.nc.psum_tensor("p_"+(name or f"ps{self._n}"), shape, dt))
    def dsem(self, name=None):
        self._n+=1; return DSem(self.nc,self.es,name or f"ds{self._n}")
    def dma(self, q, sem, out, in_, deps=(), **kw):
        q.wait(*deps)
        ins=q.e.dma_start(out=out,in_=in_,**kw); ins.then_inc(sem.h,16); sem.cnt+=16
        return (sem,sem.cnt)
    def barrier(self, extra=()):
        last=[(q,q.cnt) for q in self.engs if q.cnt>0]
        for q in self.engs:
            for ev in last:
                q.wait(ev)
            q.wait(*extra)
    def begin_phase(self):
        from contextlib import ExitStack
        self.pes=ExitStack(); self.pes.__enter__()
    def end_phase(self, extra=()):
        self.barrier(extra)
        self.pes.__exit__(None,None,None); self.pes=self.es

BF=ml_dtypes.bfloat16
N_META=16; GRID_W=64; L=16400; S=16384
def rope_tables(pos_row, pos_col):
    inv=np.power(10000.0,-np.arange(0,64,2,dtype=np.float32)/64).astype(np.float32)
    ar=pos_row[:,None].astype(np.float32)*inv[None,:]; ac=pos_col[:,None].astype(np.float32)*inv[None,:]
    cr,sr,cc,sc=np.cos(ar),np.sin(ar),np.cos(ac),np.sin(ac)
    C=np.concatenate([cr,cr,cc,cc],1).astype(np.float32); Sg=np.concatenate([-sr,sr,-sc,sc],1).astype(np.float32)
    return C,Sg
def band_mats(j, NQT):
    A=np.zeros((6,4,128,128),np.float32)
    wins=(2,4,8,16)
    for g,w in enumerate(wins):
        for out in range(128):
            lo=out-w//2; hi=out+w//2-1
            for off in range(lo,hi+1):
                if 0<=off<128: A[0,g,off,out]+=1.0/w
                elif off<0:
                    A[1,g,128+off,out]+=1.0/w
                    A[3,g,8+off,out]+=1.0/w
                else:
                    A[2,g,off-128,out]+=1.0/w
                    A[4,g,8+(off-128),out]+=1.0/w
            A[0,g,out,out]-=1.0
        A[5,g]=A[0,g]
        if j==3:
            for out in range(128):
                t=16+S-128+out
                lo_t=max(t-w//2,0); hi_t=min(t-w//2+w,L)
                cnt=hi_t-lo_t
                if cnt!=w:
                    A[5,g,:,out]=0
                    for tt in range(lo_t,hi_t):
                        off=tt-(16+S-128)
                        if 0<=off<128: A[5,g,off,out]+=1.0/cnt
                    A[5,g,out,out]-=1.0
    return np.ascontiguousarray(A.reshape(24,128,128).transpose(1,0,2)).astype(BF)


def prep_consts(k, ident_src=None):
    nc=k.nc
    c={}
    c['ones_bf']=k.sb([128,128],BF16,"c_ones_bf")
    c['ident_bf']=k.sb([128,128],BF16,"c_ident_bf")
    c['ident_f']=k.sb([128,128],F32,"c_ident_f")
    return c

def phase_a(k, C, d, NKVT, NQT):
    nc=k.nc; pe=k.pe; act=k.act; dve=k.dve; pool=k.pool; sp=k.sp
    EPS=1e-6
    wsb=k.sb([128,16,2560],BF16,"w_in_bf")
    wst=[k.sb([128,2560],F32,f"wst{i}") for i in range(2)]
    g1=k.sb([128,16],F32,"g1t");
    gqr=k.sb([128,128],F32,"gqr"); gkr=k.sb([128,128],F32,"gkr")
    pw=k.sb([128,8,256],BF16,"pool_w_bf"); pwst=k.sb([128,8,256],F32,"pool_w_st")
    psc=k.sb([128,8],F32,"pool_sc")
    Aband=k.sb([128,24,128],BF16,"Aband")
    cs=k.dsem("a_const"); wl=[k.dsem(f"a_wl{i}") for i in range(2)]
    k.dma(sp,cs,g1[:],d['g1t'][:,:]); k.dma(sp,cs,gqr[:],d['gqr'][:,:]); k.dma(sp,cs,gkr[:],d['gkr'][:,:])
    k.dma(sp,cs,pwst[:],d['pool_w'][:,:,:]); k.dma(sp,cs,psc[:],d['pool_sc'][:,:]);
    ev_c=k.dma(sp,cs,Aband[:],d['Aband'][:,:,:])
    ident=C['ident_bf']
    epsb=k.sb([128,1],F32,"epsb"); e_eps=dve.sig(nc.vector.memset(epsb[:],EPS)); act.wait(e_eps)
    k.dma(sp,cs,ident[:],d['ident_bf'][:,:]); ev_c=k.dma(sp,cs,C['ident_f'][:],d['ident_f'][:,:])
    dve.wait(ev_c)
    ev_pw=dve.sig(nc.vector.tensor_copy(out=pw[:],in_=pwst[:]))
    wev=[None,None]; cev=[None,None]; w_done=None
    for c in range(16):
        s=c%2
        wev[s]=k.dma(sp,wl[s],wst[s][:],d['w_in'][c*128:(c+1)*128,:],deps=[cev[s]])
        dve.wait(wev[s])
        cev[s]=dve.sig(nc.vector.tensor_scalar(out=wsb[:,c,:],in0=wst[s][:],scalar1=g1[:,c:c+1],scalar2=None,op0=ALU.mult))
    w_done=cev
    xt=[k.sb([128,2048],F32,f"xt{i}") for i in range(2)]
    xb=[k.sb([128,2048],BF16,f"xb{i}") for i in range(2)]
    xT=[k.sb([128,16,128],BF16,f"xT{i}") for i in range(2)]
    junk=k.sb([128,2048],BF16,"junk")
    ssq=[k.sb([128,1],F32,f"ssq{i}") for i in range(2)]
    rstd=[k.sb([128,1],F32,f"rstd{i}") for i in range(2)]
    ct=[k.sb([128,128],F32,f"ct{i}") for i in range(2)]; stt=[k.sb([128,128],F32,f"stt{i}") for i in range(2)]
    xl=[k.dsem(f"a_xl{i}") for i in range(2)]
    tp_ps=[k.ps([128,8,128],BF16,f"tp_ps{i}") for i in range(2)]
    pj_ps=[k.ps([128,512],F32,f"pj_ps{i}") for i in range(4)]
    qs=k.sb([128,1024],F32,"qs"); sq=k.sb([128,1024],F32,"sq"); t1=k.sb([128,1024],F32,"t1"); t2=k.sb([128,1024],F32,"t2")
    hs=k.sb([128,8],F32,"hs"); hr=k.sb([128,8],F32,"hr")
    qb=k.sb([128,1024],BF16,"qbf"); qT_sb=[k.sb([128,8,128],BF16,f"qT_sb{i}") for i in range(2)]
    v_sb=[k.sb([128,256],BF16,f"v_sbA{i}") for i in range(2)]
    pring=[k.sb([128,1024],BF16,f"pring{i}") for i in range(4)]
    mT=k.sb([128,8,128],BF16,"mT"); yT=[k.sb([128,8,128],BF16,f"yT{i}") for i in range(2)]
    st_q=[k.dsem(f"a_stq{i}") for i in range(2)]; st_v=[k.dsem(f"a_stv{i}") for i in range(2)]; st_y=[k.dsem(f"a_sty{i}") for i in range(2)]
    st_ev_q=[None,None]; st_ev_v=[None,None]; st_ev_y=[None,None]
    out_evs=[]
    state={'n':0,'free_x':[None,None],'free_xT':[None,None],'tp_free':[None,None],'pj_free':[None]*4,'pjn':0, 'work':None,'qT_n':0,'v_n':0}

    def norm_rope(src_ps_list, H, rst, gr, ctile, stile, dest_bf, extra_wait=()):
        W=H*128
        evs=[]
        for bi,(pst,ev) in enumerate(src_ps_list):
            act.wait(ev, state['work'])
            w=min(512,W-bi*512)
            evs.append(act.sig(nc.scalar.activation(out=qs[:,bi*512:bi*512+w],in_=pst[:,0:w],func=AF.Copy,scale=rst[:,0:1])))
        dve.wait(*evs); dve.wait(*extra_wait)
        e=dve.sig(nc.vector.tensor_tensor(out=sq[:,0:W],in0=qs[:,0:W],in1=qs[:,0:W],op=ALU.mult)); dve.wait(e)
        e=dve.sig(nc.vector.tensor_reduce(out=hs[:,0:H],in_=sq[:,0:W].rearrange("p (h d) -> p h d",h=H),axis=AX.X,op=ALU.add)); dve.wait(e)
        act.wait(e)
        e=act.sig(nc.scalar.activation(out=hr[:,0:H],in_=hs[:,0:H],func=AF.Sqrt,scale=1.0/128,bias=epsb[:,0:1])); dve.wait(e)
        e=dve.sig(nc.vector.reciprocal(out=hr[:,0:H],in_=hr[:,0:H])); dve.wait(e)
        q3=qs[:,0:W].rearrange("p (h d) -> p h d",h=H)
        e=dve.sig(nc.vector.tensor_tensor(out=sq[:,0:W].rearrange("p (h d) -> p h d",h=H),in0=q3,in1=hr[:,0:H].unsqueeze(2).to_broadcast([128,H,128]),op=ALU.mult)); dve.wait(e)
        s3=sq[:,0:W].rearrange("p (h d) -> p h d",h=H)
        e=dve.sig(nc.vector.tensor_tensor(out=q3,in0=s3,in1=gr[:].unsqueeze(1).to_broadcast([128,H,128]),op=ALU.mult)); dve.wait(e)
        e1=dve.sig(nc.vector.tensor_tensor(out=t1[:,0:W].rearrange("p (h d) -> p h d",h=H),in0=q3,in1=ctile[:].unsqueeze(1).to_broadcast([128,H,128]),op=ALU.mult))
        q5=qs[:,0:W].rearrange("p (h b f e) -> p h b f e",h=H,b=2,f=2)
        t5=t2[:,0:W].rearrange("p (h b f e) -> p h b f e",h=H,b=2,f=2)
        s5=stile[:].rearrange("p (b f e) -> p b f e",b=2,f=2)
        for f in range(2):
            for b in range(2):
                e2=dve.sig(nc.vector.tensor_tensor(out=t5[:,:,b,f,:],in0=q5[:,:,b,1-f,:],in1=s5[:,b,f,:].unsqueeze(1).to_broadcast([128,H,32]),op=ALU.mult))
        dve.wait(e1,e2)
        e=dve.sig(nc.vector.tensor_tensor(out=dest_bf[:,0:W],in0=t1[:,0:W],in1=t2[:,0:W],op=ALU.add))
        state['work']=e
        return e

    def tile(src_ap, rows, ctab, stab, tok0, do_kv=None, do_q=None, do_pool=None):
        n=state['n']; s=n%2; state['n']+=1
        dd=[state['free_x'][s]]
        if rows<128:
            dve.wait(state['free_x'][s]); z=dve.sig(nc.vector.memset(xt[s][:],0.0)); dd=[z]
        evx=k.dma(sp,xl[s],xt[s][0:rows,:],src_ap,deps=dd)
        if ctab is not None:
            k.dma(sp,xl[s],ct[s][0:rows,:],ctab[tok0:tok0+rows,:],deps=dd)
            evx=k.dma(sp,xl[s],stt[s][0:rows,:],stab[tok0:tok0+rows,:],deps=dd)
        act.wait(evx)
        e_ss=act.sig(nc.scalar.activation(out=junk[:],in_=xt[s][:],func=AF.Square,accum_out=ssq[s][:]))
        pool.wait(evx, state['free_xT'][s])
        e_xb=pool.sig(nc.gpsimd.tensor_copy(out=xb[s][:],in_=xt[s][:]))
        act.wait(e_ss)
        e=act.sig(nc.scalar.activation(out=rstd[s][:],in_=ssq[s][:],func=AF.Sqrt,scale=1.0/2048,bias=epsb[:,0:1])); dve.wait(e)
        e_rs=dve.sig(nc.vector.reciprocal(out=rstd[s][:],in_=rstd[s][:]))
        pe.wait(e_xb)
        tev=[]
        for half in range(2):
            pe.wait(state['tp_free'][half])
            for c8 in range(8):
                c=half*8+c8
                ins=nc.tensor.transpose(tp_ps[half][:,c8,:],xb[s][:,c*128:(c+1)*128],ident[:])
            tev.append(pe.sig(ins))
        state['free_x'][s]=None
        dve.wait(tev[0], state['free_xT'][s]);
        e0=dve.sig(nc.vector.tensor_copy(out=xT[s][:,0:8,:],in_=tp_ps[0][:]))
        act.wait(tev[1], state['free_xT'][s])
        e1=act.sig(nc.scalar.copy(out=xT[s][:,8:16,:],in_=tp_ps[1][:]))
        state['tp_free']=[e0,e1]
        xT_ready=[e0,e1]
        def proj(col0, ncols):
            res=[]
            for b0 in range(0,ncols,512):
                w=min(512,ncols-b0); j=state['pjn']%4; state['pjn']+=1
                pe.wait(xT_ready, w_done, state['pj_free'][j])
                for c in range(16):
                    ins=nc.tensor.matmul(pj_ps[j][:,0:w],lhsT=xT[s][:,c,:],rhs=wsb[:,c,col0+b0:col0+b0+w],start=(c==0),stop=(c==15))
                res.append((pj_ps[j],pe.sig(ins),j))
            return res
        last_pe=None
        if do_kv is not None:
            KT,V,tidx=do_kv
            r=proj(1024,512)[0]; pst,ev,j=r
            vs=state['v_n']%2; state['v_n']+=1
            act.wait(ev, e_rs, st_ev_v[vs])
            e_v=act.sig(nc.scalar.activation(out=v_sb[vs][:],in_=pst[:,256:512],func=AF.Copy,scale=rstd[s][:,0:1]))
            st_ev_v[vs]=k.dma(sp,st_v[vs],V[tidx,0:rows,:],v_sb[vs][0:rows,:],deps=[e_v]); out_evs.append(st_ev_v[vs])
            e_k=norm_rope([(pst,ev)],2,rstd[s],gkr,ct[s],stt[s],qb,extra_wait=[e_rs])
            state['pj_free'][j]=[e_k,e_v]
            qs_=state['qT_n']%2; state['qT_n']+=1
            pe.wait(e_k, state['tp_free'][0])
            for h in range(2):
                ins=nc.tensor.transpose(tp_ps[0][:,h,:],qb[:,h*128:(h+1)*128],ident[:])
            e_t=pe.sig(ins)
            dve.wait(e_t, st_ev_q[qs_])
            e_c=dve.sig(nc.vector.tensor_copy(out=qT_sb[qs_][:,0:2,:],in_=tp_ps[0][:,0:2,:]))
            state['tp_free'][0]=e_c
            st_ev_q[qs_]=k.dma(sp,st_q[qs_],KT[:,:,tok0:tok0+rows].rearrange("h d t -> d h t"),qT_sb[qs_][:,0:2,0:rows],deps=[e_c]); out_evs.append(st_ev_q[qs_])
            last_pe=e_t
        if do_q is not None:
            QT=do_q
            r=proj(0,1024)
            e_q=norm_rope([(r[0][0],r[0][1]),(r[1][0],r[1][1])],8,rstd[s],gqr,ct[s],stt[s],qb,extra_wait=[e_rs])
            state['pj_free'][r[0][2]]=e_q; state['pj_free'][r[1][2]]=e_q
            qs_=state['qT_n']%2; state['qT_n']+=1
            pe.wait(e_q, state['tp_free'][0])
            for h in range(8):
                ins=nc.tensor.transpose(tp_ps[0][:,h,:],qb[:,h*128:(h+1)*128],ident[:])
            e_t=pe.sig(ins)
            dve.wait(e_t, st_ev_q[qs_])
            e_c=dve.sig(nc.vector.tensor_copy(out=qT_sb[qs_][:],in_=tp_ps[0][:]))
            state['tp_free'][0]=e_c
            st_ev_q[qs_]=k.dma(sp,st_q[qs_],QT[:,:,tok0:tok0+128].rearrange("h d t -> d h t"),qT_sb[qs_][:],deps=[e_c]); out_evs.append(st_ev_q[qs_])
            last_pe=e_t
        if do_pool is not None:
            slot,prev_free=do_pool
            r=proj(1536,1024)
            evs=[]
            act.wait(e_rs, prev_free)
            for bi in range(2):
                act.wait(r[bi][1])
                e=act.sig(nc.scalar.activation(out=pring[slot][:,bi*512:(bi+1)*512],in_=r[bi][0][:],func=AF.Copy,scale=rstd[s][:,0:1]))
                state['pj_free'][r[bi][2]]=e; evs.append(e)
            state['p_ready']=evs[-1]
            last_pe=r[1][1]
        state['free_x'][s]=last_pe if last_pe is not None else None
        state['free_x'][s]=[e_ss,e_xb]
        state['free_xT'][s]=last_pe
        return

    def pool_tile(i, srcs, mixT):
        ys=i%2
        pe.wait(state['p_ready'], state['tp_free'][1], ev_c)
        mps=pj_ps[0];
        j0=state['pjn']%4; j1=(state['pjn']+1)%4; state['pjn']+=2
        pe.wait(state['pj_free'][j0], state['pj_free'][j1])
        for ch in range(8):
            g=ch//2; bank=pj_ps[j0] if ch<4 else pj_ps[j1]
            for si,(slot,ab) in enumerate(srcs):
                ins=nc.tensor.matmul(bank[:,(ch%4)*128:(ch%4+1)*128],lhsT=pring[slot][:,ch*128:(ch+1)*128],rhs=Aband[:,ab*4+g,:],start=(si==0),stop=(si==len(srcs)-1))
        e_m=pe.sig(ins)
        dve.wait(e_m, state.get('mT_free'))
        e0=dve.sig(nc.vector.tensor_copy(out=mT[:,0:4,:],in_=pj_ps[j0][:].rearrange("p (c t) -> p c t",c=4)))
        e1=dve.sig(nc.vector.tensor_copy(out=mT[:,4:8,:],in_=pj_ps[j1][:].rearrange("p (c t) -> p c t",c=4)))
        pe.wait(e0,e1,ev_pw)
        for ch in range(8):
            g=ch//2; dd=ch%2; bank=pj_ps[j0] if ch<4 else pj_ps[j1]
            for cc in range(2):
                ins=nc.tensor.matmul(bank[:,(ch%4)*128:(ch%4+1)*128],lhsT=pw[:,g*2+cc,dd*128:(dd+1)*128],rhs=mT[:,g*2+cc,:],start=(cc==0),stop=(cc==1))
        e_y=pe.sig(ins)
        state['mT_free']=e_y
        dve.wait(e_y, st_ev_y[ys])
        for hb,bank in enumerate((pj_ps[j0],pj_ps[j1])):
            e=dve.sig(nc.vector.tensor_tensor(out=yT[ys][:,hb*4:(hb+1)*4,:],in0=bank[:].rearrange("p (c t) -> p c t",c=4),in1=psc[:,hb*4:(hb+1)*4].unsqueeze(2).to_broadcast([128,4,128]),op=ALU.mult))
        state['pj_free'][j0]=e; state['pj_free'][j1]=e
        st_ev_y[ys]=k.dma(sp,st_y[ys],mixT[8:16,:,i*128:(i+1)*128].rearrange("c d t -> d c t"),yT[ys][:],deps=[e]); out_evs.append(st_ev_y[ys])
        return e_m

    for t in range(NKVT):
        tile(d['xkv'][t*128:(t+1)*128,:],128,d['ckv'],d['skv'],t*128,do_kv=(d['KT'],d['V'],t))
    tile(d['meta'][0:16,:],16,d['ckv'],d['skv'],NKVT*128,do_kv=(d['KT'],d['V'],NKVT))
    tile(d['xh'][:,:],128,None,None,0,do_pool=(3,None))
    em=[None]*(NQT+1)
    def srcs_for(i):
        sr=[]
        sr.append(((i-1)%3,1) if i>0 else (3,3))
        sr.append((i%3,0 if i<NQT-1 else 5))
        sr.append(((i+1)%3,2) if i<NQT-1 else (3,4))
        return sr
    for i in range(NQT):
        tile(d['xq'][i*128:(i+1)*128,:],128,d['cq'],d['sq'],i*128,do_q=d['QT'],do_pool=(i%3,em[i-2] if i>=2 else None))
        if i>=1: em[i-1]=pool_tile(i-1,srcs_for(i-1),d['mixT'])
    em[NQT-1]=pool_tile(NQT-1,srcs_for(NQT-1),d['mixT'])
    return out_evs


def attention_phase(k, QT, KT, V, attnT, nbias, ones_bf, NQT, NKF, PART, scale, deps=(), bg=None, bg_every=40):
    nc=k.nc
    NQ=NQT*128; NK=NKF*128+PART; NKT=NKF+(1 if PART else 0)
    kt_sb=k.sb([128,NK],BF16,"kt_sb"); v_sb=k.sb([128,NKT,128],BF16,"v_sb"); q_sb=k.sb([128,4,NQ],BF16,"q_sb")
    pT=[k.sb([128,512],BF16,f"pT{i}") for i in range(2)]
    rec=k.sb([128,512],F32,"rec"); ob=[k.sb([128,512],BF16,f"ob{i}") for i in range(2)]
    s_ps=[k.ps([128,512],F32,f"s_ps{i}") for i in range(3)]
    o_ps=[k.ps([128,512],F32,f"o_ps{i}") for i in range(2)]
    l_ps=[k.ps([128,512],F32,f"l_ps{i}") for i in range(2)]
    ld=k.dsem("attn_ld"); st=[k.dsem(f"attn_st{i}") for i in range(2)]
    out_evs=[]; pe=k.pe; act=k.act; dve=k.dve; sp=k.sp
    last_pe_of_head=None; norm_ev=[None,None]; st_ev=[None,None]
    gq=0
    for h in range(2):
        dd=list(deps)+([last_pe_of_head] if last_pe_of_head else [])
        NCH=8
        cw=(NK+NCH-1)//NCH
        for c in range(NCH):
            lo=c*cw; hi=min(NK,lo+cw)
            ev_k=k.dma(sp,ld,kt_sb[:,lo:hi],KT[h,:,lo:hi],deps=dd)
        ev_v=k.dma(sp,ld,v_sb[:,0:NKF,:],V[0:NKF,:,h*128:(h+1)*128].rearrange("t p d -> p t d"),deps=dd)
        if PART:
            ev_v=k.dma(sp,ld,v_sb[0:PART,NKF,:],V[NKF,0:PART,h*128:(h+1)*128],deps=dd)
        ev_q=k.dma(sp,ld,q_sb[:],QT[4*h:4*h+4].rearrange("h d q -> d h q"),deps=dd)
        ld_ev=(ld,ld.cnt)
        steps=[(qi,kt) for qi in range(NQT) for kt in range(NKT)]
        n=len(steps)
        s_ev=[None]*n; e_ev=[None]*n
        def issue_S(i):
            qi,kt=steps[i]; rows=128 if kt<NKF else PART
            pe.wait(ld_ev)
            ins=nc.tensor.matmul(s_ps[i%3][0:rows,:], lhsT=kt_sb[:,kt*128:kt*128+rows], rhs=q_sb[:,:,qi*128:(qi+1)*128], start=True, stop=True)
            s_ev[i]=pe.sig(ins)
        issue_S(0)
        if n>1: issue_S(1)
        for i,(qi,kt) in enumerate(steps):
            rows=128 if kt<NKF else PART
            par=(gq+qi)%2
            act.wait(s_ev[i])
            ins=nc.scalar.activation(out=pT[i%2][0:rows,:], in_=s_ps[i%3][0:rows,:], func=AF.Exp, bias=nbias[0:rows,0:1], scale=scale)
            e_ev[i]=act.sig(ins)
            pe.wait(e_ev[i])
            if kt==0: pe.wait(norm_ev[par])
            nc.tensor.matmul(o_ps[par][:], lhsT=v_sb[0:rows,kt,:], rhs=pT[i%2][0:rows,:], start=(kt==0), stop=(kt==NKT-1))
            ins=nc.tensor.matmul(l_ps[par][:], lhsT=ones_bf[0:rows,:], rhs=pT[i%2][0:rows,:], start=(kt==0), stop=(kt==NKT-1))
            pv_ev=pe.sig(ins)
            if i+2<n: issue_S(i+2)
            if bg is not None and i%bg_every==bg_every-1: bg()
            if kt==NKT-1:
                dve.wait(pv_ev, st_ev[par], norm_ev[1-par])
                r1=dve.sig(nc.vector.reciprocal(out=rec[:], in_=l_ps[par][:]))
                dve.wait(r1)
                ins=nc.vector.tensor_tensor(out=ob[par][:], in0=o_ps[par][:], in1=rec[:], op=ALU.mult)
                norm_ev[par]=dve.sig(ins)
                st_ev[par]=k.dma(sp,st[par],attnT[4*h:4*h+4,:,qi*128:(qi+1)*128].rearrange("h d q -> d h q"),
                                 ob[par][:].rearrange("d (h q) -> d h q",h=4),deps=[norm_ev[par]])
                out_evs.append(st_ev[par])
                last_pe_of_head=pv_ev
        gq+=NQT
    while bg is not None and bg(): pass
    return out_evs


def phase_c(k, C, d, NQT, NBLK):
    nc=k.nc; pe=k.pe; act=k.act; dve=k.dve; pool=k.pool; sp=k.sp
    EPS=1e-6; T=NQT; S2=2*T
    ident_f=C['ident_f']
    wo=k.sb([128,16,2048],BF16,"wo"); wst=[k.sb([128,2048],F32,f"wost{i}") for i in range(2)]
    g2r=k.sb([128,2048],F32,"g2r"); wr=k.sb([128,16,36],F32,"wr"); brr=k.sb([128,36],F32,"brr")
    epsb=k.sb([128,1],F32,"epsb2")
    LGT=C['LGT']
    cs=k.dsem("c_const"); wl=[k.dsem(f"c_wl{i}") for i in range(2)]
    k.dma(sp,cs,g2r[:],d['g2r'][:,:]); k.dma(sp,cs,wr[:],d['w_r'][:,:,:]); ev_c=k.dma(sp,cs,brr[:],d['b_r'][:,:])
    e_eps=dve.sig(nc.vector.memset(epsb[:],EPS))
    wev=[None,None]; cev=[None,None]
    for c in range(16):
        s=c%2
        wev[s]=k.dma(sp,wl[s],wst[s][:],d['w_out'][c*128:(c+1)*128,:],deps=[cev[s]])
        q=dve if s==0 else pool
        q.wait(wev[s])
        cev[s]=q.sig((nc.vector if s==0 else nc.gpsimd).tensor_copy(out=wo[:,c,:],in_=wst[s][:]))
    w_done=list(cev)
    mx=[k.sb([128,16,128],BF16,f"mx{i}") for i in range(2)]
    xt=[k.sb([128,2048],F32,f"cxt{i}") for i in range(2)]
    h1=[k.sb([128,2048],F32,f"h1_{i}") for i in range(2)]
    bf=k.sb([128,2048],F32,"bf"); b16=[k.sb([128,2048],BF16,f"b16_{i}") for i in range(2)]
    bT=k.sb([128,16,128],F32,"bT32"); junk=k.sb([128,2048],BF16,"cjunk")
    ssq=k.sb([128,1],F32,"cssq"); rstd=k.sb([128,1],F32,"crstd")
    po=[k.ps([128,512],F32,f"po{i}") for i in range(4)]
    tp=[k.ps([128,4,128],F32,f"ctp{i}") for i in range(2)]
    lp=k.ps([128,36],F32,"lp")
    ld=[k.dsem(f"c_ld{i}") for i in range(2)]; sth=[k.dsem(f"c_sth{i}") for i in range(2)]; stb=[k.dsem(f"c_stb{i}") for i in range(2)]
    mx_free=[None,None]; xt_free=[None,None]; h1_free=[None,None]; b16_free=[None,None]; po_free=[None]*4; tp_free=[None,None]
    bf_free=None; bT_free=None; lp_free=None
    out_evs=[]
    for i in range(T):
        s=i%2
        ev_m=k.dma(sp,ld[s],mx[s][:],d['mixT'][:,:,i*128:(i+1)*128].rearrange("c d t -> d c t"),deps=[mx_free[s]])
        ev_x=k.dma(sp,ld[s],xt[s][:],d['xq'][i*128:(i+1)*128,:],deps=[xt_free[s]])
        pe.wait(ev_x, w_done)
        pev=[]
        for n in range(4):
            pe.wait(po_free[n])
            for c in range(16):
                ins=nc.tensor.matmul(po[n][:],lhsT=mx[s][:,c,:],rhs=wo[:,c,n*512:(n+1)*512],start=(c==0),stop=(c==15))
            pev.append(pe.sig(ins))
        mx_free[s]=pev[3]
        dve.wait(h1_free[s])
        for n in range(4):
            dve.wait(pev[n])
            e=dve.sig(nc.vector.tensor_tensor(out=h1[s][:,n*512:(n+1)*512],in0=po[n][:],in1=xt[s][:,n*512:(n+1)*512],op=ALU.add))
            po_free[n]=e
        e_h1=e; xt_free[s]=e
        ev_sh=k.dma(sp,sth[s],d['h1'][i*128:(i+1)*128,:],h1[s][:],deps=[e_h1]); out_evs.append(ev_sh)
        act.wait(e_h1, e_eps)
        e=act.sig(nc.scalar.activation(out=junk[:],in_=h1[s][:],func=AF.Square,accum_out=ssq[:])); act.wait(e)
        e=act.sig(nc.scalar.activation(out=rstd[:],in_=ssq[:],func=AF.Sqrt,scale=1.0/2048,bias=epsb[:,0:1])); dve.wait(e)
        e=dve.sig(nc.vector.reciprocal(out=rstd[:],in_=rstd[:])); dve.wait(e, bf_free, ev_c)
        e_bf=dve.sig(nc.vector.scalar_tensor_tensor(out=bf[:],in0=h1[s][:],scalar=rstd[:,0:1],in1=g2r[:],op0=ALU.mult,op1=ALU.mult))
        h1_free[s]=[e_bf,ev_sh]
        pool.wait(e_bf, b16_free[s])
        e_b16=pool.sig(nc.gpsimd.tensor_copy(out=b16[s][:],in_=bf[:]))
        b16_free[s]=k.dma(sp,stb[s],d['b16'][i*128:(i+1)*128,:],b16[s][:],deps=[e_b16]); out_evs.append(b16_free[s])
        pe.wait(e_bf)
        tev=[]
        for gch in range(4):
            j=gch%2
            pe.wait(tp_free[j])
            for c4 in range(4):
                c=gch*4+c4
                ins=nc.tensor.transpose(tp[j][:,c4,:],bf[:,c*128:(c+1)*128],ident_f[:])
            e_t=pe.sig(ins)
            q=dve if j==0 else act
            q.wait(e_t, bT_free)
            if j==0: e=dve.sig(nc.vector.tensor_copy(out=bT[:,gch*4:gch*4+4,:],in_=tp[j][:]))
            else: e=act.sig(nc.scalar.copy(out=bT[:,gch*4:gch*4+4,:],in_=tp[j][:]))
            tp_free[j]=e; tev.append(e)
        bf_free=[e_t,e_b16]
        pe.wait(*tev); pe.wait(lp_free, ev_c)
        for c in range(16):
            ins=nc.tensor.matmul(lp[:],lhsT=bT[:,c,:],rhs=wr[:,c,:],start=(c==0),stop=(c==15))
        e_l=pe.sig(ins); bT_free=e_l
        dve.wait(e_l)
        lp_free=dve.sig(nc.vector.tensor_tensor(out=LGT[:,i,:],in0=lp[:],in1=brr[:],op=ALU.add))
    return out_evs

def phase_c2(k, C, d, NQT, NBLK):
    nc=k.nc; pe=k.pe; act=k.act; dve=k.dve; pool=k.pool; sp=k.sp
    T=NQT; S2=2*T; LGT=C['LGT']
    po=[k.ps([128,512],F32,f"c2po{i}") for i in range(2)]; po_free=[None,None]
    junk=k.sb([128,2048],BF16,"c2junk"); b16=[k.sb([128,2048],BF16,f"c2b16_{i}") for i in range(2)]
    ld=[k.dsem(f"c2_ld{i}") for i in range(2)]
    lp_free=None; out_evs=[]
    V=nc.vector
    def dv(ins, *w):
        return dve.sig(ins)
    cnt=[0]
    def T_(shape,dt=F32):
        cnt[0]+=1; return k.sb(shape,dt,f"rt{cnt[0]}")
    LG=LGT[:,:,0:4]; LE=LGT[:,:,4:36]
    gmax=T_([128,T]); dd=T_([128,T,4]); ohg=T_([128,T,4]); eg=T_([128,T,4]); sg=T_([128,T]); gp=T_([128,T])
    tmp=T_([128,T,32]); sel=T_([128,T,8]); v1=T_([128,T]); m1=T_([128,T,8]); sel2=T_([128,T,8]); v2=T_([128,T]); m2=T_([128,T,8])
    rr=T_([128,T]); g1_=T_([128,T]); Mall=T_([128,S2,32]); Mbf=T_([128,S2,32],BF16)
    Utri=T_([128,128],BF16); onesb=T_([128,128],BF16)
    rs=k.dsem("c_rs")
    k.dma(sp,rs,Utri[:],d['Utri'][:,:]); ev_u=k.dma(sp,rs,onesb[:],d['ones_bf'][:,:])
    thr=T_([128,32]); blkst=T_([128,NBLK])
    k.dma(sp,rs,thr[:],d['thr'][:,:]); ev_u=k.dma(sp,rs,blkst[:],d['blkst'][:,:])
    dve.wait(lp_free)
    def op(ins):
        e=dve.sig(ins); dve.wait(e); return e
    op(V.tensor_reduce(out=gmax[:],in_=LG,axis=AX.X,op=ALU.max))
    op(V.tensor_tensor(out=dd[:],in0=LG,in1=gmax[:].unsqueeze(2).to_broadcast([128,T,4]),op=ALU.subtract))
    op(V.tensor_single_scalar(out=ohg[:],in_=dd[:],scalar=0.0,op=ALU.is_ge))
    e=op(V.tensor_copy(out=eg[:],in_=dd[:]))
    act.wait(e); e=act.sig(nc.scalar.activation(out=eg[:],in_=dd[:],func=AF.Exp)); dve.wait(e)
    op(V.tensor_reduce(out=sg[:],in_=eg[:],axis=AX.X,op=ALU.add))
    op(V.reciprocal(out=gp[:],in_=sg[:]))
    op(V.tensor_tensor(out=tmp[:].rearrange("p t (g e) -> p t g e",g=4),in0=LE.rearrange("p t (g e) -> p t g e",g=4),in1=ohg[:].unsqueeze(3).to_broadcast([128,T,4,8]),op=ALU.mult))
    op(V.tensor_reduce(out=sel[:],in_=tmp[:].rearrange("p t (g e) -> p t e g",g=4),axis=AX.X,op=ALU.add))
    op(V.tensor_reduce(out=v1[:],in_=sel[:],axis=AX.X,op=ALU.max))
    op(V.tensor_tensor(out=m1[:],in0=sel[:],in1=v1[:].unsqueeze(2).to_broadcast([128,T,8]),op=ALU.is_ge))
    op(V.scalar_tensor_tensor(out=sel2[:],in0=m1[:],scalar=-1e30,in1=sel[:],op0=ALU.mult,op1=ALU.add))
    op(V.tensor_reduce(out=v2[:],in_=sel2[:],axis=AX.X,op=ALU.max))
    op(V.tensor_tensor(out=m2[:],in0=sel2[:],in1=v2[:].unsqueeze(2).to_broadcast([128,T,8]),op=ALU.is_ge))
    e=op(V.tensor_tensor(out=rr[:],in0=v2[:],in1=v1[:],op=ALU.subtract))
    act.wait(e); e=act.sig(nc.scalar.activation(out=rr[:],in_=rr[:],func=AF.Exp)); dve.wait(e)
    op(V.tensor_scalar(out=rr[:],in0=rr[:],scalar1=1.0,scalar2=None,op0=ALU.add))
    op(V.reciprocal(out=rr[:],in_=rr[:]))
    gates=C['gates']
    op(V.tensor_tensor(out=gates[:,:,0],in0=gp[:],in1=rr[:],op=ALU.mult))
    op(V.tensor_tensor(out=gates[:,:,1],in0=gp[:],in1=gates[:,:,0],op=ALU.subtract))
    M4=Mall[:].rearrange("p (t k) (g e) -> p t k g e",k=2,g=4)
    for kk,mk in enumerate((m1,m2)):
        op(V.tensor_tensor(out=M4[:,:,kk,:,:],in0=ohg[:].unsqueeze(3).to_broadcast([128,T,4,8]),in1=mk[:].unsqueeze(2).to_broadcast([128,T,4,8]),op=ALU.mult))
    e_mb=op(V.tensor_copy(out=Mbf[:],in_=Mall[:]))
    R=T_([128,S2,32]); Tot=T_([128,S2,32])
    nch=(S2*32+511)//512
    pe.wait(e_mb, ev_u)
    Mflat=Mbf[:].rearrange("p s e -> p (s e)"); Rflat=R[:].rearrange("p s e -> p (s e)"); Tflat=Tot[:].rearrange("p s e -> p (s e)")
    for c in range(nch):
        w=min(512,S2*32-c*512)
        pe.wait(po_free[0],po_free[1])
        ins=nc.tensor.matmul(po[0][:,0:w],lhsT=Utri[:],rhs=Mflat[:,c*512:c*512+w],start=True,stop=True)
        ins=nc.tensor.matmul(po[1][:,0:w],lhsT=onesb[:],rhs=Mflat[:,c*512:c*512+w],start=True,stop=True)
        e=pe.sig(ins); dve.wait(e)
        op(V.tensor_copy(out=Rflat[:,c*512:c*512+w],in_=po[0][:,0:w]))
        e=op(V.tensor_copy(out=Tflat[:,c*512:c*512+w],in_=po[1][:,0:w]))
        po_free[0]=e; po_free[1]=e
    base=T_([128,S2+1,32])
    op(V.memset(base[:,0,:],0.0))
    for s_ in range(S2):
        op(V.tensor_tensor(out=base[:,s_+1,:],in0=base[:,s_,:],in1=Tot[:,s_,:],op=ALU.add))
    counts=base[:,S2,:]
    cmp=T_([128,32,32]); nb=T_([128,32]); padded=T_([128,32]); pst=T_([128,33])
    op(V.tensor_tensor(out=cmp[:],in0=counts.unsqueeze(2).to_broadcast([128,32,32]),in1=thr[:].unsqueeze(1).to_broadcast([128,32,32]),op=ALU.is_gt))
    op(V.tensor_reduce(out=nb[:],in_=cmp[:],axis=AX.X,op=ALU.add))
    op(V.tensor_scalar(out=padded[:],in0=nb[:],scalar1=128.0,scalar2=None,op0=ALU.mult))
    op(V.memset(pst[:,0:1],0.0))
    for e_ in range(32):
        op(V.tensor_tensor(out=pst[:,e_+1:e_+2],in0=pst[:,e_:e_+1],in1=padded[:,e_:e_+1],op=ALU.add))
    RB=T_([128,S2,32]); posf=T_([128,S2])
    op(V.tensor_tensor(out=RB[:],in0=R[:],in1=base[:,0:S2,:],op=ALU.add))
    op(V.tensor_tensor(out=RB[:],in0=RB[:],in1=pst[:,0:32].unsqueeze(1).to_broadcast([128,S2,32]),op=ALU.add))
    op(V.tensor_tensor(out=RB[:],in0=RB[:],in1=Mall[:],op=ALU.mult))
    op(V.tensor_reduce(out=posf[:],in_=RB[:],axis=AX.X,op=ALU.add))
    e_pos=op(V.tensor_copy(out=C['pos_i'][:],in_=posf[:]))
    cmp2=T_([128,NBLK,32]); bef=T_([128,NBLK])
    op(V.tensor_tensor(out=cmp2[:],in0=pst[:,1:33].unsqueeze(1).to_broadcast([128,NBLK,32]),in1=blkst[:].unsqueeze(2).to_broadcast([128,NBLK,32]),op=ALU.is_le))
    op(V.tensor_reduce(out=bef[:],in_=cmp2[:],axis=AX.X,op=ALU.add))
    op(V.tensor_scalar(out=bef[:],in0=bef[:],scalar1=31.0,scalar2=None,op0=ALU.min))
    e_blk=op(V.tensor_copy(out=C['blk_e'][:],in_=bef[:]))
    sc=k.dsem("c_scat"); zs=k.dsem("c_zero")
    e_z=dve.sig(nc.vector.memset(junk[:],0.0))
    zev=None
    for r0 in range(0,NBLK,8):
        nb_=min(8,NBLK-r0)
        zev=k.dma(sp,zs,d['xs'][r0*128:(r0+nb_)*128,:].rearrange("(r p) n -> p r n",p=128),junk[:].unsqueeze(1).to_broadcast([128,nb_,2048]),deps=[e_z])
    pool.wait(zev)
    sp.wait(*out_evs)
    pool.wait(e_pos)
    scat_free=[None,None]
    for i in range(T):
        s=i%2
        ev=k.dma(sp,ld[s],b16[s][:],d['b16'][i*128:(i+1)*128,:],deps=[scat_free[s]]+out_evs)
        pool.wait(ev)
        for kk in range(2):
            ins=nc.gpsimd.indirect_dma_start(out=d['xs'][:,:],out_offset=bass.IndirectOffsetOnAxis(ap=C['pos_i'][:,i*2+kk:i*2+kk+1],axis=0),in_=b16[s][:],in_offset=None)
            ins.then_inc(sc.h,16); sc.cnt+=16
        scat_free[s]=(sc,sc.cnt)
        pool.wait(scat_free[s])
    return [(sc,sc.cnt), e_blk, e_pos]


class WConv:
    def __init__(self, k, d):
        self.k=k; self.d=d; self.n=0
        self.st=[k.sb([128,4096],F32,f"wc_st{i}") for i in range(2)]; self.bf=[k.sb([128,4096],BF16,f"wc_bf{i}") for i in range(2)]
        self.ld=[k.dsem(f"wc_ld{i}") for i in range(2)]; self.so=[k.dsem(f"wc_so{i}") for i in range(2)]
        self.st_free=[None,None]; self.bf_free=[None,None]; self.evs=[]
        self.jobs=[(e,mi,half) for e in range(32) for mi in range(3) for half in range(2)]
    def step(self):
        if self.n>=len(self.jobs): return False
        k=self.k; nc=k.nc; d=self.d
        e,mi,half=self.jobs[self.n]; s=self.n%2; self.n+=1
        src=(d['w_gate'],d['w_up'],d['w_down'])[mi]; dst=(d['wg_bf'],d['wu_bf'],d['wd_bf'])[mi]
        C_=src.shape[1]//128; F=src.shape[2]; c0=half*C_//2; c1=c0+C_//2
        ev=k.dma(k.sp,self.ld[s],self.st[s][:].rearrange("p (c f) -> p c f",c=C_//2),src[e,c0*128:c1*128,:].rearrange("(c p) f -> p c f",p=128),deps=[self.st_free[s]])
        cev=[]
        for (q,eng,lo,hi) in ((k.dve,nc.vector,0,3072),(k.pool,nc.gpsimd,3072,4096)):
            q.wait(ev,self.bf_free[s])
            cev.append(q.sig(eng.tensor_copy(out=self.bf[s][:,lo:hi],in_=self.st[s][:,lo:hi])))
        self.st_free[s]=cev
        self.bf_free[s]=k.dma(k.sp,self.so[s],dst[e*128:(e+1)*128,c0*F:c0*F+4096],self.bf[s][:],deps=cev); self.evs.append(self.bf_free[s])
        return True

DEBUG_STATIC_W=False
def phase_d(k, C, d, NBLK, deps=()):
    nc=k.nc; pe=k.pe; act=k.act; dve=k.dve; pool=k.pool; sp=k.sp
    ident=C['ident_bf']
    wg=[k.sb([128,16*512],BF16,f"wg{i}") for i in range(2)]; wu=[k.sb([128,16*512],BF16,f"wu{i}") for i in range(2)]
    wd=[k.sb([128,4*2048],BF16,f"wd{i}") for i in range(2)]
    xs=[k.sb([128,2048],BF16,f"xs{i}") for i in range(2)]; xsT=k.sb([128,16,128],BF16,"xsT")
    sg=k.sb([128,512],F32,"sg"); hb=k.sb([128,512],BF16,"hb"); hT=k.sb([128,4,128],BF16,"hT")
    ysb=[k.sb([128,2048],F32,f"ysb{i}") for i in range(2)]
    idxi=k.sb([128,NBLK],I32,"idxi"); befl=k.sb([128,NBLK],F32,"befl")
    tp=[k.ps([128,8,128],BF16,f"dtp{i}") for i in range(2)]
    pg=k.ps([128,512],F32,"pg"); pu=k.ps([128,512],F32,"pu"); pd=[k.ps([128,512],F32,f"pd{i}") for i in range(4)]
    wl=[k.dsem(f"d_wl{i}") for i in range(2)]; xl=[k.dsem(f"d_xl{i}") for i in range(2)]; ys=[k.dsem(f"d_ys{i}") for i in range(2)]
    dve.wait(*deps)
    e=dve.sig(nc.vector.tensor_copy(out=befl[:],in_=C['blk_e'][:])); dve.wait(e)
    e=dve.sig(nc.vector.tensor_scalar(out=befl[:],in0=befl[:],scalar1=128.0,scalar2=None,op0=ALU.mult)); dve.wait(e)
    e=dve.sig(nc.vector.tensor_scalar(out=befl[:],in0=befl[:],scalar1=C['iota_p'][:,0:1],scalar2=None,op0=ALU.add)); dve.wait(e)
    e_idx=dve.sig(nc.vector.tensor_copy(out=idxi[:],in_=befl[:]))
    wgv=d['wg_bf'][:,:]; wuv=d['wu_bf'][:,:]; wdv=d['wd_bf'][:,:]
    w_free=[None,None]; xs_free=[None,None]; y_free=[None,None]
    wev=[None]*NBLK; xev=[None]*NBLK
    def issue_loads(b):
        s=b%2
        pool.wait(e_idx, w_free[s], *deps)
        if DEBUG_STATIC_W:
            for (dst,src) in ((wg[s],d['wg_bf']),(wu[s],d['wu_bf']),(wd[s],d['wd_bf'])):
                wev[b]=k.dma(sp,wl[s],dst[:],src[(b%32)*128:(b%32+1)*128,:],deps=[w_free[s]]+list(deps))
            xev[b]=k.dma(sp,xl[s],xs[s][:],d['xs'][b*128:(b+1)*128,:],deps=[xs_free[s]]+list(deps))
            return
        for (dst,src) in ((wg[s],wgv),(wu[s],wuv),(wd[s],wdv)):
            ins=nc.gpsimd.indirect_dma_start(out=dst[:],out_offset=None,in_=src,in_offset=bass.IndirectOffsetOnAxis(ap=idxi[:,b:b+1],axis=0))
            ins.then_inc(wl[s].h,16); wl[s].cnt+=16
        wev[b]=(wl[s],wl[s].cnt)
        xev[b]=k.dma(sp,xl[s],xs[s][:],d['xs'][b*128:(b+1)*128,:],deps=[xs_free[s]]+list(deps))
    issue_loads(0)
    out_evs=[]
    xsT_free=None; hT_free=None; sg_free=None; hb_free=None; pd_free=None; tp_free=[None,None]; pgu_free=None
    for b in range(NBLK):
        s=b%2
        if b+1<NBLK: issue_loads(b+1)
        pe.wait(xev[b])
        tev=[]
        for half in range(2):
            pe.wait(tp_free[half])
            for c8 in range(8):
                c=half*8+c8
                ins=nc.tensor.transpose(tp[half][:,c8,:],xs[s][:,c*128:(c+1)*128],ident[:])
            tev.append(pe.sig(ins))
        xs_free[s]=tev[1]
        dve.wait(tev[0], xsT_free); e0=dve.sig(nc.vector.tensor_copy(out=xsT[:,0:8,:],in_=tp[0][:]))
        act.wait(tev[1], xsT_free); e1=act.sig(nc.scalar.copy(out=xsT[:,8:16,:],in_=tp[1][:]))
        tp_free=[e0,e1]
        pe.wait(e0,e1,wev[b],pgu_free)
        for c in range(16):
            nc.tensor.matmul(pg[:],lhsT=xsT[:,c,:],rhs=wg[s][:,c*512:(c+1)*512],start=(c==0),stop=(c==15))
        e_g=pe.sig(nc.tensor.matmul(pg[:],lhsT=xsT[:,0,:],rhs=wg[s][:,0:512],start=False,stop=True,skip_group_check=True)) if False else None
        for c in range(16):
            ins=nc.tensor.matmul(pu[:],lhsT=xsT[:,c,:],rhs=wu[s][:,c*512:(c+1)*512],start=(c==0),stop=(c==15))
        e_u=pe.sig(ins); xsT_free=e_u
        act.wait(e_u, sg_free)
        e_s=act.sig(nc.scalar.activation(out=sg[:],in_=pg[:],func=AF.Silu))
        dve.wait(e_s, hb_free)
        e_h=dve.sig(nc.vector.tensor_tensor(out=hb[:],in0=sg[:],in1=pu[:],op=ALU.mult))
        sg_free=e_h; pgu_free=e_h
        pe.wait(e_h, tp_free[0])
        for c in range(4):
            ins=nc.tensor.transpose(tp[0][:,c,:],hb[:,c*128:(c+1)*128],ident[:])
        e_t=pe.sig(ins); hb_free=e_t
        dve.wait(e_t, hT_free)
        e_c=dve.sig(nc.vector.tensor_copy(out=hT[:],in_=tp[0][:,0:4,:]))
        tp_free[0]=e_c
        pe.wait(e_c, pd_free)
        dev=[]
        for n in range(4):
            for c in range(4):
                ins=nc.tensor.matmul(pd[n][:],lhsT=hT[:,c,:],rhs=wd[s][:,c*2048+n*512:c*2048+(n+1)*512],start=(c==0),stop=(c==3))
            dev.append(pe.sig(ins))
        hT_free=dev[3]; w_free[s]=dev[3]
        evs_=[]
        for n in range(4):
            q=dve if n%2==0 else act
            q.wait(dev[n], y_free[s])
            if n%2==0: e=dve.sig(nc.vector.tensor_copy(out=ysb[s][:,n*512:(n+1)*512],in_=pd[n][:]))
            else: e=act.sig(nc.scalar.copy(out=ysb[s][:,n*512:(n+1)*512],in_=pd[n][:]))
            evs_.append(e)
        pd_free=evs_
        y_free[s]=k.dma(sp,ys[s],d['Y'][b*128:(b+1)*128,:],ysb[s][:],deps=evs_)
        out_evs.append(y_free[s])
    return out_evs

def phase_e(k, C, d, NQT, deps=()):
    nc=k.nc; dve=k.dve; pool=k.pool; sp=k.sp
    h1=[k.sb([128,2048],F32,f"eh1_{i}") for i in range(2)]; y0=[k.sb([128,2048],F32,f"ey0_{i}") for i in range(2)]; y1=[k.sb([128,2048],F32,f"ey1_{i}") for i in range(2)]
    ob=[k.sb([128,2048],F32,f"eo_{i}") for i in range(2)]
    ld=[k.dsem(f"e_ld{i}") for i in range(2)]; gl=[k.dsem(f"e_gl{i}") for i in range(2)]; st=[k.dsem(f"e_st{i}") for i in range(2)]
    in_free=[None,None]; o_free=[None,None]; out_evs=[]
    gates=C['gates']
    for i in range(NQT):
        s=i%2
        ev_h=k.dma(sp,ld[s],h1[s][:],d['h1'][i*128:(i+1)*128,:],deps=[in_free[s]]+list(deps))
        pool.wait(in_free[s], *deps)
        for kk,dst in enumerate((y0[s],y1[s])):
            ins=nc.gpsimd.indirect_dma_start(out=dst[:],out_offset=None,in_=d['Y'][:,:],in_offset=bass.IndirectOffsetOnAxis(ap=C['pos_i'][:,i*2+kk:i*2+kk+1],axis=0))
            ins.then_inc(gl[s].h,16); gl[s].cnt+=16
        ev_g=(gl[s],gl[s].cnt)
        pool.wait(ev_g)
        dve.wait(ev_h,ev_g,o_free[s])
        e=dve.sig(nc.vector.scalar_tensor_tensor(out=ob[s][:],in0=y0[s][:],scalar=gates[:,i,0:1],in1=h1[s][:],op0=ALU.mult,op1=ALU.add)); dve.wait(e)
        e=dve.sig(nc.vector.scalar_tensor_tensor(out=ob[s][:],in0=y1[s][:],scalar=gates[:,i,1:2],in1=ob[s][:],op0=ALU.mult,op1=ALU.add))
        in_free[s]=e
        o_free[s]=k.dma(sp,st[s],d['out'][i*128:(i+1)*128,:],ob[s][:],deps=[e]); out_evs.append(o_free[s])
    return out_evs


def build(NKVT, NQT, PART=16):
    NQ=NQT*128; NK=NKVT*128+PART; NBLK=2*NQT+32
    nc=bass.Bass("TRN2", target_bir_lowering=False)
    def din(name,shape,dt=F32): return nc.dram_tensor(name,list(shape),dt,kind="ExternalInput").ap()
    def scr(name,shape,dt): return nc.dram_tensor(name,list(shape),dt).ap()
    d={}
    d['xkv']=din('xkv',[NKVT*128,2048]); d['xq']=din('xq',[NQ,2048]); d['xh']=din('xh',[128,2048]); d['meta']=din('meta',[128,2048])
    d['g1t']=din('g1t',[128,16]); d['gqr']=din('gqr',[128,128]); d['gkr']=din('gkr',[128,128])
    d['pool_w']=din('pool_w',[128,8,256]); d['pool_sc']=din('pool_sc',[128,8]); d['Aband']=din('Aband',[128,24,128],BF16)
    d['ident_bf']=din('ident_bf',[128,128],BF16); d['ident_f']=din('ident_f',[128,128]); d['ones_bf']=din('ones_bf',[128,128],BF16)
    d['w_in']=din('w_in',[2048,2560])
    d['ckv']=din('ckv',[(NKVT+1)*128,128]); d['skv']=din('skv',[(NKVT+1)*128,128]); d['cq']=din('cq',[NQ,128]); d['sq']=din('sq',[NQ,128])
    d['w_out']=din('w_out',[2048,2048]); d['g2r']=din('g2r',[128,2048]); d['w_r']=din('w_r',[128,16,36]); d['b_r']=din('b_r',[128,36])
    d['Utri']=din('Utri',[128,128],BF16); d['thr']=din('thr',[128,32]); d['blkst']=din('blkst',[128,NBLK]); d['iota_p']=din('iota_p',[128,1])
    d['w_gate']=din('w_gate',[32,2048,512]); d['w_up']=din('w_up',[32,2048,512]); d['w_down']=din('w_down',[32,512,2048])
    d['out']=nc.dram_tensor('out',[NQ,2048],F32,kind="ExternalOutput").ap()
    d['KT']=scr('KT',[2,128,NK],BF16); d['V']=scr('V',[NKVT+1,128,256],BF16); d['QT']=scr('QT',[8,128,NQ],BF16); d['mixT']=scr('mixT',[16,128,NQ],BF16)
    d['h1']=scr('h1',[NQ,2048],F32); d['b16']=scr('b16',[NQ,2048],BF16); d['xs']=scr('xs',[NBLK*128,2048],BF16); d['Y']=scr('Y',[NBLK*128,2048],F32)
    d['wg_bf']=scr('wg_bf',[4096,8192],BF16); d['wu_bf']=scr('wu_bf',[4096,8192],BF16); d['wd_bf']=scr('wd_bf',[4096,8192],BF16)
    with ExitStack() as es:
        k=K(nc,es); C={}
        C['ident_bf']=k.sb([128,128],BF16,"c_ident_bf"); C['ident_f']=k.sb([128,128],F32,"c_ident_f"); C['ones_bf']=k.sb([128,128],BF16,"c_ones_bf")
        C['iota_p']=k.sb([128,1],F32,"c_iota_p"); C['nbias']=k.sb([128,1],F32,"c_nbias")
        C['LGT']=k.sb([128,NQT,36],F32,"c_LGT"); C['pos_i']=k.sb([128,2*NQT],I32,"c_pos_i"); C['gates']=k.sb([128,NQT,2],F32,"c_gates"); C['blk_e']=k.sb([128,NBLK],I32,"c_blk_e")
        gqa=k.sb([128,128],F32,"c_gqa"); gka=k.sb([128,128],F32,"c_gka"); mq=k.sb([128,1],F32,"c_mq"); mk=k.sb([128,1],F32,"c_mk")
        s0=k.dsem("c0")
        k.dma(k.sp,s0,C['ones_bf'][:],d['ones_bf'][:,:]); k.dma(k.sp,s0,C['iota_p'][:],d['iota_p'][:,:])
        k.dma(k.sp,s0,gqa[:],d['gqr'][:,:]); ev=k.dma(k.sp,s0,gka[:],d['gkr'][:,:])
        dve=k.dve; V=nc.vector
        dve.wait(ev)
        def op(ins):
            e=dve.sig(ins); dve.wait(e); return e
        m2=k.sb([128,2],F32,"c_m2")
        for (ga,mm,col) in ((gqa,mq,0),(gka,mk,1)):
            op(V.tensor_reduce(out=mm[:],in_=ga[:],axis=AX.X,op=ALU.max))
            op(V.tensor_scalar(out=ga[:],in0=ga[:],scalar1=-1.0,scalar2=None,op0=ALU.mult))
            op(V.tensor_reduce(out=m2[:,col:col+1],in_=ga[:],axis=AX.X,op=ALU.max))
            op(V.tensor_tensor(out=mm[:],in0=mm[:],in1=m2[:,col:col+1],op=ALU.max))
        op(V.tensor_tensor(out=mq[:],in0=mq[:],in1=mk[:],op=ALU.mult))
        e_nb=op(V.tensor_scalar(out=C['nbias'][:],in0=mq[:],scalar1=-(128.0**0.5),scalar2=None,op0=ALU.mult))
        k.begin_phase(); evs=phase_a(k,C,d,NKVT,NQT); k.end_phase(evs)
        k.begin_phase(); wc=WConv(k,d)
        evs=attention_phase(k,d['QT'],d['KT'],d['V'],d['mixT'],C['nbias'],C['ones_bf'],NQT,NKVT,PART,128.0**-0.5,deps=[e_nb,(s0,s0.cnt)],bg=wc.step,bg_every=max(1,(2*NQT*(NKVT+1))//200))
        k.end_phase(evs+wc.evs)
        k.begin_phase(); evs=phase_c(k,C,d,NQT,NBLK); k.end_phase(evs)
        k.begin_phase(); evs=phase_c2(k,C,d,NQT,NBLK); k.end_phase(evs)
        k.begin_phase(); evs=phase_d(k,C,d,NBLK); k.end_phase(evs)
        k.begin_phase(); evs=phase_e(k,C,d,NQT); k.end_phase(evs)
    return nc

def const_inputs(j, NKVT, NQT, r0, NBLK):
    BF_=BF
    c={}
    r=np.arange(NKVT*128)
    Ck,Sk=rope_tables((r//GRID_W).astype(np.float32),(r%GRID_W).astype(np.float32))
    Cm,Sm=rope_tables(np.zeros(128,np.float32),np.zeros(128,np.float32))
    c['ckv']=np.concatenate([Ck,Cm]); c['skv']=np.concatenate([Sk,Sm])
    c['cq']=np.ascontiguousarray(Ck[r0:r0+NQT*128]); c['sq']=np.ascontiguousarray(Sk[r0:r0+NQT*128])
    c['Aband']=band_mats(j,NQT)
    c['ident_bf']=np.eye(128).astype(BF_); c['ident_f']=np.eye(128,dtype=np.float32); c['ones_bf']=np.ones((128,128),BF_)
    c['Utri']=np.triu(np.ones((128,128),np.float32),1).astype(BF_)
    c['thr']=np.tile((128*np.arange(32,dtype=np.float32))[None],(128,1)); c['blkst']=np.tile((128*np.arange(NBLK,dtype=np.float32))[None],(128,1))
    c['iota_p']=np.arange(128,dtype=np.float32).reshape(128,1)
    return c

def layout_params(p):
    f=lambda a: np.ascontiguousarray(np.asarray(a,dtype=np.float32))
    o={}
    o['g1t']=f(np.asarray(p['norm1_g'])[0].reshape(16,128).T)
    o['gqr']=f(np.tile(np.asarray(p['q_norm_g'])[0][None],(128,1))); o['gkr']=f(np.tile(np.asarray(p['k_norm_g'])[0][None],(128,1)))
    o['pool_w']=f(np.asarray(p['pool_w'])[0].reshape(4,2,128,256).transpose(2,0,1,3).reshape(128,8,256))
    o['pool_sc']=f(np.asarray(p['pool_scale'])[0].reshape(8,128).T)
    o['w_in']=f(np.asarray(p['w_in'])[0]); o['w_out']=f(np.asarray(p['w_out'])[0])
    o['g2r']=f(np.tile(np.asarray(p['norm2_g'])[0][None],(128,1)))
    wr=np.concatenate([np.asarray(p['w_router_group'])[0],np.asarray(p['w_router_expert'])[0]],1)
    o['w_r']=f(wr.reshape(16,128,36).transpose(1,0,2))
    o['b_r']=f(np.tile(np.concatenate([np.asarray(p['b_router_group'])[0],np.asarray(p['b_router_expert'])[0]])[None],(128,1)))
    o['w_gate']=f(np.asarray(p['w_gate'])[0]); o['w_up']=f(np.asarray(p['w_up'])[0]); o['w_down']=f(np.asarray(p['w_down'])[0])
    return o

def kernel(**inputs):
    x=np.asarray(inputs['x'],dtype=np.float32); meta=np.asarray(inputs['meta_tokens'],dtype=np.float32)
    B,S,D=x.shape
    NKVT=S//128; NQT=NKVT//4; NBLK=2*NQT+32
    P=layout_params(inputs)
    mp=np.zeros((128,D),np.float32); mp[:N_META]=meta
    nc=build(NKVT,NQT)
    in_maps=[]
    for c in range(8):
        b=c//4; j=c%4; r0=j*NQT*128; r1=r0+NQT*128
        m=dict(P); m.update(const_inputs(j,NKVT,NQT,r0,NBLK))
        m['xkv']=np.ascontiguousarray(x[b]); m['xq']=np.ascontiguousarray(x[b,r0:r1]); m['meta']=mp
        xh=np.zeros((128,D),np.float32)
        xh[0:8]=meta[8:16] if j==0 else x[b,r0-8:r0]
        if j<3: xh[8:16]=x[b,r1:r1+8]
        m['xh']=xh
        in_maps.append(m)
    res=run_bass_kernel_spmd(nc,in_maps,core_ids=list(range(8)))
    out=np.zeros((B,S,D),np.float32)
    for c in range(8):
        b=c//4; j=c%4; r0=j*NQT*128
        out[b,r0:r0+NQT*128]=np.asarray(res.results[c]['out'],dtype=np.float32)
    return out
```

```python
from contextlib import ExitStack
import ml_dtypes
from concourse.bass_utils import run_bass_kernel_spmd
import numpy as np
import concourse.bass as bass
import concourse.mybir as mybir
F32=mybir.dt.float32; BF16=mybir.dt.bfloat16; I32=mybir.dt.int32
AF=mybir.ActivationFunctionType; ALU=mybir.AluOpType; AX=mybir.AxisListType

class EngQ:
    def __init__(self, nc, es, name, e):
        self.name=name; self.e=e; self.h=es.enter_context(nc.semaphore("tl_"+name)); self.cnt=0; self.seen={}; self.uid="tl_"+name
    def wait(self, *evs):
        for ev in evs:
            if ev is None: continue
            if isinstance(ev, (list,tuple)) and len(ev)>0 and not hasattr(ev[0],'h'):
                self.wait(*ev); continue
            src,val=ev
            if src is self and False: pass
            if self.seen.get(src.uid,0)>=val: continue
            self.e.wait_ge(src.h,val); self.seen[src.uid]=val
    def sig(self, ins):
        self.cnt+=1; ins.then_inc(self.h,1); return (self,self.cnt)

class DSem:
    def __init__(self, nc, es, name):
        self.h=es.enter_context(nc.semaphore(name)); self.cnt=0; self.uid=name

class K:
    def __init__(self, nc, es):
        self.nc=nc; self.es=es
        self.pe=EngQ(nc,es,"pe",nc.tensor); self.dve=EngQ(nc,es,"dve",nc.vector)
        self.act=EngQ(nc,es,"act",nc.scalar); self.pool=EngQ(nc,es,"pool",nc.gpsimd); self.sp=EngQ(nc,es,"sp",nc.sync)
        self.engs=[self.pe,self.dve,self.act,self.pool,self.sp]
        self._n=0; self.pes=es
    def sb(self, shape, dt, name=None):
        self._n+=1; return self.pes.enter_context(self.nc.sbuf_tensor("s_"+(name or f"sb{self._n}"), shape, dt))
    def ps(self, shape, dt, name=None):
        self._n+=1; return self.pes.enter_context(self.nc.psum_tensor("p_"+(name or f"ps{self._n}"), shape, dt))
    def dsem(self, name=None):
        self._n+=1; return DSem(self.nc,self.es,name or f"ds{self._n}")
    def dma(self, q, sem, out, in_, deps=(), **kw):
        q.wait(*deps)
        ins=q.e.dma_start(out=out,in_=in_,**kw); ins.then_inc(sem.h,16); sem.cnt+=16
        return (sem,sem.cnt)
    def barrier(self, extra=()):
        last=[(q,q.cnt) for q in self.engs if q.cnt>0]
        for q in self.engs:
            for ev in last:
                q.wait(ev)
            q.wait(*extra)
    def begin_phase(self):
        from contextlib import ExitStack
        self.pes=ExitStack(); self.pes.__enter__()
    def end_phase(self, extra=()):
        self.barrier(extra)
        self.pes.__exit__(None,None,None); self.pes=self.es

BF=ml_dtypes.bfloat16
N_META=16; GRID_W=64; L=16400; S=16384
def rope_tables(pos_row, pos_col):
    inv=np.power(10000.0,-np.arange(0,64,2,dtype=np.float32)/64).astype(np.float32)
    ar=pos_row[:,None].astype(np.float32)*inv[None,:]; ac=pos_col[:,None].astype(np.float32)*inv[None,:]
    cr,sr,cc,sc=np.cos(ar),np.sin(ar),np.cos(ac),np.sin(ac)
    C=np.concatenate([cr,cr,cc,cc],1).astype(np.float32); Sg=np.concatenate([-sr,sr,-sc,sc],1).astype(np.float32)
    return C,Sg
def band_mats(j, NQT):
    A=np.zeros((6,4,128,128),np.float32)
    wins=(2,4,8,16)
    for g,w in enumerate(wins):
        for out in range(128):
            lo=out-w//2; hi=out+w//2-1
            for off in range(lo,hi+1):
                if 0<=off<128: A[0,g,off,out]+=1.0/w
                elif off<0:
                    A[1,g,128+off,out]+=1.0/w
                    A[3,g,8+off,out]+=1.0/w
                else:
                    A[2,g,off-128,out]+=1.0/w
                    A[4,g,8+(off-128),out]+=1.0/w
            A[0,g,out,out]-=1.0
        A[5,g]=A[0,g]
        if j==3:
            for out in range(128):
                t=16+S-128+out
                lo_t=max(t-w//2,0); hi_t=min(t-w//2+w,L)
                cnt=hi_t-lo_t
                if cnt!=w:
                    A[5,g,:,out]=0
                    for tt in range(lo_t,hi_t):
                        off=tt-(16+S-128)
                        if 0<=off<128: A[5,g,off,out]+=1.0/cnt
                    A[5,g,out,out]-=1.0
    return np.ascontiguousarray(A.reshape(24,128,128).transpose(1,0,2)).astype(BF)


def prep_consts(k, ident_src=None):
    nc=k.nc
    c={}
    c['ones_bf']=k.sb([128,128],BF16,"c_ones_bf")
    c['ident_bf']=k.sb([128,128],BF16,"c_ident_bf")
    c['ident_f']=k.sb([128,128],F32,"c_ident_f")
    return c

def phase_a(k, C, d, NKVT, NQT):
    nc=k.nc; pe=k.pe; act=k.act; dve=k.dve; pool=k.pool; sp=k.sp
    EPS=1e-6
    wsb=k.sb([128,16,2560],BF16,"w_in_bf")
    wst=[k.sb([128,2560],F32,f"wst{i}") for i in range(2)]
    g1=k.sb([128,16],F32,"g1t");
    gqr=k.sb([128,128],F32,"gqr"); gkr=k.sb([128,128],F32,"gkr")
    pw=k.sb([128,8,256],BF16,"pool_w_bf"); pwst=k.sb([128,8,256],F32,"pool_w_st")
    psc=k.sb([128,8],F32,"pool_sc")
    Aband=k.sb([128,24,128],BF16,"Aband")
    cs=k.dsem("a_const"); wl=[k.dsem(f"a_wl{i}") for i in range(2)]
    k.dma(sp,cs,g1[:],d['g1t'][:,:]); k.dma(sp,cs,gqr[:],d['gqr'][:,:]); k.dma(sp,cs,gkr[:],d['gkr'][:,:])
    k.dma(sp,cs,pwst[:],d['pool_w'][:,:,:]); k.dma(sp,cs,psc[:],d['pool_sc'][:,:]);
    ev_c=k.dma(sp,cs,Aband[:],d['Aband'][:,:,:])
    ident=C['ident_bf']
    epsb=k.sb([128,1],F32,"epsb"); e_eps=dve.sig(nc.vector.memset(epsb[:],EPS)); act.wait(e_eps)
    k.dma(sp,cs,ident[:],d['ident_bf'][:,:]); ev_c=k.dma(sp,cs,C['ident_f'][:],d['ident_f'][:,:])
    dve.wait(ev_c)
    ev_pw=dve.sig(nc.vector.tensor_copy(out=pw[:],in_=pwst[:]))
    wev=[None,None]; cev=[None,None]; w_done=None
    for c in range(16):
        s=c%2
        wev[s]=k.dma(sp,wl[s],wst[s][:],d['w_in'][c*128:(c+1)*128,:],deps=[cev[s]])
        dve.wait(wev[s])
        cev[s]=dve.sig(nc.vector.tensor_scalar(out=wsb[:,c,:],in0=wst[s][:],scalar1=g1[:,c:c+1],scalar2=None,op0=ALU.mult))
    w_done=cev
    xt=[k.sb([128,2048],F32,f"xt{i}") for i in range(2)]
    xb=[k.sb([128,2048],BF16,f"xb{i}") for i in range(2)]
    xT=[k.sb([128,16,128],BF16,f"xT{i}") for i in range(2)]
    junk=k.sb([128,2048],BF16,"junk")
    ssq=[k.sb([128,1],F32,f"ssq{i}") for i in range(2)]
    rstd=[k.sb([128,1],F32,f"rstd{i}") for i in range(2)]
    ct=[k.sb([128,128],F32,f"ct{i}") for i in range(2)]; stt=[k.sb([128,128],F32,f"stt{i}") for i in range(2)]
    xl=[k.dsem(f"a_xl{i}") for i in range(2)]
    tp_ps=[k.ps([128,8,128],BF16,f"tp_ps{i}") for i in range(2)]
    pj_ps=[k.ps([128,512],F32,f"pj_ps{i}") for i in range(4)]
    qs=k.sb([128,1024],F32,"qs"); sq=k.sb([128,1024],F32,"sq"); t1=k.sb([128,1024],F32,"t1"); t2=k.sb([128,1024],F32,"t2")
    hs=k.sb([128,8],F32,"hs"); hr=k.sb([128,8],F32,"hr")
    qb=k.sb([128,1024],BF16,"qbf"); qT_sb=[k.sb([128,8,128],BF16,f"qT_sb{i}") for i in range(2)]
    v_sb=[k.sb([128,256],BF16,f"v_sbA{i}") for i in range(2)]
    pring=[k.sb([128,1024],BF16,f"pring{i}") for i in range(4)]
    mT=k.sb([128,8,128],BF16,"mT"); yT=[k.sb([128,8,128],BF16,f"yT{i}") for i in range(2)]
    st_q=[k.dsem(f"a_stq{i}") for i in range(2)]; st_v=[k.dsem(f"a_stv{i}") for i in range(2)]; st_y=[k.dsem(f"a_sty{i}") for i in range(2)]
    st_ev_q=[None,None]; st_ev_v=[None,None]; st_ev_y=[None,None]
    out_evs=[]
    state={'back_done':[None,None],'n':0,'free_x':[None,None],'free_xT':[None,None],'tp_free':[None,None],'pj_free':[None]*4,'pjn':0, 'work':None,'qT_n':0,'v_n':0}

    def norm_rope(src_ps_list, H, rst, gr, ctile, stile, dest_bf, extra_wait=()):
        W=H*128
        evs=[]
        for bi,(pst,ev) in enumerate(src_ps_list):
            act.wait(ev, state['work'])
            w=min(512,W-bi*512)
            evs.append(act.sig(nc.scalar.activation(out=qs[:,bi*512:bi*512+w],in_=pst[:,0:w],func=AF.Copy,scale=rst[:,0:1])))
        dve.wait(*evs); dve.wait(*extra_wait)
        e=dve.sig(nc.vector.tensor_tensor(out=sq[:,0:W],in0=qs[:,0:W],in1=qs[:,0:W],op=ALU.mult)); dve.wait(e)
        e=dve.sig(nc.vector.tensor_reduce(out=hs[:,0:H],in_=sq[:,0:W].rearrange("p (h d) -> p h d",h=H),axis=AX.X,op=ALU.add)); dve.wait(e)
        act.wait(e)
        e=act.sig(nc.scalar.activation(out=hr[:,0:H],in_=hs[:,0:H],func=AF.Sqrt,scale=1.0/128,bias=epsb[:,0:1])); dve.wait(e)
        e=dve.sig(nc.vector.reciprocal(out=hr[:,0:H],in_=hr[:,0:H])); dve.wait(e)
        q3=qs[:,0:W].rearrange("p (h d) -> p h d",h=H)
        e=dve.sig(nc.vector.tensor_tensor(out=sq[:,0:W].rearrange("p (h d) -> p h d",h=H),in0=q3,in1=hr[:,0:H].unsqueeze(2).to_broadcast([128,H,128]),op=ALU.mult)); dve.wait(e)
        s3=sq[:,0:W].rearrange("p (h d) -> p h d",h=H)
        e=dve.sig(nc.vector.tensor_tensor(out=q3,in0=s3,in1=gr[:].unsqueeze(1).to_broadcast([128,H,128]),op=ALU.mult)); dve.wait(e)
        e1=dve.sig(nc.vector.tensor_tensor(out=t1[:,0:W].rearrange("p (h d) -> p h d",h=H),in0=q3,in1=ctile[:].unsqueeze(1).to_broadcast([128,H,128]),op=ALU.mult))
        q5=qs[:,0:W].rearrange("p (h b f e) -> p h b f e",h=H,b=2,f=2)
        t5=t2[:,0:W].rearrange("p (h b f e) -> p h b f e",h=H,b=2,f=2)
        s5=stile[:].rearrange("p (b f e) -> p b f e",b=2,f=2)
        for f in range(2):
            for b in range(2):
                e2=dve.sig(nc.vector.tensor_tensor(out=t5[:,:,b,f,:],in0=q5[:,:,b,1-f,:],in1=s5[:,b,f,:].unsqueeze(1).to_broadcast([128,H,32]),op=ALU.mult))
        dve.wait(e1,e2)
        e=dve.sig(nc.vector.tensor_tensor(out=dest_bf[:,0:W],in0=t1[:,0:W],in1=t2[:,0:W],op=ALU.add))
        state['work']=e
        return e

    def tile(src_ap, rows, ctab, stab, tok0, do_kv=None, do_q=None, do_pool=None):
        n=state['n']; s=n%2; state['n']+=1
        dd=[state['free_x'][s], state['back_done'][s]]
        if rows<128:
            dve.wait(state['free_x'][s]); z=dve.sig(nc.vector.memset(xt[s][:],0.0)); dd=[z]
        evx=k.dma(sp,xl[s],xt[s][0:rows,:],src_ap,deps=dd)
        if ctab is not None:
            k.dma(sp,xl[s],ct[s][0:rows,:],ctab[tok0:tok0+rows,:],deps=dd)
            evx=k.dma(sp,xl[s],stt[s][0:rows,:],stab[tok0:tok0+rows,:],deps=dd)
        act.wait(evx)
        e_ss=act.sig(nc.scalar.activation(out=junk[:],in_=xt[s][:],func=AF.Square,accum_out=ssq[s][:]))
        pool.wait(evx, state['free_xT'][s])
        e_xb0=pool.sig(nc.gpsimd.tensor_copy(out=xb[s][:,0:768],in_=xt[s][:,0:768]))
        dve.wait(evx, state['free_xT'][s])
        e_xb1=dve.sig(nc.vector.tensor_copy(out=xb[s][:,768:2048],in_=xt[s][:,768:2048]))
        e_xb=[e_xb0,e_xb1]
        act.wait(e_ss)
        act.wait(state['back_done'][s])
        e=act.sig(nc.scalar.activation(out=rstd[s][:],in_=ssq[s][:],func=AF.Sqrt,scale=1.0/2048,bias=epsb[:,0:1])); dve.wait(e, state['back_done'][s])
        e_rs=dve.sig(nc.vector.reciprocal(out=rstd[s][:],in_=rstd[s][:]))
        pe.wait(e_xb)
        tev=[]
        for half in range(2):
            pe.wait(state['tp_free'][half])
            for c8 in range(8):
                c=half*8+c8
                ins=nc.tensor.transpose(tp_ps[half][:,c8,:],xb[s][:,c*128:(c+1)*128],ident[:])
            tev.append(pe.sig(ins))
        state['free_x'][s]=None
        dve.wait(tev[0], state['free_xT'][s]);
        e0=dve.sig(nc.vector.tensor_copy(out=xT[s][:,0:8,:],in_=tp_ps[0][:]))
        act.wait(tev[1], state['free_xT'][s])
        e1=act.sig(nc.scalar.copy(out=xT[s][:,8:16,:],in_=tp_ps[1][:]))
        state['tp_free']=[e0,e1]
        xT_ready=[e0,e1]
        def proj(col0, ncols):
            res=[]
            for b0 in range(0,ncols,512):
                w=min(512,ncols-b0); j=state['pjn']%4; state['pjn']+=1
                pe.wait(xT_ready, w_done, state['pj_free'][j])
                for c in range(16):
                    ins=nc.tensor.matmul(pj_ps[j][:,0:w],lhsT=xT[s][:,c,:],rhs=wsb[:,c,col0+b0:col0+b0+w],start=(c==0),stop=(c==15))
                res.append((pj_ps[j],pe.sig(ins),j))
            return res
        last_pe=None
        if do_kv is not None:
            KT,V,tidx=do_kv
            r=proj(1024,512)[0]; pst,ev,j=r
            state['free_x'][s]=[e_ss]+e_xb; state['free_xT'][s]=ev
            def back():
                vs=state['v_n']%2; state['v_n']+=1
                act.wait(ev, e_rs, st_ev_v[vs])
                e_v=act.sig(nc.scalar.activation(out=v_sb[vs][:],in_=pst[:,256:512],func=AF.Copy,scale=rstd[s][:,0:1]))
                st_ev_v[vs]=k.dma(sp,st_v[vs],V[tidx,0:rows,:],v_sb[vs][0:rows,:],deps=[e_v]); out_evs.append(st_ev_v[vs])
                e_k=norm_rope([(pst,ev)],2,rstd[s],gkr,ct[s],stt[s],qb,extra_wait=[e_rs,state.get('qb_free')])
                state['pj_free'][j]=[e_k,e_v]
                qs_=state['qT_n']%2; state['qT_n']+=1
                pe.wait(e_k, state['tp_free'][0])
                for h in range(2):
                    ins=nc.tensor.transpose(tp_ps[0][:,h,:],qb[:,h*128:(h+1)*128],ident[:])
                e_t=pe.sig(ins)
                dve.wait(e_t, st_ev_q[qs_])
                e_c=dve.sig(nc.vector.tensor_copy(out=qT_sb[qs_][:,0:2,:],in_=tp_ps[0][:,0:2,:]))
                state['tp_free'][0]=e_c
                st_ev_q[qs_]=k.dma(sp,st_q[qs_],KT[:,:,tok0:tok0+rows].rearrange("h d t -> d h t"),qT_sb[qs_][:,0:2,0:rows],deps=[e_c]); out_evs.append(st_ev_q[qs_])
                state['qb_free']=e_t
                state['back_done'][s]=[e_k,e_v]
            return back
        if do_q is not None:
            QT=do_q
            r=proj(0,1024)
            e_q=norm_rope([(r[0][0],r[0][1]),(r[1][0],r[1][1])],8,rstd[s],gqr,ct[s],stt[s],qb,extra_wait=[e_rs,state.get('qb_free')])
            state['pj_free'][r[0][2]]=e_q; state['pj_free'][r[1][2]]=e_q
            qs_=state['qT_n']%2; state['qT_n']+=1
            pe.wait(e_q, state['tp_free'][0])
            for h in range(8):
                ins=nc.tensor.transpose(tp_ps[0][:,h,:],qb[:,h*128:(h+1)*128],ident[:])
            e_t=pe.sig(ins); state['qb_free']=e_t
            dve.wait(e_t, st_ev_q[qs_])
            e_c=dve.sig(nc.vector.tensor_copy(out=qT_sb[qs_][:],in_=tp_ps[0][:]))
            state['tp_free'][0]=e_c
            st_ev_q[qs_]=k.dma(sp,st_q[qs_],QT[:,:,tok0:tok0+128].rearrange("h d t -> d h t"),qT_sb[qs_][:],deps=[e_c]); out_evs.append(st_ev_q[qs_])
            last_pe=e_t
        if do_pool is not None:
            slot,prev_free=do_pool
            r=proj(1536,1024)
            evs=[]
            act.wait(e_rs, prev_free)
            for bi in range(2):
                act.wait(r[bi][1])
                e=act.sig(nc.scalar.activation(out=pring[slot][:,bi*512:(bi+1)*512],in_=r[bi][0][:],func=AF.Copy,scale=rstd[s][:,0:1]))
                state['pj_free'][r[bi][2]]=e; evs.append(e)
            state['p_ready']=evs[-1]
            last_pe=r[1][1]
        state['free_x'][s]=last_pe if last_pe is not None else None
        state['free_x'][s]=[e_ss]+e_xb
        state['free_xT'][s]=last_pe
        state['back_done'][s]=[state['work'], state.get('p_ready')]
        return None

    def pool_tile(i, srcs, mixT):
        ys=i%2
        pe.wait(state['p_ready'], state['tp_free'][1], ev_c)
        mps=pj_ps[0];
        j0=state['pjn']%4; j1=(state['pjn']+1)%4; state['pjn']+=2
        pe.wait(state['pj_free'][j0], state['pj_free'][j1])
        for ch in range(8):
            g=ch//2; bank=pj_ps[j0] if ch<4 else pj_ps[j1]
            for si,(slot,ab) in enumerate(srcs):
                ins=nc.tensor.matmul(bank[:,(ch%4)*128:(ch%4+1)*128],lhsT=pring[slot][:,ch*128:(ch+1)*128],rhs=Aband[:,ab*4+g,:],start=(si==0),stop=(si==len(srcs)-1))
        e_m=pe.sig(ins)
        dve.wait(e_m, state.get('mT_free'))
        e0=dve.sig(nc.vector.tensor_copy(out=mT[:,0:4,:],in_=pj_ps[j0][:].rearrange("p (c t) -> p c t",c=4)))
        e1=dve.sig(nc.vector.tensor_copy(out=mT[:,4:8,:],in_=pj_ps[j1][:].rearrange("p (c t) -> p c t",c=4)))
        pe.wait(e0,e1,ev_pw)
        for ch in range(8):
            g=ch//2; dd=ch%2; bank=pj_ps[j0] if ch<4 else pj_ps[j1]
            for cc in range(2):
                ins=nc.tensor.matmul(bank[:,(ch%4)*128:(ch%4+1)*128],lhsT=pw[:,g*2+cc,dd*128:(dd+1)*128],rhs=mT[:,g*2+cc,:],start=(cc==0),stop=(cc==1))
        e_y=pe.sig(ins)
        state['mT_free']=e_y
        dve.wait(e_y, st_ev_y[ys])
        for hb,bank in enumerate((pj_ps[j0],pj_ps[j1])):
            e=dve.sig(nc.vector.tensor_tensor(out=yT[ys][:,hb*4:(hb+1)*4,:],in0=bank[:].rearrange("p (c t) -> p c t",c=4),in1=psc[:,hb*4:(hb+1)*4].unsqueeze(2).to_broadcast([128,4,128]),op=ALU.mult))
        state['pj_free'][j0]=e; state['pj_free'][j1]=e
        st_ev_y[ys]=k.dma(sp,st_y[ys],mixT[8:16,:,i*128:(i+1)*128].rearrange("c d t -> d c t"),yT[ys][:],deps=[e]); out_evs.append(st_ev_y[ys])
        return e_m

    pending=None
    for t in range(NKVT):
        b_=tile(d['xkv'][t*128:(t+1)*128,:],128,d['ckv'],d['skv'],t*128,do_kv=(d['KT'],d['V'],t))
        if pending is not None: pending()
        pending=b_
    b_=tile(d['meta'][0:16,:],16,d['ckv'],d['skv'],NKVT*128,do_kv=(d['KT'],d['V'],NKVT))
    pending(); b_()
    tile(d['xh'][:,:],128,None,None,0,do_pool=(3,None))
    em=[None]*(NQT+1)
    def srcs_for(i):
        sr=[]
        sr.append(((i-1)%3,1) if i>0 else (3,3))
        sr.append((i%3,0 if i<NQT-1 else 5))
        sr.append(((i+1)%3,2) if i<NQT-1 else (3,4))
        return sr
    for i in range(NQT):
        tile(d['xq'][i*128:(i+1)*128,:],128,d['cq'],d['sq'],i*128,do_q=d['QT'],do_pool=(i%3,em[i-2] if i>=2 else None))
        if i>=1: em[i-1]=pool_tile(i-1,srcs_for(i-1),d['mixT'])
    em[NQT-1]=pool_tile(NQT-1,srcs_for(NQT-1),d['mixT'])
    return out_evs


def attention_phase(k, QT, KT, V, attnT, nbias, ones_bf, NQT, NKF, PART, scale, deps=(), bg=None, bg_every=40):
    nc=k.nc
    NQ=NQT*128; NK=NKF*128+PART; NKT=NKF+(1 if PART else 0)
    kt_sb=k.sb([128,NK],BF16,"kt_sb"); v_sb=k.sb([128,NKT,128],BF16,"v_sb"); q_sb=k.sb([128,4,NQ],BF16,"q_sb")
    pT=[k.sb([128,512],BF16,f"pT{i}") for i in range(2)]
    rec=k.sb([128,512],F32,"rec"); ob=[k.sb([128,512],BF16,f"ob{i}") for i in range(2)]
    s_ps=[k.ps([128,512],F32,f"s_ps{i}") for i in range(3)]
    o_ps=[k.ps([128,512],F32,f"o_ps{i}") for i in range(2)]
    l_ps=[k.ps([128,512],F32,f"l_ps{i}") for i in range(2)]
    ld=k.dsem("attn_ld"); st=[k.dsem(f"attn_st{i}") for i in range(2)]
    out_evs=[]; pe=k.pe; act=k.act; dve=k.dve; sp=k.sp
    last_pe_of_head=None; norm_ev=[None,None]; st_ev=[None,None]
    gq=0
    for h in range(2):
        dd=list(deps)+([last_pe_of_head] if last_pe_of_head else [])
        NCH=8
        cw=(NK+NCH-1)//NCH
        for c in range(NCH):
            lo=c*cw; hi=min(NK,lo+cw)
            ev_k=k.dma(sp,ld,kt_sb[:,lo:hi],KT[h,:,lo:hi],deps=dd)
        ev_v=k.dma(sp,ld,v_sb[:,0:NKF,:],V[0:NKF,:,h*128:(h+1)*128].rearrange("t p d -> p t d"),deps=dd)
        if PART:
            ev_v=k.dma(sp,ld,v_sb[0:PART,NKF,:],V[NKF,0:PART,h*128:(h+1)*128],deps=dd)
        ev_q=k.dma(sp,ld,q_sb[:],QT[4*h:4*h+4].rearrange("h d q -> d h q"),deps=dd)
        ld_ev=(ld,ld.cnt)
        steps=[(qi,kt) for qi in range(NQT) for kt in range(NKT)]
        n=len(steps)
        s_ev=[None]*n; e_ev=[None]*n
        def issue_S(i):
            qi,kt=steps[i]; rows=128 if kt<NKF else PART
            pe.wait(ld_ev)
            ins=nc.tensor.matmul(s_ps[i%3][0:rows,:], lhsT=kt_sb[:,kt*128:kt*128+rows], rhs=q_sb[:,:,qi*128:(qi+1)*128], start=True, stop=True)
            s_ev[i]=pe.sig(ins)
        issue_S(0)
        if n>1: issue_S(1)
        for i,(qi,kt) in enumerate(steps):
            rows=128 if kt<NKF else PART
            par=(gq+qi)%2
            act.wait(s_ev[i])
            ins=nc.scalar.activation(out=pT[i%2][0:rows,:], in_=s_ps[i%3][0:rows,:], func=AF.Exp, bias=nbias[0:rows,0:1], scale=scale)
            e_ev[i]=act.sig(ins)
            pe.wait(e_ev[i])
            if kt==0: pe.wait(norm_ev[par])
            nc.tensor.matmul(o_ps[par][:], lhsT=v_sb[0:rows,kt,:], rhs=pT[i%2][0:rows,:], start=(kt==0), stop=(kt==NKT-1))
            ins=nc.tensor.matmul(l_ps[par][:], lhsT=ones_bf[0:rows,:], rhs=pT[i%2][0:rows,:], start=(kt==0), stop=(kt==NKT-1))
            pv_ev=pe.sig(ins)
            if i+2<n: issue_S(i+2)
            if bg is not None and i%bg_every==bg_every-1: bg()
            if kt==NKT-1:
                dve.wait(pv_ev, st_ev[par], norm_ev[1-par])
                r1=dve.sig(nc.vector.reciprocal(out=rec[:], in_=l_ps[par][:]))
                dve.wait(r1)
                ins=nc.vector.tensor_tensor(out=ob[par][:], in0=o_ps[par][:], in1=rec[:], op=ALU.mult)
                norm_ev[par]=dve.sig(ins)
                st_ev[par]=k.dma(sp,st[par],attnT[4*h:4*h+4,:,qi*128:(qi+1)*128].rearrange("h d q -> d h q"),
                                 ob[par][:].rearrange("d (h q) -> d h q",h=4),deps=[norm_ev[par]])
                out_evs.append(st_ev[par])
                last_pe_of_head=pv_ev
        gq+=NQT
    while bg is not None and bg(): pass
    return out_evs


def phase_c(k, C, d, NQT, NBLK):
    nc=k.nc; pe=k.pe; act=k.act; dve=k.dve; pool=k.pool; sp=k.sp
    EPS=1e-6; T=NQT; S2=2*T
    ident_f=C['ident_f']
    wo=k.sb([128,16,2048],BF16,"wo"); wst=[k.sb([128,2048],F32,f"wost{i}") for i in range(2)]
    g2r=k.sb([128,2048],F32,"g2r"); wr=k.sb([128,16,36],F32,"wr"); brr=k.sb([128,36],F32,"brr")
    epsb=k.sb([128,1],F32,"epsb2")
    LGT=C['LGT']
    cs=k.dsem("c_const"); wl=[k.dsem(f"c_wl{i}") for i in range(2)]
    k.dma(sp,cs,g2r[:],d['g2r'][:,:]); k.dma(sp,cs,wr[:],d['w_r'][:,:,:]); ev_c=k.dma(sp,cs,brr[:],d['b_r'][:,:])
    e_eps=dve.sig(nc.vector.memset(epsb[:],EPS))
    wev=[None,None]; cev=[None,None]
    for c in range(16):
        s=c%2
        wev[s]=k.dma(sp,wl[s],wst[s][:],d['w_out'][c*128:(c+1)*128,:],deps=[cev[s]])
        q=dve if s==0 else pool
        q.wait(wev[s])
        cev[s]=q.sig((nc.vector if s==0 else nc.gpsimd).tensor_copy(out=wo[:,c,:],in_=wst[s][:]))
    w_done=list(cev)
    mx=[k.sb([128,16,128],BF16,f"mx{i}") for i in range(2)]
    xt=[k.sb([128,2048],F32,f"cxt{i}") for i in range(2)]
    h1=[k.sb([128,2048],F32,f"h1_{i}") for i in range(2)]
    bf=k.sb([128,2048],F32,"bf"); b16=[k.sb([128,2048],BF16,f"b16_{i}") for i in range(2)]
    bT=k.sb([128,16,128],F32,"bT32"); junk=k.sb([128,2048],BF16,"cjunk")
    ssq=k.sb([128,1],F32,"cssq"); rstd=k.sb([128,1],F32,"crstd")
    po=[k.ps([128,512],F32,f"po{i}") for i in range(4)]
    tp=[k.ps([128,4,128],F32,f"ctp{i}") for i in range(2)]
    lp=k.ps([128,36],F32,"lp")
    ld=[k.dsem(f"c_ld{i}") for i in range(2)]; sth=[k.dsem(f"c_sth{i}") for i in range(2)]; stb=[k.dsem(f"c_stb{i}") for i in range(2)]
    mx_free=[None,None]; xt_free=[None,None]; h1_free=[None,None]; b16_free=[None,None]; po_free=[None]*4; tp_free=[None,None]
    bf_free=None; bT_free=None; lp_free=None
    out_evs=[]
    for i in range(T):
        s=i%2
        ev_m=k.dma(sp,ld[s],mx[s][:],d['mixT'][:,:,i*128:(i+1)*128].rearrange("c d t -> d c t"),deps=[mx_free[s]])
        ev_x=k.dma(sp,ld[s],xt[s][:],d['xq'][i*128:(i+1)*128,:],deps=[xt_free[s]])
        pe.wait(ev_x, w_done)
        pev=[]
        for n in range(4):
            pe.wait(po_free[n])
            for c in range(16):
                ins=nc.tensor.matmul(po[n][:],lhsT=mx[s][:,c,:],rhs=wo[:,c,n*512:(n+1)*512],start=(c==0),stop=(c==15))
            pev.append(pe.sig(ins))
        mx_free[s]=pev[3]
        dve.wait(h1_free[s])
        for n in range(4):
            dve.wait(pev[n])
            e=dve.sig(nc.vector.tensor_tensor(out=h1[s][:,n*512:(n+1)*512],in0=po[n][:],in1=xt[s][:,n*512:(n+1)*512],op=ALU.add))
            po_free[n]=e
        e_h1=e; xt_free[s]=e
        ev_sh=k.dma(sp,sth[s],d['h1'][i*128:(i+1)*128,:],h1[s][:],deps=[e_h1]); out_evs.append(ev_sh)
        act.wait(e_h1, e_eps)
        e=act.sig(nc.scalar.activation(out=junk[:],in_=h1[s][:],func=AF.Square,accum_out=ssq[:])); act.wait(e)
        e=act.sig(nc.scalar.activation(out=rstd[:],in_=ssq[:],func=AF.Sqrt,scale=1.0/2048,bias=epsb[:,0:1])); dve.wait(e)
        e=dve.sig(nc.vector.reciprocal(out=rstd[:],in_=rstd[:])); dve.wait(e, bf_free, ev_c)
        e_bf=dve.sig(nc.vector.scalar_tensor_tensor(out=bf[:],in0=h1[s][:],scalar=rstd[:,0:1],in1=g2r[:],op0=ALU.mult,op1=ALU.mult))
        h1_free[s]=[e_bf,ev_sh]
        pool.wait(e_bf, b16_free[s])
        e_b16=pool.sig(nc.gpsimd.tensor_copy(out=b16[s][:],in_=bf[:]))
        b16_free[s]=k.dma(sp,stb[s],d['b16'][i*128:(i+1)*128,:],b16[s][:],deps=[e_b16]); out_evs.append(b16_free[s])
        pe.wait(e_bf)
        tev=[]
        for gch in range(4):
            j=gch%2
            pe.wait(tp_free[j])
            for c4 in range(4):
                c=gch*4+c4
                ins=nc.tensor.transpose(tp[j][:,c4,:],bf[:,c*128:(c+1)*128],ident_f[:])
            e_t=pe.sig(ins)
            q=dve if j==0 else act
            q.wait(e_t, bT_free)
            if j==0: e=dve.sig(nc.vector.tensor_copy(out=bT[:,gch*4:gch*4+4,:],in_=tp[j][:]))
            else: e=act.sig(nc.scalar.copy(out=bT[:,gch*4:gch*4+4,:],in_=tp[j][:]))
            tp_free[j]=e; tev.append(e)
        bf_free=[e_t,e_b16]
        pe.wait(*tev); pe.wait(lp_free, ev_c)
        for c in range(16):
            ins=nc.tensor.matmul(lp[:],lhsT=bT[:,c,:],rhs=wr[:,c,:],start=(c==0),stop=(c==15))
        e_l=pe.sig(ins); bT_free=e_l
        dve.wait(e_l)
        lp_free=dve.sig(nc.vector.tensor_tensor(out=LGT[:,i,:],in0=lp[:],in1=brr[:],op=ALU.add))
    return out_evs

def phase_c2(k, C, d, NQT, NBLK):
    nc=k.nc; pe=k.pe; act=k.act; dve=k.dve; pool=k.pool; sp=k.sp
    T=NQT; S2=2*T; LGT=C['LGT']
    po=[k.ps([128,512],F32,f"c2po{i}") for i in range(2)]; po_free=[None,None]
    junk=k.sb([128,2048],BF16,"c2junk"); b16=[k.sb([128,2048],BF16,f"c2b16_{i}") for i in range(2)]
    ld=[k.dsem(f"c2_ld{i}") for i in range(2)]
    lp_free=None; out_evs=[]
    V=nc.vector
    def dv(ins, *w):
        return dve.sig(ins)
    cnt=[0]
    def T_(shape,dt=F32):
        cnt[0]+=1; return k.sb(shape,dt,f"rt{cnt[0]}")
    LG=LGT[:,:,0:4]; LE=LGT[:,:,4:36]
    gmax=T_([128,T]); dd=T_([128,T,4]); ohg=T_([128,T,4]); eg=T_([128,T,4]); sg=T_([128,T]); gp=T_([128,T])
    tmp=T_([128,T,32]); sel=T_([128,T,8]); v1=T_([128,T]); m1=T_([128,T,8]); sel2=T_([128,T,8]); v2=T_([128,T]); m2=T_([128,T,8])
    rr=T_([128,T]); g1_=T_([128,T]); Mall=T_([128,S2,32]); Mbf=T_([128,S2,32],BF16)
    Utri=T_([128,128],BF16); onesb=T_([128,128],BF16)
    rs=k.dsem("c_rs")
    k.dma(sp,rs,Utri[:],d['Utri'][:,:]); ev_u=k.dma(sp,rs,onesb[:],d['ones_bf'][:,:])
    thr=T_([128,32]); blkst=T_([128,NBLK])
    k.dma(sp,rs,thr[:],d['thr'][:,:]); ev_u=k.dma(sp,rs,blkst[:],d['blkst'][:,:])
    dve.wait(lp_free)
    def op(ins):
        e=dve.sig(ins); dve.wait(e); return e
    op(V.tensor_reduce(out=gmax[:],in_=LG,axis=AX.X,op=ALU.max))
    op(V.tensor_tensor(out=dd[:],in0=LG,in1=gmax[:].unsqueeze(2).to_broadcast([128,T,4]),op=ALU.subtract))
    op(V.tensor_single_scalar(out=ohg[:],in_=dd[:],scalar=0.0,op=ALU.is_ge))
    e=op(V.tensor_copy(out=eg[:],in_=dd[:]))
    act.wait(e); e=act.sig(nc.scalar.activation(out=eg[:],in_=dd[:],func=AF.Exp)); dve.wait(e)
    op(V.tensor_reduce(out=sg[:],in_=eg[:],axis=AX.X,op=ALU.add))
    op(V.reciprocal(out=gp[:],in_=sg[:]))
    op(V.tensor_tensor(out=tmp[:].rearrange("p t (g e) -> p t g e",g=4),in0=LE.rearrange("p t (g e) -> p t g e",g=4),in1=ohg[:].unsqueeze(3).to_broadcast([128,T,4,8]),op=ALU.mult))
    op(V.tensor_reduce(out=sel[:],in_=tmp[:].rearrange("p t (g e) -> p t e g",g=4),axis=AX.X,op=ALU.add))
    op(V.tensor_reduce(out=v1[:],in_=sel[:],axis=AX.X,op=ALU.max))
    op(V.tensor_tensor(out=m1[:],in0=sel[:],in1=v1[:].unsqueeze(2).to_broadcast([128,T,8]),op=ALU.is_ge))
    op(V.scalar_tensor_tensor(out=sel2[:],in0=m1[:],scalar=-1e30,in1=sel[:],op0=ALU.mult,op1=ALU.add))
    op(V.tensor_reduce(out=v2[:],in_=sel2[:],axis=AX.X,op=ALU.max))
    op(V.tensor_tensor(out=m2[:],in0=sel2[:],in1=v2[:].unsqueeze(2).to_broadcast([128,T,8]),op=ALU.is_ge))
    e=op(V.tensor_tensor(out=rr[:],in0=v2[:],in1=v1[:],op=ALU.subtract))
    act.wait(e); e=act.sig(nc.scalar.activation(out=rr[:],in_=rr[:],func=AF.Exp)); dve.wait(e)
    op(V.tensor_scalar(out=rr[:],in0=rr[:],scalar1=1.0,scalar2=None,op0=ALU.add))
    op(V.reciprocal(out=rr[:],in_=rr[:]))
    gates=C['gates']
    op(V.tensor_tensor(out=gates[:,:,0],in0=gp[:],in1=rr[:],op=ALU.mult))
    op(V.tensor_tensor(out=gates[:,:,1],in0=gp[:],in1=gates[:,:,0],op=ALU.subtract))
    M4=Mall[:].rearrange("p (t k) (g e) -> p t k g e",k=2,g=4)
    for kk,mk in enumerate((m1,m2)):
        op(V.tensor_tensor(out=M4[:,:,kk,:,:],in0=ohg[:].unsqueeze(3).to_broadcast([128,T,4,8]),in1=mk[:].unsqueeze(2).to_broadcast([128,T,4,8]),op=ALU.mult))
    e_mb=op(V.tensor_copy(out=Mbf[:],in_=Mall[:]))
    R=T_([128,S2,32]); Tot=T_([128,S2,32])
    nch=(S2*32+511)//512
    pe.wait(e_mb, ev_u)
    Mflat=Mbf[:].rearrange("p s e -> p (s e)"); Rflat=R[:].rearrange("p s e -> p (s e)"); Tflat=Tot[:].rearrange("p s e -> p (s e)")
    for c in range(nch):
        w=min(512,S2*32-c*512)
        pe.wait(po_free[0],po_free[1])
        ins=nc.tensor.matmul(po[0][:,0:w],lhsT=Utri[:],rhs=Mflat[:,c*512:c*512+w],start=True,stop=True)
        ins=nc.tensor.matmul(po[1][:,0:w],lhsT=onesb[:],rhs=Mflat[:,c*512:c*512+w],start=True,stop=True)
        e=pe.sig(ins); dve.wait(e)
        op(V.tensor_copy(out=Rflat[:,c*512:c*512+w],in_=po[0][:,0:w]))
        e=op(V.tensor_copy(out=Tflat[:,c*512:c*512+w],in_=po[1][:,0:w]))
        po_free[0]=e; po_free[1]=e
    base=T_([128,S2+1,32])
    op(V.memset(base[:,0,:],0.0))
    for s_ in range(S2):
        op(V.tensor_tensor(out=base[:,s_+1,:],in0=base[:,s_,:],in1=Tot[:,s_,:],op=ALU.add))
    counts=base[:,S2,:]
    cmp=T_([128,32,32]); nb=T_([128,32]); padded=T_([128,32]); pst=T_([128,33])
    op(V.tensor_tensor(out=cmp[:],in0=counts.unsqueeze(2).to_broadcast([128,32,32]),in1=thr[:].unsqueeze(1).to_broadcast([128,32,32]),op=ALU.is_gt))
    op(V.tensor_reduce(out=nb[:],in_=cmp[:],axis=AX.X,op=ALU.add))
    op(V.tensor_scalar(out=padded[:],in0=nb[:],scalar1=128.0,scalar2=None,op0=ALU.mult))
    op(V.memset(pst[:,0:1],0.0))
    for e_ in range(32):
        op(V.tensor_tensor(out=pst[:,e_+1:e_+2],in0=pst[:,e_:e_+1],in1=padded[:,e_:e_+1],op=ALU.add))
    RB=T_([128,S2,32]); posf=T_([128,S2])
    op(V.tensor_tensor(out=RB[:],in0=R[:],in1=base[:,0:S2,:],op=ALU.add))
    op(V.tensor_tensor(out=RB[:],in0=RB[:],in1=pst[:,0:32].unsqueeze(1).to_broadcast([128,S2,32]),op=ALU.add))
    op(V.tensor_tensor(out=RB[:],in0=RB[:],in1=Mall[:],op=ALU.mult))
    op(V.tensor_reduce(out=posf[:],in_=RB[:],axis=AX.X,op=ALU.add))
    e_pos=op(V.tensor_copy(out=C['pos_i'][:],in_=posf[:]))
    cmp2=T_([128,NBLK,32]); bef=T_([128,NBLK])
    op(V.tensor_tensor(out=cmp2[:],in0=pst[:,1:33].unsqueeze(1).to_broadcast([128,NBLK,32]),in1=blkst[:].unsqueeze(2).to_broadcast([128,NBLK,32]),op=ALU.is_le))
    op(V.tensor_reduce(out=bef[:],in_=cmp2[:],axis=AX.X,op=ALU.add))
    op(V.tensor_scalar(out=bef[:],in0=bef[:],scalar1=31.0,scalar2=None,op0=ALU.min))
    e_blk=op(V.tensor_copy(out=C['blk_e'][:],in_=bef[:]))
    sc=k.dsem("c_scat"); zs=k.dsem("c_zero")
    e_z=dve.sig(nc.vector.memset(junk[:],0.0))
    zev=None
    for r0 in range(0,NBLK,8):
        nb_=min(8,NBLK-r0)
        zev=k.dma(sp,zs,d['xs'][r0*128:(r0+nb_)*128,:].rearrange("(r p) n -> p r n",p=128),junk[:].unsqueeze(1).to_broadcast([128,nb_,2048]),deps=[e_z])
    pool.wait(zev)
    sp.wait(*out_evs)
    pool.wait(e_pos)
    scat_free=[None,None]
    for i in range(T):
        s=i%2
        ev=k.dma(sp,ld[s],b16[s][:],d['b16'][i*128:(i+1)*128,:],deps=[scat_free[s]]+out_evs)
        pool.wait(ev)
        for kk in range(2):
            ins=nc.gpsimd.indirect_dma_start(out=d['xs'][:,:],out_offset=bass.IndirectOffsetOnAxis(ap=C['pos_i'][:,i*2+kk:i*2+kk+1],axis=0),in_=b16[s][:],in_offset=None)
            ins.then_inc(sc.h,16); sc.cnt+=16
        scat_free[s]=(sc,sc.cnt)
        pool.wait(scat_free[s])
    return [(sc,sc.cnt), e_blk, e_pos]


class WConv:
    def __init__(self, k, d):
        self.k=k; self.d=d; self.n=0
        self.st=[k.sb([128,4096],F32,f"wc_st{i}") for i in range(2)]; self.bf=[k.sb([128,4096],BF16,f"wc_bf{i}") for i in range(2)]
        self.ld=[k.dsem(f"wc_ld{i}") for i in range(2)]; self.so=[k.dsem(f"wc_so{i}") for i in range(2)]
        self.st_free=[None,None]; self.bf_free=[None,None]; self.evs=[]
        self.jobs=[(e,mi,half) for e in range(32) for mi in range(3) for half in range(2)]
    def step(self):
        if self.n>=len(self.jobs): return False
        k=self.k; nc=k.nc; d=self.d
        e,mi,half=self.jobs[self.n]; s=self.n%2; self.n+=1
        src=(d['w_gate'],d['w_up'],d['w_down'])[mi]; dst=(d['wg_bf'],d['wu_bf'],d['wd_bf'])[mi]
        C_=src.shape[1]//128; F=src.shape[2]; c0=half*C_//2; c1=c0+C_//2
        ev=k.dma(k.sp,self.ld[s],self.st[s][:].rearrange("p (c f) -> p c f",c=C_//2),src[e,c0*128:c1*128,:].rearrange("(c p) f -> p c f",p=128),deps=[self.st_free[s]])
        cev=[]
        for (q,eng,lo,hi) in ((k.dve,nc.vector,0,3072),(k.pool,nc.gpsimd,3072,4096)):
            q.wait(ev,self.bf_free[s])
            cev.append(q.sig(eng.tensor_copy(out=self.bf[s][:,lo:hi],in_=self.st[s][:,lo:hi])))
        self.st_free[s]=cev
        self.bf_free[s]=k.dma(k.sp,self.so[s],dst[e*128:(e+1)*128,c0*F:c0*F+4096],self.bf[s][:],deps=cev); self.evs.append(self.bf_free[s])
        return True

DEBUG_STATIC_W=False
def phase_d(k, C, d, NBLK, deps=()):
    nc=k.nc; pe=k.pe; act=k.act; dve=k.dve; pool=k.pool; sp=k.sp
    ident=C['ident_bf']
    wg=[k.sb([128,16*512],BF16,f"wg{i}") for i in range(2)]; wu=[k.sb([128,16*512],BF16,f"wu{i}") for i in range(2)]
    wd=[k.sb([128,4*2048],BF16,f"wd{i}") for i in range(2)]
    xs=[k.sb([128,2048],BF16,f"xs{i}") for i in range(2)]; xsT=k.sb([128,16,128],BF16,"xsT")
    sg=k.sb([128,512],F32,"sg"); hb=k.sb([128,512],BF16,"hb"); hT=k.sb([128,4,128],BF16,"hT")
    ysb=[k.sb([128,2048],F32,f"ysb{i}") for i in range(2)]
    idxi=k.sb([128,NBLK],I32,"idxi"); befl=k.sb([128,NBLK],F32,"befl")
    tp=[k.ps([128,8,128],BF16,f"dtp{i}") for i in range(2)]
    pg=k.ps([128,512],F32,"pg"); pu=k.ps([128,512],F32,"pu"); pd=[k.ps([128,512],F32,f"pd{i}") for i in range(4)]
    wl=[k.dsem(f"d_wl{i}") for i in range(2)]; xl=[k.dsem(f"d_xl{i}") for i in range(2)]; ys=[k.dsem(f"d_ys{i}") for i in range(2)]
    dve.wait(*deps)
    e=dve.sig(nc.vector.tensor_copy(out=befl[:],in_=C['blk_e'][:])); dve.wait(e)
    e=dve.sig(nc.vector.tensor_scalar(out=befl[:],in0=befl[:],scalar1=128.0,scalar2=None,op0=ALU.mult)); dve.wait(e)
    e=dve.sig(nc.vector.tensor_scalar(out=befl[:],in0=befl[:],scalar1=C['iota_p'][:,0:1],scalar2=None,op0=ALU.add)); dve.wait(e)
    e_idx=dve.sig(nc.vector.tensor_copy(out=idxi[:],in_=befl[:]))
    wgv=d['wg_bf'][:,:]; wuv=d['wu_bf'][:,:]; wdv=d['wd_bf'][:,:]
    w_free=[None,None]; xs_free=[None,None]; y_free=[None,None]
    wev=[None]*NBLK; xev=[None]*NBLK
    def issue_loads(b):
        s=b%2
        pool.wait(e_idx, w_free[s], *deps)
        if DEBUG_STATIC_W:
            for (dst,src) in ((wg[s],d['wg_bf']),(wu[s],d['wu_bf']),(wd[s],d['wd_bf'])):
                wev[b]=k.dma(sp,wl[s],dst[:],src[(b%32)*128:(b%32+1)*128,:],deps=[w_free[s]]+list(deps))
            xev[b]=k.dma(sp,xl[s],xs[s][:],d['xs'][b*128:(b+1)*128,:],deps=[xs_free[s]]+list(deps))
            return
        for (dst,src) in ((wg[s],wgv),(wu[s],wuv),(wd[s],wdv)):
            ins=nc.gpsimd.indirect_dma_start(out=dst[:],out_offset=None,in_=src,in_offset=bass.IndirectOffsetOnAxis(ap=idxi[:,b:b+1],axis=0))
            ins.then_inc(wl[s].h,16); wl[s].cnt+=16
        wev[b]=(wl[s],wl[s].cnt)
        xev[b]=k.dma(sp,xl[s],xs[s][:],d['xs'][b*128:(b+1)*128,:],deps=[xs_free[s]]+list(deps))
    issue_loads(0)
    out_evs=[]
    xsT_free=None; hT_free=None; sg_free=None; hb_free=None; pd_free=None; tp_free=[None,None]; pgu_free=None
    for b in range(NBLK):
        s=b%2
        if b+1<NBLK: issue_loads(b+1)
        pe.wait(xev[b])
        tev=[]
        for half in range(2):
            pe.wait(tp_free[half])
            for c8 in range(8):
                c=half*8+c8
                ins=nc.tensor.transpose(tp[half][:,c8,:],xs[s][:,c*128:(c+1)*128],ident[:])
            tev.append(pe.sig(ins))
        xs_free[s]=tev[1]
        dve.wait(tev[0], xsT_free); e0=dve.sig(nc.vector.tensor_copy(out=xsT[:,0:8,:],in_=tp[0][:]))
        act.wait(tev[1], xsT_free); e1=act.sig(nc.scalar.copy(out=xsT[:,8:16,:],in_=tp[1][:]))
        tp_free=[e0,e1]
        pe.wait(e0,e1,wev[b],pgu_free)
        for c in range(16):
            nc.tensor.matmul(pg[:],lhsT=xsT[:,c,:],rhs=wg[s][:,c*512:(c+1)*512],start=(c==0),stop=(c==15))
        e_g=pe.sig(nc.tensor.matmul(pg[:],lhsT=xsT[:,0,:],rhs=wg[s][:,0:512],start=False,stop=True,skip_group_check=True)) if False else None
        for c in range(16):
            ins=nc.tensor.matmul(pu[:],lhsT=xsT[:,c,:],rhs=wu[s][:,c*512:(c+1)*512],start=(c==0),stop=(c==15))
        e_u=pe.sig(ins); xsT_free=e_u
        act.wait(e_u, sg_free)
        e_s=act.sig(nc.scalar.activation(out=sg[:],in_=pg[:],func=AF.Silu))
        dve.wait(e_s, hb_free)
        e_h=dve.sig(nc.vector.tensor_tensor(out=hb[:],in0=sg[:],in1=pu[:],op=ALU.mult))
        sg_free=e_h; pgu_free=e_h
        pe.wait(e_h, tp_free[0])
        for c in range(4):
            ins=nc.tensor.transpose(tp[0][:,c,:],hb[:,c*128:(c+1)*128],ident[:])
        e_t=pe.sig(ins); hb_free=e_t
        dve.wait(e_t, hT_free)
        e_c=dve.sig(nc.vector.tensor_copy(out=hT[:],in_=tp[0][:,0:4,:]))
        tp_free[0]=e_c
        pe.wait(e_c, pd_free)
        dev=[]
        for n in range(4):
            for c in range(4):
                ins=nc.tensor.matmul(pd[n][:],lhsT=hT[:,c,:],rhs=wd[s][:,c*2048+n*512:c*2048+(n+1)*512],start=(c==0),stop=(c==3))
            dev.append(pe.sig(ins))
        hT_free=dev[3]; w_free[s]=dev[3]
        evs_=[]
        for n in range(4):
            q=dve if n%2==0 else act
            q.wait(dev[n], y_free[s])
            if n%2==0: e=dve.sig(nc.vector.tensor_copy(out=ysb[s][:,n*512:(n+1)*512],in_=pd[n][:]))
            else: e=act.sig(nc.scalar.copy(out=ysb[s][:,n*512:(n+1)*512],in_=pd[n][:]))
            evs_.append(e)
        pd_free=evs_
        y_free[s]=k.dma(sp,ys[s],d['Y'][b*128:(b+1)*128,:],ysb[s][:],deps=evs_)
        out_evs.append(y_free[s])
    return out_evs

def phase_e(k, C, d, NQT, deps=()):
    nc=k.nc; dve=k.dve; pool=k.pool; sp=k.sp
    h1=[k.sb([128,2048],F32,f"eh1_{i}") for i in range(2)]; y0=[k.sb([128,2048],F32,f"ey0_{i}") for i in range(2)]; y1=[k.sb([128,2048],F32,f"ey1_{i}") for i in range(2)]
    ob=[k.sb([128,2048],F32,f"eo_{i}") for i in range(2)]
    ld=[k.dsem(f"e_ld{i}") for i in range(2)]; gl=[k.dsem(f"e_gl{i}") for i in range(2)]; st=[k.dsem(f"e_st{i}") for i in range(2)]
    in_free=[None,None]; o_free=[None,None]; out_evs=[]
    gates=C['gates']
    for i in range(NQT):
        s=i%2
        ev_h=k.dma(sp,ld[s],h1[s][:],d['h1'][i*128:(i+1)*128,:],deps=[in_free[s]]+list(deps))
        pool.wait(in_free[s], *deps)
        for kk,dst in enumerate((y0[s],y1[s])):
            ins=nc.gpsimd.indirect_dma_start(out=dst[:],out_offset=None,in_=d['Y'][:,:],in_offset=bass.IndirectOffsetOnAxis(ap=C['pos_i'][:,i*2+kk:i*2+kk+1],axis=0))
            ins.then_inc(gl[s].h,16); gl[s].cnt+=16
        ev_g=(gl[s],gl[s].cnt)
        pool.wait(ev_g)
        dve.wait(ev_h,ev_g,o_free[s])
        e=dve.sig(nc.vector.scalar_tensor_tensor(out=ob[s][:],in0=y0[s][:],scalar=gates[:,i,0:1],in1=h1[s][:],op0=ALU.mult,op1=ALU.add)); dve.wait(e)
        e=dve.sig(nc.vector.scalar_tensor_tensor(out=ob[s][:],in0=y1[s][:],scalar=gates[:,i,1:2],in1=ob[s][:],op0=ALU.mult,op1=ALU.add))
        in_free[s]=e
        o_free[s]=k.dma(sp,st[s],d['out'][i*128:(i+1)*128,:],ob[s][:],deps=[e]); out_evs.append(o_free[s])
    return out_evs


def build(NKVT, NQT, PART=16):
    NQ=NQT*128; NK=NKVT*128+PART; NBLK=2*NQT+32
    nc=bass.Bass("TRN2", target_bir_lowering=False)
    def din(name,shape,dt=F32): return nc.dram_tensor(name,list(shape),dt,kind="ExternalInput").ap()
    def scr(name,shape,dt): return nc.dram_tensor(name,list(shape),dt).ap()
    d={}
    d['xkv']=din('xkv',[NKVT*128,2048]); d['xq']=din('xq',[NQ,2048]); d['xh']=din('xh',[128,2048]); d['meta']=din('meta',[128,2048])
    d['g1t']=din('g1t',[128,16]); d['gqr']=din('gqr',[128,128]); d['gkr']=din('gkr',[128,128])
    d['pool_w']=din('pool_w',[128,8,256]); d['pool_sc']=din('pool_sc',[128,8]); d['Aband']=din('Aband',[128,24,128],BF16)
    d['ident_bf']=din('ident_bf',[128,128],BF16); d['ident_f']=din('ident_f',[128,128]); d['ones_bf']=din('ones_bf',[128,128],BF16)
    d['w_in']=din('w_in',[2048,2560])
    d['ckv']=din('ckv',[(NKVT+1)*128,128]); d['skv']=din('skv',[(NKVT+1)*128,128]); d['cq']=din('cq',[NQ,128]); d['sq']=din('sq',[NQ,128])
    d['w_out']=din('w_out',[2048,2048]); d['g2r']=din('g2r',[128,2048]); d['w_r']=din('w_r',[128,16,36]); d['b_r']=din('b_r',[128,36])
    d['Utri']=din('Utri',[128,128],BF16); d['thr']=din('thr',[128,32]); d['blkst']=din('blkst',[128,NBLK]); d['iota_p']=din('iota_p',[128,1])
    d['w_gate']=din('w_gate',[32,2048,512]); d['w_up']=din('w_up',[32,2048,512]); d['w_down']=din('w_down',[32,512,2048])
    d['out']=nc.dram_tensor('out',[NQ,2048],F32,kind="ExternalOutput").ap()
    d['KT']=scr('KT',[2,128,NK],BF16); d['V']=scr('V',[NKVT+1,128,256],BF16); d['QT']=scr('QT',[8,128,NQ],BF16); d['mixT']=scr('mixT',[16,128,NQ],BF16)
    d['h1']=scr('h1',[NQ,2048],F32); d['b16']=scr('b16',[NQ,2048],BF16); d['xs']=scr('xs',[NBLK*128,2048],BF16); d['Y']=scr('Y',[NBLK*128,2048],F32)
    d['wg_bf']=scr('wg_bf',[4096,8192],BF16); d['wu_bf']=scr('wu_bf',[4096,8192],BF16); d['wd_bf']=scr('wd_bf',[4096,8192],BF16)
    with ExitStack() as es:
        k=K(nc,es); C={}
        C['ident_bf']=k.sb([128,128],BF16,"c_ident_bf"); C['ident_f']=k.sb([128,128],F32,"c_ident_f"); C['ones_bf']=k.sb([128,128],BF16,"c_ones_bf")
        C['iota_p']=k.sb([128,1],F32,"c_iota_p"); C['nbias']=k.sb([128,1],F32,"c_nbias")
        C['LGT']=k.sb([128,NQT,36],F32,"c_LGT"); C['pos_i']=k.sb([128,2*NQT],I32,"c_pos_i"); C['gates']=k.sb([128,NQT,2],F32,"c_gates"); C['blk_e']=k.sb([128,NBLK],I32,"c_blk_e")
        gqa=k.sb([128,128],F32,"c_gqa"); gka=k.sb([128,128],F32,"c_gka"); mq=k.sb([128,1],F32,"c_mq"); mk=k.sb([128,1],F32,"c_mk")
        s0=k.dsem("c0")
        k.dma(k.sp,s0,C['ones_bf'][:],d['ones_bf'][:,:]); k.dma(k.sp,s0,C['iota_p'][:],d['iota_p'][:,:])
        k.dma(k.sp,s0,gqa[:],d['gqr'][:,:]); ev=k.dma(k.sp,s0,gka[:],d['gkr'][:,:])
        dve=k.dve; V=nc.vector
        dve.wait(ev)
        def op(ins):
            e=dve.sig(ins); dve.wait(e); return e
        m2=k.sb([128,2],F32,"c_m2")
        for (ga,mm,col) in ((gqa,mq,0),(gka,mk,1)):
            op(V.tensor_reduce(out=mm[:],in_=ga[:],axis=AX.X,op=ALU.max))
            op(V.tensor_scalar(out=ga[:],in0=ga[:],scalar1=-1.0,scalar2=None,op0=ALU.mult))
            op(V.tensor_reduce(out=m2[:,col:col+1],in_=ga[:],axis=AX.X,op=ALU.max))
            op(V.tensor_tensor(out=mm[:],in0=mm[:],in1=m2[:,col:col+1],op=ALU.max))
        op(V.tensor_tensor(out=mq[:],in0=mq[:],in1=mk[:],op=ALU.mult))
        e_nb=op(V.tensor_scalar(out=C['nbias'][:],in0=mq[:],scalar1=-(128.0**0.5),scalar2=None,op0=ALU.mult))
        k.begin_phase(); evs=phase_a(k,C,d,NKVT,NQT); k.end_phase(evs)
        k.begin_phase(); wc=WConv(k,d)
        evs=attention_phase(k,d['QT'],d['KT'],d['V'],d['mixT'],C['nbias'],C['ones_bf'],NQT,NKVT,PART,128.0**-0.5,deps=[e_nb,(s0,s0.cnt)],bg=wc.step,bg_every=max(1,(2*NQT*(NKVT+1))//200))
        k.end_phase(evs+wc.evs)
        k.begin_phase(); evs=phase_c(k,C,d,NQT,NBLK); k.end_phase(evs)
        k.begin_phase(); evs=phase_c2(k,C,d,NQT,NBLK); k.end_phase(evs)
        k.begin_phase(); evs=phase_d(k,C,d,NBLK); k.end_phase(evs)
        k.begin_phase(); evs=phase_e(k,C,d,NQT); k.end_phase(evs)
    return nc

def const_inputs(j, NKVT, NQT, r0, NBLK):
    BF_=BF
    c={}
    r=np.arange(NKVT*128)
    Ck,Sk=rope_tables((r//GRID_W).astype(np.float32),(r%GRID_W).astype(np.float32))
    Cm,Sm=rope_tables(np.zeros(128,np.float32),np.zeros(128,np.float32))
    c['ckv']=np.concatenate([Ck,Cm]); c['skv']=np.concatenate([Sk,Sm])
    c['cq']=np.ascontiguousarray(Ck[r0:r0+NQT*128]); c['sq']=np.ascontiguousarray(Sk[r0:r0+NQT*128])
    c['Aband']=band_mats(j,NQT)
    c['ident_bf']=np.eye(128).astype(BF_); c['ident_f']=np.eye(128,dtype=np.float32); c['ones_bf']=np.ones((128,128),BF_)
    c['Utri']=np.triu(np.ones((128,128),np.float32),1).astype(BF_)
    c['thr']=np.tile((128*np.arange(32,dtype=np.float32))[None],(128,1)); c['blkst']=np.tile((128*np.arange(NBLK,dtype=np.float32))[None],(128,1))
    c['iota_p']=np.arange(128,dtype=np.float32).reshape(128,1)
    return c

def layout_params(p):
    f=lambda a: np.ascontiguousarray(np.asarray(a,dtype=np.float32))
    o={}
    o['g1t']=f(np.asarray(p['norm1_g'])[0].reshape(16,128).T)
    o['gqr']=f(np.tile(np.asarray(p['q_norm_g'])[0][None],(128,1))); o['gkr']=f(np.tile(np.asarray(p['k_norm_g'])[0][None],(128,1)))
    o['pool_w']=f(np.asarray(p['pool_w'])[0].reshape(4,2,128,256).transpose(2,0,1,3).reshape(128,8,256))
    o['pool_sc']=f(np.asarray(p['pool_scale'])[0].reshape(8,128).T)
    o['w_in']=f(np.asarray(p['w_in'])[0]); o['w_out']=f(np.asarray(p['w_out'])[0])
    o['g2r']=f(np.tile(np.asarray(p['norm2_g'])[0][None],(128,1)))
    wr=np.concatenate([np.asarray(p['w_router_group'])[0],np.asarray(p['w_router_expert'])[0]],1)
    o['w_r']=f(wr.reshape(16,128,36).transpose(1,0,2))
    o['b_r']=f(np.tile(np.concatenate([np.asarray(p['b_router_group'])[0],np.asarray(p['b_router_expert'])[0]])[None],(128,1)))
    o['w_gate']=f(np.asarray(p['w_gate'])[0]); o['w_up']=f(np.asarray(p['w_up'])[0]); o['w_down']=f(np.asarray(p['w_down'])[0])
    return o

def kernel(**inputs):
    x=np.asarray(inputs['x'],dtype=np.float32); meta=np.asarray(inputs['meta_tokens'],dtype=np.float32)
    B,S,D=x.shape
    NKVT=S//128; NQT=NKVT//4; NBLK=2*NQT+32
    P=layout_params(inputs)
    mp=np.zeros((128,D),np.float32); mp[:N_META]=meta
    nc=build(NKVT,NQT)
    in_maps=[]
    for c in range(8):
        b=c//4; j=c%4; r0=j*NQT*128; r1=r0+NQT*128
        m=dict(P); m.update(const_inputs(j,NKVT,NQT,r0,NBLK))
        m['xkv']=np.ascontiguousarray(x[b]); m['xq']=np.ascontiguousarray(x[b,r0:r1]); m['meta']=mp
        xh=np.zeros((128,D),np.float32)
        xh[0:8]=meta[8:16] if j==0 else x[b,r0-8:r0]
        if j<3: xh[8:16]=x[b,r1:r1+8]
        m['xh']=xh
        in_maps.append(m)
    res=run_bass_kernel_spmd(nc,in_maps,core_ids=list(range(8)))
    out=np.zeros((B,S,D),np.float32)
    for c in range(8):
        b=c//4; j=c%4; r0=j*NQT*128
        out[b,r0:r0+NQT*128]=np.asarray(res.results[c]['out'],dtype=np.float32)
    return out
```

```python
from contextlib import ExitStack
import ml_dtypes
from concourse.bass_utils import run_bass_kernel_spmd
import numpy as np
import concourse.bass as bass
import concourse.mybir as mybir
F32=mybir.dt.float32; BF16=mybir.dt.bfloat16; I32=mybir.dt.int32
AF=mybir.ActivationFunctionType; ALU=mybir.AluOpType; AX=mybir.AxisListType

class EngQ:
    def __init__(self, nc, es, name, e):
        self.name=name; self.e=e; self.h=es.enter_context(nc.semaphore("tl_"+name)); self.cnt=0; self.seen={}; self.uid="tl_"+name
    def wait(self, *evs):
        for ev in evs:
            if ev is None: continue
            if isinstance(ev, (list,tuple)) and len(ev)>0 and not hasattr(ev[0],'h'):
                self.wait(*ev); continue
            src,val=ev
            if src is self and False: pass
            if self.seen.get(src.uid,0)>=val: continue
            self.e.wait_ge(src.h,val); self.seen[src.uid]=val
    def sig(self, ins):
        self.cnt+=1; ins.then_inc(self.h,1); return (self,self.cnt)

class DSem:
    def __init__(self, nc, es, name):
        self.h=es.enter_context(nc.semaphore(name)); self.cnt=0; self.uid=name

class K:
    def __init__(self, nc, es):
        self.nc=nc; self.es=es
        self.pe=EngQ(nc,es,"pe",nc.tensor); self.dve=EngQ(nc,es,"dve",nc.vector)
        self.act=EngQ(nc,es,"act",nc.scalar); self.pool=EngQ(nc,es,"pool",nc.gpsimd); self.sp=EngQ(nc,es,"sp",nc.sync)
        self.engs=[self.pe,self.dve,self.act,self.pool,self.sp]
        self._n=0; self.pes=es
    def sb(self, shape, dt, name=None):
        self._n+=1; return self.pes.enter_context(self.nc.sbuf_tensor("s_"+(name or f"sb{self._n}"), shape, dt))
    def ps(self, shape, dt, name=None):
        self._n+=1; return self.pes.enter_context(self.nc.psum_tensor("p_"+(name or f"ps{self._n}"), shape, dt))
    def dsem(self, name=None):
        self._n+=1; return DSem(self.nc,self.es,name or f"ds{self._n}")
    def dma(self, q, sem, out, in_, deps=(), **kw):
        q.wait(*deps)
        ins=q.e.dma_start(out=out,in_=in_,**kw); ins.then_inc(sem.h,16); sem.cnt+=16
        return (sem,sem.cnt)
    def barrier(self, extra=()):
        last=[(q,q.cnt) for q in self.engs if q.cnt>0]
        for q in self.engs:
            for ev in last:
                q.wait(ev)
            q.wait(*extra)
    def begin_phase(self):
        from contextlib import ExitStack
        self.pes=ExitStack(); self.pes.__enter__()
    def end_phase(self, extra=()):
        self.barrier(extra)
        self.pes.__exit__(None,None,None); self.pes=self.es

BF=ml_dtypes.bfloat16
N_META=16; GRID_W=64; L=16400; S=16384
def rope_tables(pos_row, pos_col):
    inv=np.power(10000.0,-np.arange(0,64,2,dtype=np.float32)/64).astype(np.float32)
    ar=pos_row[:,None].astype(np.float32)*inv[None,:]; ac=pos_col[:,None].astype(np.float32)*inv[None,:]
    cr,sr,cc,sc=np.cos(ar),np.sin(ar),np.cos(ac),np.sin(ac)
    C=np.concatenate([cr,cr,cc,cc],1).astype(np.float32); Sg=np.concatenate([-sr,sr,-sc,sc],1).astype(np.float32)
    return C,Sg
def band_mats(j, NQT):
    A=np.zeros((6,4,128,128),np.float32)
    wins=(2,4,8,16)
    for g,w in enumerate(wins):
        for out in range(128):
            lo=out-w//2; hi=out+w//2-1
            for off in range(lo,hi+1):
                if 0<=off<128: A[0,g,off,out]+=1.0/w
                elif off<0:
                    A[1,g,128+off,out]+=1.0/w
                    A[3,g,8+off,out]+=1.0/w
                else:
                    A[2,g,off-128,out]+=1.0/w
                    A[4,g,8+(off-128),out]+=1.0/w
            A[0,g,out,out]-=1.0
        A[5,g]=A[0,g]
        if j==3:
            for out in range(128):
                t=16+S-128+out
                lo_t=max(t-w//2,0); hi_t=min(t-w//2+w,L)
                cnt=hi_t-lo_t
                if cnt!=w:
                    A[5,g,:,out]=0
                    for tt in range(lo_t,hi_t):
                        off=tt-(16+S-128)
                        if 0<=off<128: A[5,g,off,out]+=1.0/cnt
                    A[5,g,out,out]-=1.0
    return np.ascontiguousarray(A.reshape(24,128,128).transpose(1,0,2)).astype(BF)


def prep_consts(k, ident_src=None):
    nc=k.nc
    c={}
    c['ones_bf']=k.sb([128,128],BF16,"c_ones_bf")
    c['ident_bf']=k.sb([128,128],BF16,"c_ident_bf")
    c['ident_f']=k.sb([128,128],F32,"c_ident_f")
    return c

def phase_a(k, C, d, NKVT, NQT):
    nc=k.nc; pe=k.pe; act=k.act; dve=k.dve; pool=k.pool; sp=k.sp
    EPS=1e-6
    wsb=k.sb([128,16,2560],BF16,"w_in_bf")
    wst=[k.sb([128,2560],F32,f"wst{i}") for i in range(2)]
    g1=k.sb([128,16],F32,"g1t");
    gqr=k.sb([128,128],F32,"gqr"); gkr=k.sb([128,128],F32,"gkr")
    pw=k.sb([128,8,256],BF16,"pool_w_bf"); pwst=k.sb([128,8,256],F32,"pool_w_st")
    psc=k.sb([128,8],F32,"pool_sc")
    Aband=k.sb([128,24,128],BF16,"Aband")
    cs=k.dsem("a_const"); wl=[k.dsem(f"a_wl{i}") for i in range(2)]
    k.dma(sp,cs,g1[:],d['g1t'][:,:]); k.dma(sp,cs,gqr[:],d['gqr'][:,:]); k.dma(sp,cs,gkr[:],d['gkr'][:,:])
    k.dma(sp,cs,pwst[:],d['pool_w'][:,:,:]); k.dma(sp,cs,psc[:],d['pool_sc'][:,:]);
    ev_c=k.dma(sp,cs,Aband[:],d['Aband'][:,:,:])
    ident=C['ident_bf']
    epsb=k.sb([128,1],F32,"epsb"); e_eps=dve.sig(nc.vector.memset(epsb[:],EPS)); act.wait(e_eps)
    k.dma(sp,cs,ident[:],d['ident_bf'][:,:]); ev_c=k.dma(sp,cs,C['ident_f'][:],d['ident_f'][:,:])
    dve.wait(ev_c)
    ev_pw=dve.sig(nc.vector.tensor_copy(out=pw[:],in_=pwst[:]))
    wev=[None,None]; cev=[None,None]; w_done=None
    for c in range(16):
        s=c%2
        wev[s]=k.dma(sp,wl[s],wst[s][:],d['w_in'][c*128:(c+1)*128,:],deps=[cev[s]])
        dve.wait(wev[s])
        cev[s]=dve.sig(nc.vector.tensor_scalar(out=wsb[:,c,:],in0=wst[s][:],scalar1=g1[:,c:c+1],scalar2=None,op0=ALU.mult))
    w_done=cev
    xt=[k.sb([128,2048],F32,f"xt{i}") for i in range(2)]
    xb=[k.sb([128,2048],BF16,f"xb{i}") for i in range(2)]
    xT=[k.sb([128,16,128],BF16,f"xT{i}") for i in range(2)]
    junk=k.sb([128,2048],BF16,"junk")
    ssq=[k.sb([128,1],F32,f"ssq{i}") for i in range(2)]
    rstd=[k.sb([128,1],F32,f"rstd{i}") for i in range(2)]
    ct=[k.sb([128,128],F32,f"ct{i}") for i in range(2)]; stt=[k.sb([128,128],F32,f"stt{i}") for i in range(2)]
    xl=[k.dsem(f"a_xl{i}") for i in range(2)]
    tp_ps=[k.ps([128,8,128],BF16,f"tp_ps{i}") for i in range(2)]
    pj_ps=[k.ps([128,512],F32,f"pj_ps{i}") for i in range(4)]
    qs=k.sb([128,1024],F32,"qs"); sq=k.sb([128,1024],F32,"sq"); t1=k.sb([128,1024],F32,"t1"); t2=k.sb([128,1024],F32,"t2")
    hs=k.sb([128,8],F32,"hs"); hr=k.sb([128,8],F32,"hr")
    qb=k.sb([128,1024],BF16,"qbf"); qT_sb=[k.sb([128,8,128],BF16,f"qT_sb{i}") for i in range(2)]
    v_sb=[k.sb([128,256],BF16,f"v_sbA{i}") for i in range(2)]
    pring=[k.sb([128,1024],BF16,f"pring{i}") for i in range(4)]
    mT=k.sb([128,8,128],BF16,"mT"); yT=[k.sb([128,8,128],BF16,f"yT{i}") for i in range(2)]
    st_q=[k.dsem(f"a_stq{i}") for i in range(2)]; st_v=[k.dsem(f"a_stv{i}") for i in range(2)]; st_y=[k.dsem(f"a_sty{i}") for i in range(2)]
    st_ev_q=[None,None]; st_ev_v=[None,None]; st_ev_y=[None,None]
    out_evs=[]
    state={'back_done':[None,None],'n':0,'free_x':[None,None],'free_xT':[None,None],'tp_free':[None,None],'pj_free':[None]*4,'pjn':0, 'work':None,'qT_n':0,'v_n':0}

    def norm_rope(src_ps_list, H, rst, gr, ctile, stile, dest_bf, extra_wait=()):
        W=H*128
        evs=[]
        for bi,(pst,ev) in enumerate(src_ps_list):
            act.wait(ev, state['work'])
            w=min(512,W-bi*512)
            evs.append(act.sig(nc.scalar.activation(out=qs[:,bi*512:bi*512+w],in_=pst[:,0:w],func=AF.Copy,scale=rst[:,0:1])))
        dve.wait(*evs); dve.wait(*extra_wait)
        e=dve.sig(nc.vector.tensor_tensor(out=sq[:,0:W],in0=qs[:,0:W],in1=qs[:,0:W],op=ALU.mult)); dve.wait(e)
        e=dve.sig(nc.vector.tensor_reduce(out=hs[:,0:H],in_=sq[:,0:W].rearrange("p (h d) -> p h d",h=H),axis=AX.X,op=ALU.add)); dve.wait(e)
        act.wait(e)
        e=act.sig(nc.scalar.activation(out=hr[:,0:H],in_=hs[:,0:H],func=AF.Sqrt,scale=1.0/128,bias=epsb[:,0:1])); dve.wait(e)
        e=dve.sig(nc.vector.reciprocal(out=hr[:,0:H],in_=hr[:,0:H])); dve.wait(e)
        q3=qs[:,0:W].rearrange("p (h d) -> p h d",h=H)
        e=dve.sig(nc.vector.tensor_tensor(out=sq[:,0:W].rearrange("p (h d) -> p h d",h=H),in0=q3,in1=hr[:,0:H].unsqueeze(2).to_broadcast([128,H,128]),op=ALU.mult)); dve.wait(e)
        s3=sq[:,0:W].rearrange("p (h d) -> p h d",h=H)
        e=dve.sig(nc.vector.tensor_tensor(out=q3,in0=s3,in1=gr[:].unsqueeze(1).to_broadcast([128,H,128]),op=ALU.mult)); dve.wait(e)
        e1=dve.sig(nc.vector.tensor_tensor(out=t1[:,0:W].rearrange("p (h d) -> p h d",h=H),in0=q3,in1=ctile[:].unsqueeze(1).to_broadcast([128,H,128]),op=ALU.mult))
        q5=qs[:,0:W].rearrange("p (h b f e) -> p h b f e",h=H,b=2,f=2)
        t5=t2[:,0:W].rearrange("p (h b f e) -> p h b f e",h=H,b=2,f=2)
        s5=stile[:].rearrange("p (b f e) -> p b f e",b=2,f=2)
        for f in range(2):
            for b in range(2):
                e2=dve.sig(nc.vector.tensor_tensor(out=t5[:,:,b,f,:],in0=q5[:,:,b,1-f,:],in1=s5[:,b,f,:].unsqueeze(1).to_broadcast([128,H,32]),op=ALU.mult))
        dve.wait(e1,e2)
        e=dve.sig(nc.vector.tensor_tensor(out=dest_bf[:,0:W],in0=t1[:,0:W],in1=t2[:,0:W],op=ALU.add))
        state['work']=e
        return e

    def tile(src_ap, rows, ctab, stab, tok0, do_kv=None, do_q=None, do_pool=None):
        n=state['n']; s=n%2; state['n']+=1
        dd=[state['free_x'][s], state['back_done'][s]]
        if rows<128:
            dve.wait(state['free_x'][s]); z=dve.sig(nc.vector.memset(xt[s][:],0.0)); dd=[z]
        evx=k.dma(sp,xl[s],xt[s][0:rows,:],src_ap,deps=dd)
        if ctab is not None:
            k.dma(sp,xl[s],ct[s][0:rows,:],ctab[tok0:tok0+rows,:],deps=dd)
            evx=k.dma(sp,xl[s],stt[s][0:rows,:],stab[tok0:tok0+rows,:],deps=dd)
        act.wait(evx)
        e_ss=act.sig(nc.scalar.activation(out=junk[:],in_=xt[s][:],func=AF.Square,accum_out=ssq[s][:]))
        pool.wait(evx, state['free_xT'][s])
        e_xb0=pool.sig(nc.gpsimd.tensor_copy(out=xb[s][:,0:768],in_=xt[s][:,0:768]))
        dve.wait(evx, state['free_xT'][s])
        e_xb1=dve.sig(nc.vector.tensor_copy(out=xb[s][:,768:2048],in_=xt[s][:,768:2048]))
        e_xb=[e_xb0,e_xb1]
        act.wait(e_ss)
        act.wait(state['back_done'][s])
        e=act.sig(nc.scalar.activation(out=rstd[s][:],in_=ssq[s][:],func=AF.Sqrt,scale=1.0/2048,bias=epsb[:,0:1])); dve.wait(e, state['back_done'][s])
        e_rs=dve.sig(nc.vector.reciprocal(out=rstd[s][:],in_=rstd[s][:]))
        pe.wait(e_xb)
        tev=[]
        for half in range(2):
            pe.wait(state['tp_free'][half])
            for c8 in range(8):
                c=half*8+c8
                ins=nc.tensor.transpose(tp_ps[half][:,c8,:],xb[s][:,c*128:(c+1)*128],ident[:])
            tev.append(pe.sig(ins))
        state['free_x'][s]=None
        dve.wait(tev[0], state['free_xT'][s]);
        e0=dve.sig(nc.vector.tensor_copy(out=xT[s][:,0:8,:],in_=tp_ps[0][:]))
        act.wait(tev[1], state['free_xT'][s])
        e1=act.sig(nc.scalar.copy(out=xT[s][:,8:16,:],in_=tp_ps[1][:]))
        state['tp_free']=[e0,e1]
        xT_ready=[e0,e1]
        def proj(col0, ncols):
            res=[]
            for b0 in range(0,ncols,512):
                w=min(512,ncols-b0); j=state['pjn']%4; state['pjn']+=1
                pe.wait(xT_ready, w_done, state['pj_free'][j])
                for c in range(16):
                    ins=nc.tensor.matmul(pj_ps[j][:,0:w],lhsT=xT[s][:,c,:],rhs=wsb[:,c,col0+b0:col0+b0+w],start=(c==0),stop=(c==15))
                res.append((pj_ps[j],pe.sig(ins),j))
            return res
        last_pe=None
        if do_kv is not None:
            KT,V,tidx=do_kv
            r=proj(1024,512)[0]; pst,ev,j=r
            state['free_x'][s]=[e_ss]+e_xb; state['free_xT'][s]=ev
            def back():
                vs=state['v_n']%2; state['v_n']+=1
                act.wait(ev, e_rs, st_ev_v[vs])
                e_v=act.sig(nc.scalar.activation(out=v_sb[vs][:],in_=pst[:,256:512],func=AF.Copy,scale=rstd[s][:,0:1]))
                st_ev_v[vs]=k.dma(sp,st_v[vs],V[tidx,0:rows,:],v_sb[vs][0:rows,:],deps=[e_v]); out_evs.append(st_ev_v[vs])
                e_k=norm_rope([(pst,ev)],2,rstd[s],gkr,ct[s],stt[s],qb,extra_wait=[e_rs,state.get('qb_free')])
                state['pj_free'][j]=[e_k,e_v]
                qs_=state['qT_n']%2; state['qT_n']+=1
                pe.wait(e_k, state['tp_free'][0])
                for h in range(2):
                    ins=nc.tensor.transpose(tp_ps[0][:,h,:],qb[:,h*128:(h+1)*128],ident[:])
                e_t=pe.sig(ins)
                dve.wait(e_t, st_ev_q[qs_])
                e_c=dve.sig(nc.vector.tensor_copy(out=qT_sb[qs_][:,0:2,:],in_=tp_ps[0][:,0:2,:]))
                state['tp_free'][0]=e_c
                st_ev_q[qs_]=k.dma(sp,st_q[qs_],KT[:,:,tok0:tok0+rows].rearrange("h d t -> d h t"),qT_sb[qs_][:,0:2,0:rows],deps=[e_c]); out_evs.append(st_ev_q[qs_])
                state['qb_free']=e_t
                state['back_done'][s]=[e_k,e_v]
            return back
        if do_q is not None:
            QT=do_q
            r=proj(0,1024)
            e_q=norm_rope([(r[0][0],r[0][1]),(r[1][0],r[1][1])],8,rstd[s],gqr,ct[s],stt[s],qb,extra_wait=[e_rs,state.get('qb_free')])
            state['pj_free'][r[0][2]]=e_q; state['pj_free'][r[1][2]]=e_q
            qs_=state['qT_n']%2; state['qT_n']+=1
            pe.wait(e_q, state['tp_free'][0])
            for h in range(8):
                ins=nc.tensor.transpose(tp_ps[0][:,h,:],qb[:,h*128:(h+1)*128],ident[:])
            e_t=pe.sig(ins); state['qb_free']=e_t
            dve.wait(e_t, st_ev_q[qs_])
            e_c=dve.sig(nc.vector.tensor_copy(out=qT_sb[qs_][:],in_=tp_ps[0][:]))
            state['tp_free'][0]=e_c
            st_ev_q[qs_]=k.dma(sp,st_q[qs_],QT[:,:,tok0:tok0+128].rearrange("h d t -> d h t"),qT_sb[qs_][:],deps=[e_c]); out_evs.append(st_ev_q[qs_])
            last_pe=e_t
        if do_pool is not None:
            slot,prev_free=do_pool
            r=proj(1536,1024)
            evs=[]
            act.wait(e_rs, prev_free)
            for bi in range(2):
                act.wait(r[bi][1])
                e=act.sig(nc.scalar.activation(out=pring[slot][:,bi*512:(bi+1)*512],in_=r[bi][0][:],func=AF.Copy,scale=rstd[s][:,0:1]))
                state['pj_free'][r[bi][2]]=e; evs.append(e)
            state['p_ready']=evs[-1]
            last_pe=r[1][1]
        state['free_x'][s]=last_pe if last_pe is not None else None
        state['free_x'][s]=[e_ss]+e_xb
        state['free_xT'][s]=last_pe
        state['back_done'][s]=[state['work'], state.get('p_ready')]
        return None

    def pool_tile(i, srcs, mixT):
        ys=i%2
        pe.wait(state['p_ready'], state['tp_free'][1], ev_c)
        mps=pj_ps[0];
        j0=state['pjn']%4; j1=(state['pjn']+1)%4; state['pjn']+=2
        pe.wait(state['pj_free'][j0], state['pj_free'][j1])
        for ch in range(8):
            g=ch//2; bank=pj_ps[j0] if ch<4 else pj_ps[j1]
            for si,(slot,ab) in enumerate(srcs):
                ins=nc.tensor.matmul(bank[:,(ch%4)*128:(ch%4+1)*128],lhsT=pring[slot][:,ch*128:(ch+1)*128],rhs=Aband[:,ab*4+g,:],start=(si==0),stop=(si==len(srcs)-1))
        e_m=pe.sig(ins)
        dve.wait(e_m, state.get('mT_free'))
        e0=dve.sig(nc.vector.tensor_copy(out=mT[:,0:4,:],in_=pj_ps[j0][:].rearrange("p (c t) -> p c t",c=4)))
        e1=dve.sig(nc.vector.tensor_copy(out=mT[:,4:8,:],in_=pj_ps[j1][:].rearrange("p (c t) -> p c t",c=4)))
        pe.wait(e0,e1,ev_pw)
        for ch in range(8):
            g=ch//2; dd=ch%2; bank=pj_ps[j0] if ch<4 else pj_ps[j1]
            for cc in range(2):
                ins=nc.tensor.matmul(bank[:,(ch%4)*128:(ch%4+1)*128],lhsT=pw[:,g*2+cc,dd*128:(dd+1)*128],rhs=mT[:,g*2+cc,:],start=(cc==0),stop=(cc==1))
        e_y=pe.sig(ins)
        state['mT_free']=e_y
        dve.wait(e_y, st_ev_y[ys])
        for hb,bank in enumerate((pj_ps[j0],pj_ps[j1])):
            e=dve.sig(nc.vector.tensor_tensor(out=yT[ys][:,hb*4:(hb+1)*4,:],in0=bank[:].rearrange("p (c t) -> p c t",c=4),in1=psc[:,hb*4:(hb+1)*4].unsqueeze(2).to_broadcast([128,4,128]),op=ALU.mult))
        state['pj_free'][j0]=e; state['pj_free'][j1]=e
        st_ev_y[ys]=k.dma(sp,st_y[ys],mixT[8:16,:,i*128:(i+1)*128].rearrange("c d t -> d c t"),yT[ys][:],deps=[e]); out_evs.append(st_ev_y[ys])
        return e_m

    pending=None
    for t in range(NKVT):
        b_=tile(d['xkv'][t*128:(t+1)*128,:],128,d['ckv'],d['skv'],t*128,do_kv=(d['KT'],d['V'],t))
        if pending is not None: pending()
        pending=b_
    b_=tile(d['meta'][0:16,:],16,d['ckv'],d['skv'],NKVT*128,do_kv=(d['KT'],d['V'],NKVT))
    pending(); b_()
    tile(d['xh'][:,:],128,None,None,0,do_pool=(3,None))
    em=[None]*(NQT+1)
    def srcs_for(i):
        sr=[]
        sr.append(((i-1)%3,1) if i>0 else (3,3))
        sr.append((i%3,0 if i<NQT-1 else 5))
        sr.append(((i+1)%3,2) if i<NQT-1 else (3,4))
        return sr
    for i in range(NQT):
        tile(d['xq'][i*128:(i+1)*128,:],128,d['cq'],d['sq'],i*128,do_q=d['QT'],do_pool=(i%3,em[i-2] if i>=2 else None))
        if i>=1: em[i-1]=pool_tile(i-1,srcs_for(i-1),d['mixT'])
    em[NQT-1]=pool_tile(NQT-1,srcs_for(NQT-1),d['mixT'])
    return out_evs


def attention_phase(k, QT, KT, V, attnT, nbias, ones_bf, NQT, NKF, PART, scale, deps=(), bg=None, bg_every=40):
    nc=k.nc
    NQ=NQT*128; NK=NKF*128+PART; NKT=NKF+(1 if PART else 0)
    kt_sb=k.sb([128,NK],BF16,"kt_sb"); v_sb=k.sb([128,NKT,128],BF16,"v_sb"); q_sb=k.sb([128,4,NQ],BF16,"q_sb")
    pT=[k.sb([128,512],BF16,f"pT{i}") for i in range(2)]
    rec=k.sb([128,512],F32,"rec"); ob=[k.sb([128,512],BF16,f"ob{i}") for i in range(2)]
    s_ps=[k.ps([128,512],F32,f"s_ps{i}") for i in range(3)]
    o_ps=[k.ps([128,512],F32,f"o_ps{i}") for i in range(2)]
    l_ps=[k.ps([128,512],F32,f"l_ps{i}") for i in range(2)]
    ld=k.dsem("attn_ld"); st=[k.dsem(f"attn_st{i}") for i in range(2)]
    out_evs=[]; pe=k.pe; act=k.act; dve=k.dve; sp=k.sp
    last_pe_of_head=None; norm_ev=[None,None]; st_ev=[None,None]
    gq=0
    for h in range(2):
        dd=list(deps)+([last_pe_of_head] if last_pe_of_head else [])
        NCH=8
        cw=(NK+NCH-1)//NCH
        for c in range(NCH):
            lo=c*cw; hi=min(NK,lo+cw)
            ev_k=k.dma(sp,ld,kt_sb[:,lo:hi],KT[h,:,lo:hi],deps=dd)
        ev_v=k.dma(sp,ld,v_sb[:,0:NKF,:],V[0:NKF,:,h*128:(h+1)*128].rearrange("t p d -> p t d"),deps=dd)
        if PART:
            ev_v=k.dma(sp,ld,v_sb[0:PART,NKF,:],V[NKF,0:PART,h*128:(h+1)*128],deps=dd)
        ev_q=k.dma(sp,ld,q_sb[:],QT[4*h:4*h+4].rearrange("h d q -> d h q"),deps=dd)
        ld_ev=(ld,ld.cnt)
        steps=[(qi,kt) for qi in range(NQT) for kt in range(NKT)]
        n=len(steps)
        s_ev=[None]*n; e_ev=[None]*n
        def issue_S(i):
            qi,kt=steps[i]; rows=128 if kt<NKF else PART
            pe.wait(ld_ev)
            ins=nc.tensor.matmul(s_ps[i%3][0:rows,:], lhsT=kt_sb[:,kt*128:kt*128+rows], rhs=q_sb[:,:,qi*128:(qi+1)*128], start=True, stop=True)
            s_ev[i]=pe.sig(ins)
        issue_S(0)
        if n>1: issue_S(1)
        for i,(qi,kt) in enumerate(steps):
            rows=128 if kt<NKF else PART
            par=(gq+qi)%2
            act.wait(s_ev[i])
            ins=nc.scalar.activation(out=pT[i%2][0:rows,:], in_=s_ps[i%3][0:rows,:], func=AF.Exp, bias=nbias[0:rows,0:1], scale=scale)
            e_ev[i]=act.sig(ins)
            pe.wait(e_ev[i])
            if kt==0: pe.wait(norm_ev[par])
            nc.tensor.matmul(o_ps[par][:], lhsT=v_sb[0:rows,kt,:], rhs=pT[i%2][0:rows,:], start=(kt==0), stop=(kt==NKT-1))
            ins=nc.tensor.matmul(l_ps[par][:], lhsT=ones_bf[0:rows,:], rhs=pT[i%2][0:rows,:], start=(kt==0), stop=(kt==NKT-1))
            pv_ev=pe.sig(ins)
            if i+2<n: issue_S(i+2)
            if bg is not None and i%bg_every==bg_every-1: bg()
            if kt==NKT-1:
                dve.wait(pv_ev, st_ev[par], norm_ev[1-par])
                r1=dve.sig(nc.vector.reciprocal(out=rec[:], in_=l_ps[par][:]))
                dve.wait(r1)
                ins=nc.vector.tensor_tensor(out=ob[par][:], in0=o_ps[par][:], in1=rec[:], op=ALU.mult)
                norm_ev[par]=dve.sig(ins)
                st_ev[par]=k.dma(sp,st[par],attnT[4*h:4*h+4,:,qi*128:(qi+1)*128].rearrange("h d q -> d h q"),
                                 ob[par][:].rearrange("d (h q) -> d h q",h=4),deps=[norm_ev[par]])
                out_evs.append(st_ev[par])
                last_pe_of_head=pv_ev
        gq+=NQT
    while bg is not None and bg(): pass
    return out_evs


def phase_c(k, C, d, NQT, NBLK):
    nc=k.nc; pe=k.pe; act=k.act; dve=k.dve; pool=k.pool; sp=k.sp
    EPS=1e-6; T=NQT; S2=2*T
    ident_f=C['ident_f']
    wo=k.sb([128,16,2048],BF16,"wo"); wst=[k.sb([128,2048],F32,f"wost{i}") for i in range(2)]
    g2r=k.sb([128,2048],F32,"g2r"); wr=k.sb([128,16,36],F32,"wr"); brr=k.sb([128,36],F32,"brr")
    epsb=k.sb([128,1],F32,"epsb2")
    LGT=C['LGT']
    cs=k.dsem("c_const"); wl=[k.dsem(f"c_wl{i}") for i in range(2)]
    k.dma(sp,cs,g2r[:],d['g2r'][:,:]); k.dma(sp,cs,wr[:],d['w_r'][:,:,:]); ev_c=k.dma(sp,cs,brr[:],d['b_r'][:,:])
    e_eps=dve.sig(nc.vector.memset(epsb[:],EPS))
    wev=[None,None]; cev=[None,None]
    for c in range(16):
        s=c%2
        wev[s]=k.dma(sp,wl[s],wst[s][:],d['w_out'][c*128:(c+1)*128,:],deps=[cev[s]])
        q=dve if s==0 else pool
        q.wait(wev[s])
        cev[s]=q.sig((nc.vector if s==0 else nc.gpsimd).tensor_copy(out=wo[:,c,:],in_=wst[s][:]))
    w_done=list(cev)
    mx=[k.sb([128,16,128],BF16,f"mx{i}") for i in range(2)]
    xt=[k.sb([128,2048],F32,f"cxt{i}") for i in range(2)]
    h1=[k.sb([128,2048],F32,f"h1_{i}") for i in range(2)]
    bf=k.sb([128,2048],F32,"bf"); b16=[k.sb([128,2048],BF16,f"b16_{i}") for i in range(2)]
    bT=k.sb([128,16,128],F32,"bT32"); junk=k.sb([128,2048],BF16,"cjunk")
    ssq=k.sb([128,1],F32,"cssq"); rstd=k.sb([128,1],F32,"crstd")
    po=[k.ps([128,512],F32,f"po{i}") for i in range(4)]
    tp=[k.ps([128,4,128],F32,f"ctp{i}") for i in range(2)]
    lp=k.ps([128,36],F32,"lp")
    ld=[k.dsem(f"c_ld{i}") for i in range(2)]; sth=[k.dsem(f"c_sth{i}") for i in range(2)]; stb=[k.dsem(f"c_stb{i}") for i in range(2)]
    mx_free=[None,None]; xt_free=[None,None]; h1_free=[None,None]; b16_free=[None,None]; po_free=[None]*4; tp_free=[None,None]
    bf_free=None; bT_free=None; lp_free=None
    out_evs=[]
    S_={'bf_free':None,'bT_free':None,'lp_free':None}
    def c_tile(i):
            s=i%2
            ev_m=k.dma(sp,ld[s],mx[s][:],d['mixT'][:,:,i*128:(i+1)*128].rearrange("c d t -> d c t"),deps=[mx_free[s]])
            ev_x=k.dma(sp,ld[s],xt[s][:],d['xq'][i*128:(i+1)*128,:],deps=[xt_free[s]])
            pe.wait(ev_x, w_done)
            pev=[]
            for n in range(4):
                pe.wait(po_free[n])
                for c in range(16):
                    ins=nc.tensor.matmul(po[n][:],lhsT=mx[s][:,c,:],rhs=wo[:,c,n*512:(n+1)*512],start=(c==0),stop=(c==15))
                pev.append(pe.sig(ins))
            mx_free[s]=pev[3]
            dve.wait(h1_free[s])
            for n in range(4):
                dve.wait(pev[n])
                e=dve.sig(nc.vector.tensor_tensor(out=h1[s][:,n*512:(n+1)*512],in0=po[n][:],in1=xt[s][:,n*512:(n+1)*512],op=ALU.add))
                po_free[n]=e
            e_h1=e; xt_free[s]=e

            ev_sh=k.dma(sp,sth[s],d['h1'][i*128:(i+1)*128,:],h1[s][:],deps=[e_h1]); out_evs.append(ev_sh)
            def back():
                bf_free=S_['bf_free']; bT_free=S_['bT_free']; lp_free=S_['lp_free']
                act.wait(e_h1, e_eps)
                e=act.sig(nc.scalar.activation(out=junk[:],in_=h1[s][:],func=AF.Square,accum_out=ssq[:])); act.wait(e)
                e=act.sig(nc.scalar.activation(out=rstd[:],in_=ssq[:],func=AF.Sqrt,scale=1.0/2048,bias=epsb[:,0:1])); dve.wait(e)
                e=dve.sig(nc.vector.reciprocal(out=rstd[:],in_=rstd[:])); dve.wait(e, bf_free, ev_c)
                e_bf=dve.sig(nc.vector.scalar_tensor_tensor(out=bf[:],in0=h1[s][:],scalar=rstd[:,0:1],in1=g2r[:],op0=ALU.mult,op1=ALU.mult))
                h1_free[s]=[e_bf,ev_sh]
                pool.wait(e_bf, b16_free[s])
                e_b16=pool.sig(nc.gpsimd.tensor_copy(out=b16[s][:],in_=bf[:]))
                b16_free[s]=k.dma(sp,stb[s],d['b16'][i*128:(i+1)*128,:],b16[s][:],deps=[e_b16]); out_evs.append(b16_free[s])
                pe.wait(e_bf)
                tev=[]
                for gch in range(4):
                    j=gch%2
                    pe.wait(tp_free[j])
                    for c4 in range(4):
                        c=gch*4+c4
                        ins=nc.tensor.transpose(tp[j][:,c4,:],bf[:,c*128:(c+1)*128],ident_f[:])
                    e_t=pe.sig(ins)
                    q=dve if j==0 else act
                    q.wait(e_t, bT_free)
                    if j==0: e=dve.sig(nc.vector.tensor_copy(out=bT[:,gch*4:gch*4+4,:],in_=tp[j][:]))
                    else: e=act.sig(nc.scalar.copy(out=bT[:,gch*4:gch*4+4,:],in_=tp[j][:]))
                    tp_free[j]=e; tev.append(e)
                bf_free=[e_t,e_b16]
                pe.wait(*tev); pe.wait(lp_free, ev_c)
                for c in range(16):
                    ins=nc.tensor.matmul(lp[:],lhsT=bT[:,c,:],rhs=wr[:,c,:],start=(c==0),stop=(c==15))
                e_l=pe.sig(ins); bT_free=e_l
                dve.wait(e_l)
                lp_free=dve.sig(nc.vector.tensor_tensor(out=LGT[:,i,:],in0=lp[:],in1=brr[:],op=ALU.add))

                S_['bf_free']=bf_free; S_['bT_free']=bT_free; S_['lp_free']=lp_free
            return back
    pending=None
    for i in range(T):
        b_=c_tile(i)
        if pending is not None: pending()
        pending=b_
    pending()
    return out_evs

def phase_c2(k, C, d, NQT, NBLK):
    nc=k.nc; pe=k.pe; act=k.act; dve=k.dve; pool=k.pool; sp=k.sp
    T=NQT; S2=2*T; LGT=C['LGT']
    po=[k.ps([128,512],F32,f"c2po{i}") for i in range(2)]; po_free=[None,None]
    junk=k.sb([128,2048],BF16,"c2junk"); b16=[k.sb([128,2048],BF16,f"c2b16_{i}") for i in range(2)]
    ld=[k.dsem(f"c2_ld{i}") for i in range(2)]
    lp_free=None; out_evs=[]
    V=nc.vector
    def dv(ins, *w):
        return dve.sig(ins)
    cnt=[0]
    def T_(shape,dt=F32):
        cnt[0]+=1; return k.sb(shape,dt,f"rt{cnt[0]}")
    LG=LGT[:,:,0:4]; LE=LGT[:,:,4:36]
    gmax=T_([128,T]); dd=T_([128,T,4]); ohg=T_([128,T,4]); eg=T_([128,T,4]); sg=T_([128,T]); gp=T_([128,T])
    tmp=T_([128,T,32]); sel=T_([128,T,8]); v1=T_([128,T]); m1=T_([128,T,8]); sel2=T_([128,T,8]); v2=T_([128,T]); m2=T_([128,T,8])
    rr=T_([128,T]); g1_=T_([128,T]); Mall=T_([128,S2,32]); Mbf=T_([128,S2,32],BF16)
    Utri=T_([128,128],BF16); onesb=T_([128,128],BF16)
    rs=k.dsem("c_rs")
    k.dma(sp,rs,Utri[:],d['Utri'][:,:]); ev_u=k.dma(sp,rs,onesb[:],d['ones_bf'][:,:])
    thr=T_([128,32]); blkst=T_([128,NBLK])
    k.dma(sp,rs,thr[:],d['thr'][:,:]); ev_u=k.dma(sp,rs,blkst[:],d['blkst'][:,:])
    dve.wait(lp_free)
    def op(ins):
        e=dve.sig(ins); dve.wait(e); return e
    op(V.tensor_reduce(out=gmax[:],in_=LG,axis=AX.X,op=ALU.max))
    op(V.tensor_tensor(out=dd[:],in0=LG,in1=gmax[:].unsqueeze(2).to_broadcast([128,T,4]),op=ALU.subtract))
    op(V.tensor_single_scalar(out=ohg[:],in_=dd[:],scalar=0.0,op=ALU.is_ge))
    e=op(V.tensor_copy(out=eg[:],in_=dd[:]))
    act.wait(e); e=act.sig(nc.scalar.activation(out=eg[:],in_=dd[:],func=AF.Exp)); dve.wait(e)
    op(V.tensor_reduce(out=sg[:],in_=eg[:],axis=AX.X,op=ALU.add))
    op(V.reciprocal(out=gp[:],in_=sg[:]))
    op(V.tensor_tensor(out=tmp[:].rearrange("p t (g e) -> p t g e",g=4),in0=LE.rearrange("p t (g e) -> p t g e",g=4),in1=ohg[:].unsqueeze(3).to_broadcast([128,T,4,8]),op=ALU.mult))
    op(V.tensor_reduce(out=sel[:],in_=tmp[:].rearrange("p t (g e) -> p t e g",g=4),axis=AX.X,op=ALU.add))
    op(V.tensor_reduce(out=v1[:],in_=sel[:],axis=AX.X,op=ALU.max))
    op(V.tensor_tensor(out=m1[:],in0=sel[:],in1=v1[:].unsqueeze(2).to_broadcast([128,T,8]),op=ALU.is_ge))
    op(V.scalar_tensor_tensor(out=sel2[:],in0=m1[:],scalar=-1e30,in1=sel[:],op0=ALU.mult,op1=ALU.add))
    op(V.tensor_reduce(out=v2[:],in_=sel2[:],axis=AX.X,op=ALU.max))
    op(V.tensor_tensor(out=m2[:],in0=sel2[:],in1=v2[:].unsqueeze(2).to_broadcast([128,T,8]),op=ALU.is_ge))
    e=op(V.tensor_tensor(out=rr[:],in0=v2[:],in1=v1[:],op=ALU.subtract))
    act.wait(e); e=act.sig(nc.scalar.activation(out=rr[:],in_=rr[:],func=AF.Exp)); dve.wait(e)
    op(V.tensor_scalar(out=rr[:],in0=rr[:],scalar1=1.0,scalar2=None,op0=ALU.add))
    op(V.reciprocal(out=rr[:],in_=rr[:]))
    gates=C['gates']
    op(V.tensor_tensor(out=gates[:,:,0],in0=gp[:],in1=rr[:],op=ALU.mult))
    op(V.tensor_tensor(out=gates[:,:,1],in0=gp[:],in1=gates[:,:,0],op=ALU.subtract))
    M4=Mall[:].rearrange("p (t k) (g e) -> p t k g e",k=2,g=4)
    for kk,mk in enumerate((m1,m2)):
        op(V.tensor_tensor(out=M4[:,:,kk,:,:],in0=ohg[:].unsqueeze(3).to_broadcast([128,T,4,8]),in1=mk[:].unsqueeze(2).to_broadcast([128,T,4,8]),op=ALU.mult))
    e_mb=op(V.tensor_copy(out=Mbf[:],in_=Mall[:]))
    R=T_([128,S2,32]); Tot=T_([128,S2,32])
    nch=(S2*32+511)//512
    pe.wait(e_mb, ev_u)
    Mflat=Mbf[:].rearrange("p s e -> p (s e)"); Rflat=R[:].rearrange("p s e -> p (s e)"); Tflat=Tot[:].rearrange("p s e -> p (s e)")
    for c in range(nch):
        w=min(512,S2*32-c*512)
        pe.wait(po_free[0],po_free[1])
        ins=nc.tensor.matmul(po[0][:,0:w],lhsT=Utri[:],rhs=Mflat[:,c*512:c*512+w],start=True,stop=True)
        ins=nc.tensor.matmul(po[1][:,0:w],lhsT=onesb[:],rhs=Mflat[:,c*512:c*512+w],start=True,stop=True)
        e=pe.sig(ins); dve.wait(e)
        op(V.tensor_copy(out=Rflat[:,c*512:c*512+w],in_=po[0][:,0:w]))
        e=op(V.tensor_copy(out=Tflat[:,c*512:c*512+w],in_=po[1][:,0:w]))
        po_free[0]=e; po_free[1]=e
    base=T_([128,S2+1,32])
    op(V.memset(base[:,0,:],0.0))
    for s_ in range(S2):
        op(V.tensor_tensor(out=base[:,s_+1,:],in0=base[:,s_,:],in1=Tot[:,s_,:],op=ALU.add))
    counts=base[:,S2,:]
    cmp=T_([128,32,32]); nb=T_([128,32]); padded=T_([128,32]); pst=T_([128,33])
    op(V.tensor_tensor(out=cmp[:],in0=counts.unsqueeze(2).to_broadcast([128,32,32]),in1=thr[:].unsqueeze(1).to_broadcast([128,32,32]),op=ALU.is_gt))
    op(V.tensor_reduce(out=nb[:],in_=cmp[:],axis=AX.X,op=ALU.add))
    op(V.tensor_scalar(out=padded[:],in0=nb[:],scalar1=128.0,scalar2=None,op0=ALU.mult))
    op(V.memset(pst[:,0:1],0.0))
    for e_ in range(32):
        op(V.tensor_tensor(out=pst[:,e_+1:e_+2],in0=pst[:,e_:e_+1],in1=padded[:,e_:e_+1],op=ALU.add))
    RB=T_([128,S2,32]); posf=T_([128,S2])
    op(V.tensor_tensor(out=RB[:],in0=R[:],in1=base[:,0:S2,:],op=ALU.add))
    op(V.tensor_tensor(out=RB[:],in0=RB[:],in1=pst[:,0:32].unsqueeze(1).to_broadcast([128,S2,32]),op=ALU.add))
    op(V.tensor_tensor(out=RB[:],in0=RB[:],in1=Mall[:],op=ALU.mult))
    op(V.tensor_reduce(out=posf[:],in_=RB[:],axis=AX.X,op=ALU.add))
    e_pos=op(V.tensor_copy(out=C['pos_i'][:],in_=posf[:]))
    cmp2=T_([128,NBLK,32]); bef=T_([128,NBLK])
    op(V.tensor_tensor(out=cmp2[:],in0=pst[:,1:33].unsqueeze(1).to_broadcast([128,NBLK,32]),in1=blkst[:].unsqueeze(2).to_broadcast([128,NBLK,32]),op=ALU.is_le))
    op(V.tensor_reduce(out=bef[:],in_=cmp2[:],axis=AX.X,op=ALU.add))
    op(V.tensor_scalar(out=bef[:],in0=bef[:],scalar1=31.0,scalar2=None,op0=ALU.min))
    e_blk=op(V.tensor_copy(out=C['blk_e'][:],in_=bef[:]))
    sc=k.dsem("c_scat"); zs=k.dsem("c_zero")
    e_z=dve.sig(nc.vector.memset(junk[:],0.0))
    zev=None
    for r0 in range(0,NBLK,8):
        nb_=min(8,NBLK-r0)
        zev=k.dma(sp,zs,d['xs'][r0*128:(r0+nb_)*128,:].rearrange("(r p) n -> p r n",p=128),junk[:].unsqueeze(1).to_broadcast([128,nb_,2048]),deps=[e_z])
    pool.wait(zev)
    sp.wait(*out_evs)
    pool.wait(e_pos)
    scat_free=[None,None]
    for i in range(T):
        s=i%2
        ev=k.dma(sp,ld[s],b16[s][:],d['b16'][i*128:(i+1)*128,:],deps=[scat_free[s]]+out_evs)
        pool.wait(ev)
        for kk in range(2):
            ins=nc.gpsimd.indirect_dma_start(out=d['xs'][:,:],out_offset=bass.IndirectOffsetOnAxis(ap=C['pos_i'][:,i*2+kk:i*2+kk+1],axis=0),in_=b16[s][:],in_offset=None)
            ins.then_inc(sc.h,16); sc.cnt+=16
        scat_free[s]=(sc,sc.cnt)
        pool.wait(scat_free[s])
    return [(sc,sc.cnt), e_blk, e_pos]


class WConv:
    def __init__(self, k, d):
        self.k=k; self.d=d; self.n=0
        self.st=[k.sb([128,4096],F32,f"wc_st{i}") for i in range(2)]; self.bf=[k.sb([128,4096],BF16,f"wc_bf{i}") for i in range(2)]
        self.ld=[k.dsem(f"wc_ld{i}") for i in range(2)]; self.so=[k.dsem(f"wc_so{i}") for i in range(2)]
        self.st_free=[None,None]; self.bf_free=[None,None]; self.evs=[]
        self.jobs=[(e,mi,half) for e in range(32) for mi in range(3) for half in range(2)]
    def step(self):
        if self.n>=len(self.jobs): return False
        k=self.k; nc=k.nc; d=self.d
        e,mi,half=self.jobs[self.n]; s=self.n%2; self.n+=1
        src=(d['w_gate'],d['w_up'],d['w_down'])[mi]; dst=(d['wg_bf'],d['wu_bf'],d['wd_bf'])[mi]
        C_=src.shape[1]//128; F=src.shape[2]; c0=half*C_//2; c1=c0+C_//2
        ev=k.dma(k.sp,self.ld[s],self.st[s][:].rearrange("p (c f) -> p c f",c=C_//2),src[e,c0*128:c1*128,:].rearrange("(c p) f -> p c f",p=128),deps=[self.st_free[s]])
        cev=[]
        for (q,eng,lo,hi) in ((k.dve,nc.vector,0,3072),(k.pool,nc.gpsimd,3072,4096)):
            q.wait(ev,self.bf_free[s])
            cev.append(q.sig(eng.tensor_copy(out=self.bf[s][:,lo:hi],in_=self.st[s][:,lo:hi])))
        self.st_free[s]=cev
        self.bf_free[s]=k.dma(k.sp,self.so[s],dst[e*128:(e+1)*128,c0*F:c0*F+4096],self.bf[s][:],deps=cev); self.evs.append(self.bf_free[s])
        return True

DEBUG_STATIC_W=False
def phase_d(k, C, d, NBLK, deps=()):
    nc=k.nc; pe=k.pe; act=k.act; dve=k.dve; pool=k.pool; sp=k.sp
    ident=C['ident_bf']
    wg=[k.sb([128,16*512],BF16,f"wg{i}") for i in range(2)]; wu=[k.sb([128,16*512],BF16,f"wu{i}") for i in range(2)]
    wd=[k.sb([128,4*2048],BF16,f"wd{i}") for i in range(2)]
    xs=[k.sb([128,2048],BF16,f"xs{i}") for i in range(2)]; xsT=k.sb([128,16,128],BF16,"xsT")
    sg=k.sb([128,512],F32,"sg"); hb=k.sb([128,512],BF16,"hb"); hT=k.sb([128,4,128],BF16,"hT")
    ysb=[k.sb([128,2048],F32,f"ysb{i}") for i in range(2)]
    idxi=k.sb([128,NBLK],I32,"idxi"); befl=k.sb([128,NBLK],F32,"befl")
    tp=[k.ps([128,8,128],BF16,f"dtp{i}") for i in range(2)]
    pg=k.ps([128,512],F32,"pg"); pu=k.ps([128,512],F32,"pu"); pd=[k.ps([128,512],F32,f"pd{i}") for i in range(4)]
    wl=[k.dsem(f"d_wl{i}") for i in range(2)]; xl=[k.dsem(f"d_xl{i}") for i in range(2)]; ys=[k.dsem(f"d_ys{i}") for i in range(2)]
    dve.wait(*deps)
    e=dve.sig(nc.vector.tensor_copy(out=befl[:],in_=C['blk_e'][:])); dve.wait(e)
    e=dve.sig(nc.vector.tensor_scalar(out=befl[:],in0=befl[:],scalar1=128.0,scalar2=None,op0=ALU.mult)); dve.wait(e)
    e=dve.sig(nc.vector.tensor_scalar(out=befl[:],in0=befl[:],scalar1=C['iota_p'][:,0:1],scalar2=None,op0=ALU.add)); dve.wait(e)
    e_idx=dve.sig(nc.vector.tensor_copy(out=idxi[:],in_=befl[:]))
    wgv=d['wg_bf'][:,:]; wuv=d['wu_bf'][:,:]; wdv=d['wd_bf'][:,:]
    w_free=[None,None]; xs_free=[None,None]; y_free=[None,None]
    wev=[None]*NBLK; xev=[None]*NBLK
    def issue_loads(b):
        s=b%2
        pool.wait(e_idx, w_free[s], *deps)
        if DEBUG_STATIC_W:
            for (dst,src) in ((wg[s],d['wg_bf']),(wu[s],d['wu_bf']),(wd[s],d['wd_bf'])):
                wev[b]=k.dma(sp,wl[s],dst[:],src[(b%32)*128:(b%32+1)*128,:],deps=[w_free[s]]+list(deps))
            xev[b]=k.dma(sp,xl[s],xs[s][:],d['xs'][b*128:(b+1)*128,:],deps=[xs_free[s]]+list(deps))
            return
        for (dst,src) in ((wg[s],wgv),(wu[s],wuv),(wd[s],wdv)):
            ins=nc.gpsimd.indirect_dma_start(out=dst[:],out_offset=None,in_=src,in_offset=bass.IndirectOffsetOnAxis(ap=idxi[:,b:b+1],axis=0))
            ins.then_inc(wl[s].h,16); wl[s].cnt+=16
        wev[b]=(wl[s],wl[s].cnt)
        xev[b]=k.dma(sp,xl[s],xs[s][:],d['xs'][b*128:(b+1)*128,:],deps=[xs_free[s]]+list(deps))
    issue_loads(0)
    out_evs=[]
    xsT_free=None; hT_free=None; sg_free=None; hb_free=None; pd_free=None; tp_free=[None,None]; pgu_free=None
    for b in range(NBLK):
        s=b%2
        if b+1<NBLK: issue_loads(b+1)
        pe.wait(xev[b])
        tev=[]
        for half in range(2):
            pe.wait(tp_free[half])
            for c8 in range(8):
                c=half*8+c8
                ins=nc.tensor.transpose(tp[half][:,c8,:],xs[s][:,c*128:(c+1)*128],ident[:])
            tev.append(pe.sig(ins))
        xs_free[s]=tev[1]
        dve.wait(tev[0], xsT_free); e0=dve.sig(nc.vector.tensor_copy(out=xsT[:,0:8,:],in_=tp[0][:]))
        act.wait(tev[1], xsT_free); e1=act.sig(nc.scalar.copy(out=xsT[:,8:16,:],in_=tp[1][:]))
        tp_free=[e0,e1]
        pe.wait(e0,e1,wev[b],pgu_free)
        for c in range(16):
            nc.tensor.matmul(pg[:],lhsT=xsT[:,c,:],rhs=wg[s][:,c*512:(c+1)*512],start=(c==0),stop=(c==15))
        e_g=pe.sig(nc.tensor.matmul(pg[:],lhsT=xsT[:,0,:],rhs=wg[s][:,0:512],start=False,stop=True,skip_group_check=True)) if False else None
        for c in range(16):
            ins=nc.tensor.matmul(pu[:],lhsT=xsT[:,c,:],rhs=wu[s][:,c*512:(c+1)*512],start=(c==0),stop=(c==15))
        e_u=pe.sig(ins); xsT_free=e_u
        act.wait(e_u, sg_free)
        e_s=act.sig(nc.scalar.activation(out=sg[:],in_=pg[:],func=AF.Silu))
        dve.wait(e_s, hb_free)
        e_h=dve.sig(nc.vector.tensor_tensor(out=hb[:],in0=sg[:],in1=pu[:],op=ALU.mult))
        sg_free=e_h; pgu_free=e_h
        pe.wait(e_h, tp_free[0])
        for c in range(4):
            ins=nc.tensor.transpose(tp[0][:,c,:],hb[:,c*128:(c+1)*128],ident[:])
        e_t=pe.sig(ins); hb_free=e_t
        dve.wait(e_t, hT_free)
        e_c=dve.sig(nc.vector.tensor_copy(out=hT[:],in_=tp[0][:,0:4,:]))
        tp_free[0]=e_c
        pe.wait(e_c, pd_free)
        dev=[]
        for n in range(4):
            for c in range(4):
                ins=nc.tensor.matmul(pd[n][:],lhsT=hT[:,c,:],rhs=wd[s][:,c*2048+n*512:c*2048+(n+1)*512],start=(c==0),stop=(c==3))
            dev.append(pe.sig(ins))
        hT_free=dev[3]; w_free[s]=dev[3]
        evs_=[]
        for n in range(4):
            q=dve if n%2==0 else act
            q.wait(dev[n], y_free[s])
            if n%2==0: e=dve.sig(nc.vector.tensor_copy(out=ysb[s][:,n*512:(n+1)*512],in_=pd[n][:]))
            else: e=act.sig(nc.scalar.copy(out=ysb[s][:,n*512:(n+1)*512],in_=pd[n][:]))
            evs_.append(e)
        pd_free=evs_
        y_free[s]=k.dma(sp,ys[s],d['Y'][b*128:(b+1)*128,:],ysb[s][:],deps=evs_)
        out_evs.append(y_free[s])
    return out_evs

def phase_e(k, C, d, NQT, deps=()):
    nc=k.nc; dve=k.dve; pool=k.pool; sp=k.sp
    h1=[k.sb([128,2048],F32,f"eh1_{i}") for i in range(2)]; y0=[k.sb([128,2048],F32,f"ey0_{i}") for i in range(2)]; y1=[k.sb([128,2048],F32,f"ey1_{i}") for i in range(2)]
    ob=[k.sb([128,2048],F32,f"eo_{i}") for i in range(2)]
    ld=[k.dsem(f"e_ld{i}") for i in range(2)]; gl=[k.dsem(f"e_gl{i}") for i in range(2)]; st=[k.dsem(f"e_st{i}") for i in range(2)]
    in_free=[None,None]; o_free=[None,None]; out_evs=[]
    gates=C['gates']
    for i in range(NQT):
        s=i%2
        ev_h=k.dma(sp,ld[s],h1[s][:],d['h1'][i*128:(i+1)*128,:],deps=[in_free[s]]+list(deps))
        pool.wait(in_free[s], *deps)
        for kk,dst in enumerate((y0[s],y1[s])):
            ins=nc.gpsimd.indirect_dma_start(out=dst[:],out_offset=None,in_=d['Y'][:,:],in_offset=bass.IndirectOffsetOnAxis(ap=C['pos_i'][:,i*2+kk:i*2+kk+1],axis=0))
            ins.then_inc(gl[s].h,16); gl[s].cnt+=16
        ev_g=(gl[s],gl[s].cnt)
        pool.wait(ev_g)
        dve.wait(ev_h,ev_g,o_free[s])
        e=dve.sig(nc.vector.scalar_tensor_tensor(out=ob[s][:],in0=y0[s][:],scalar=gates[:,i,0:1],in1=h1[s][:],op0=ALU.mult,op1=ALU.add)); dve.wait(e)
        e=dve.sig(nc.vector.scalar_tensor_tensor(out=ob[s][:],in0=y1[s][:],scalar=gates[:,i,1:2],in1=ob[s][:],op0=ALU.mult,op1=ALU.add))
        in_free[s]=e
        o_free[s]=k.dma(sp,st[s],d['out'][i*128:(i+1)*128,:],ob[s][:],deps=[e]); out_evs.append(o_free[s])
    return out_evs


def build(NKVT, NQT, PART=16):
    NQ=NQT*128; NK=NKVT*128+PART; NBLK=2*NQT+32
    nc=bass.Bass("TRN2", target_bir_lowering=False)
    def din(name,shape,dt=F32): return nc.dram_tensor(name,list(shape),dt,kind="ExternalInput").ap()
    def scr(name,shape,dt): return nc.dram_tensor(name,list(shape),dt).ap()
    d={}
    d['xkv']=din('xkv',[NKVT*128,2048]); d['xq']=din('xq',[NQ,2048]); d['xh']=din('xh',[128,2048]); d['meta']=din('meta',[128,2048])
    d['g1t']=din('g1t',[128,16]); d['gqr']=din('gqr',[128,128]); d['gkr']=din('gkr',[128,128])
    d['pool_w']=din('pool_w',[128,8,256]); d['pool_sc']=din('pool_sc',[128,8]); d['Aband']=din('Aband',[128,24,128],BF16)
    d['ident_bf']=din('ident_bf',[128,128],BF16); d['ident_f']=din('ident_f',[128,128]); d['ones_bf']=din('ones_bf',[128,128],BF16)
    d['w_in']=din('w_in',[2048,2560])
    d['ckv']=din('ckv',[(NKVT+1)*128,128]); d['skv']=din('skv',[(NKVT+1)*128,128]); d['cq']=din('cq',[NQ,128]); d['sq']=din('sq',[NQ,128])
    d['w_out']=din('w_out',[2048,2048]); d['g2r']=din('g2r',[128,2048]); d['w_r']=din('w_r',[128,16,36]); d['b_r']=din('b_r',[128,36])
    d['Utri']=din('Utri',[128,128],BF16); d['thr']=din('thr',[128,32]); d['blkst']=din('blkst',[128,NBLK]); d['iota_p']=din('iota_p',[128,1])
    d['w_gate']=din('w_gate',[32,2048,512]); d['w_up']=din('w_up',[32,2048,512]); d['w_down']=din('w_down',[32,512,2048])
    d['out']=nc.dram_tensor('out',[NQ,2048],F32,kind="ExternalOutput").ap()
    d['KT']=scr('KT',[2,128,NK],BF16); d['V']=scr('V',[NKVT+1,128,256],BF16); d['QT']=scr('QT',[8,128,NQ],BF16); d['mixT']=scr('mixT',[16,128,NQ],BF16)
    d['h1']=scr('h1',[NQ,2048],F32); d['b16']=scr('b16',[NQ,2048],BF16); d['xs']=scr('xs',[NBLK*128,2048],BF16); d['Y']=scr('Y',[NBLK*128,2048],F32)
    d['wg_bf']=scr('wg_bf',[4096,8192],BF16); d['wu_bf']=scr('wu_bf',[4096,8192],BF16); d['wd_bf']=scr('wd_bf',[4096,8192],BF16)
    with ExitStack() as es:
        k=K(nc,es); C={}
        C['ident_bf']=k.sb([128,128],BF16,"c_ident_bf"); C['ident_f']=k.sb([128,128],F32,"c_ident_f"); C['ones_bf']=k.sb([128,128],BF16,"c_ones_bf")
        C['iota_p']=k.sb([128,1],F32,"c_iota_p"); C['nbias']=k.sb([128,1],F32,"c_nbias")
        C['LGT']=k.sb([128,NQT,36],F32,"c_LGT"); C['pos_i']=k.sb([128,2*NQT],I32,"c_pos_i"); C['gates']=k.sb([128,NQT,2],F32,"c_gates"); C['blk_e']=k.sb([128,NBLK],I32,"c_blk_e")
        gqa=k.sb([128,128],F32,"c_gqa"); gka=k.sb([128,128],F32,"c_gka"); mq=k.sb([128,1],F32,"c_mq"); mk=k.sb([128,1],F32,"c_mk")
        s0=k.dsem("c0")
        k.dma(k.sp,s0,C['ones_bf'][:],d['ones_bf'][:,:]); k.dma(k.sp,s0,C['iota_p'][:],d['iota_p'][:,:])
        k.dma(k.sp,s0,gqa[:],d['gqr'][:,:]); ev=k.dma(k.sp,s0,gka[:],d['gkr'][:,:])
        dve=k.dve; V=nc.vector
        dve.wait(ev)
        def op(ins):
            e=dve.sig(ins); dve.wait(e); return e
        m2=k.sb([128,2],F32,"c_m2")
        for (ga,mm,col) in ((gqa,mq,0),(gka,mk,1)):
            op(V.tensor_reduce(out=mm[:],in_=ga[:],axis=AX.X,op=ALU.max))
            op(V.tensor_scalar(out=ga[:],in0=ga[:],scalar1=-1.0,scalar2=None,op0=ALU.mult))
            op(V.tensor_reduce(out=m2[:,col:col+1],in_=ga[:],axis=AX.X,op=ALU.max))
            op(V.tensor_tensor(out=mm[:],in0=mm[:],in1=m2[:,col:col+1],op=ALU.max))
        op(V.tensor_tensor(out=mq[:],in0=mq[:],in1=mk[:],op=ALU.mult))
        e_nb=op(V.tensor_scalar(out=C['nbias'][:],in0=mq[:],scalar1=-(128.0**0.5),scalar2=None,op0=ALU.mult))
        k.begin_phase(); evs=phase_a(k,C,d,NKVT,NQT); k.end_phase(evs)
        k.begin_phase(); wc=WConv(k,d)
        evs=attention_phase(k,d['QT'],d['KT'],d['V'],d['mixT'],C['nbias'],C['ones_bf'],NQT,NKVT,PART,128.0**-0.5,deps=[e_nb,(s0,s0.cnt)],bg=wc.step,bg_every=max(1,(2*NQT*(NKVT+1))//200))
        k.end_phase(evs+wc.evs)
        k.begin_phase(); evs=phase_c(k,C,d,NQT,NBLK); k.end_phase(evs)
        k.begin_phase(); evs=phase_c2(k,C,d,NQT,NBLK); k.end_phase(evs)
        k.begin_phase(); evs=phase_d(k,C,d,NBLK); k.end_phase(evs)
        k.begin_phase(); evs=phase_e(k,C,d,NQT); k.end_phase(evs)
    return nc

def const_inputs(j, NKVT, NQT, r0, NBLK):
    BF_=BF
    c={}
    r=np.arange(NKVT*128)
    Ck,Sk=rope_tables((r//GRID_W).astype(np.float32),(r%GRID_W).astype(np.float32))
    Cm,Sm=rope_tables(np.zeros(128,np.float32),np.zeros(128,np.float32))
    c['ckv']=np.concatenate([Ck,Cm]); c['skv']=np.concatenate([Sk,Sm])
    c['cq']=np.ascontiguousarray(Ck[r0:r0+NQT*128]); c['sq']=np.ascontiguousarray(Sk[r0:r0+NQT*128])
    c['Aband']=band_mats(j,NQT)
    c['ident_bf']=np.eye(128).astype(BF_); c['ident_f']=np.eye(128,dtype=np.float32); c['ones_bf']=np.ones((128,128),BF_)
    c['Utri']=np.triu(np.ones((128,128),np.float32),1).astype(BF_)
    c['thr']=np.tile((128*np.arange(32,dtype=np.float32))[None],(128,1)); c['blkst']=np.tile((128*np.arange(NBLK,dtype=np.float32))[None],(128,1))
    c['iota_p']=np.arange(128,dtype=np.float32).reshape(128,1)
    return c

def layout_params(p):
    f=lambda a: np.ascontiguousarray(np.asarray(a,dtype=np.float32))
    o={}
    o['g1t']=f(np.asarray(p['norm1_g'])[0].reshape(16,128).T)
    o['gqr']=f(np.tile(np.asarray(p['q_norm_g'])[0][None],(128,1))); o['gkr']=f(np.tile(np.asarray(p['k_norm_g'])[0][None],(128,1)))
    o['pool_w']=f(np.asarray(p['pool_w'])[0].reshape(4,2,128,256).transpose(2,0,1,3).reshape(128,8,256))
    o['pool_sc']=f(np.asarray(p['pool_scale'])[0].reshape(8,128).T)
    o['w_in']=f(np.asarray(p['w_in'])[0]); o['w_out']=f(np.asarray(p['w_out'])[0])
    o['g2r']=f(np.tile(np.asarray(p['norm2_g'])[0][None],(128,1)))
    wr=np.concatenate([np.asarray(p['w_router_group'])[0],np.asarray(p['w_router_expert'])[0]],1)
    o['w_r']=f(wr.reshape(16,128,36).transpose(1,0,2))
    o['b_r']=f(np.tile(np.concatenate([np.asarray(p['b_router_group'])[0],np.asarray(p['b_router_expert'])[0]])[None],(128,1)))
    o['w_gate']=f(np.asarray(p['w_gate'])[0]); o['w_up']=f(np.asarray(p['w_up'])[0]); o['w_down']=f(np.asarray(p['w_down'])[0])
    return o

def kernel(**inputs):
    x=np.asarray(inputs['x'],dtype=np.float32); meta=np.asarray(inputs['meta_tokens'],dtype=np.float32)
    B,S,D=x.shape
    NKVT=S//128; NQT=NKVT//4; NBLK=2*NQT+32
    P=layout_params(inputs)
    mp=np.zeros((128,D),np.float32); mp[:N_META]=meta
    nc=build(NKVT,NQT)
    in_maps=[]
    for c in range(8):
        b=c//4; j=c%4; r0=j*NQT*128; r1=r0+NQT*128
        m=dict(P); m.update(const_inputs(j,NKVT,NQT,r0,NBLK))
        m['xkv']=np.ascontiguousarray(x[b]); m['xq']=np.ascontiguousarray(x[b,r0:r1]); m['meta']=mp
        xh=np.zeros((128,D),np.float32)
        xh[0:8]=meta[8:16] if j==0 else x[b,r0-8:r0]
        if j<3: xh[8:16]=x[b,r1:r1+8]
        m['xh']=xh
        in_maps.append(m)
    res=run_bass_kernel_spmd(nc,in_maps,core_ids=list(range(8)))
    out=np.zeros((B,S,D),np.float32)
    for c in range(8):
        b=c//4; j=c%4; r0=j*NQT*128
        out[b,r0:r0+NQT*128]=np.asarray(res.results[c]['out'],dtype=np.float32)
    return out
```
